# Optimizing a Trainium2 kernel written in Bass

```python
import jax, jax.numpy as jnp
from jax import lax
import numpy as np

D_MODEL = 2048
BATCH = 2
SEQ = 8192
DEPTH = 1

CHUNK = 64
SB_HEADS = 8
SB_HEAD_DIM = 128
SB_WIDTH = SB_HEADS * SB_HEAD_DIM
Q_BLOCK = 128
SGU_GROUPS = 8
SGU_GROUP_DIM = 128
SGU_WIDTH = SGU_GROUPS * SGU_GROUP_DIM
SGU_BLOCK = 128
N_BRANCHES = 2
IN_COLS = 3 * SB_WIDTH + 2 * SGU_WIDTH + N_BRANCHES * D_MODEL
N_GROUPS = 4
EXPERTS_PER_GROUP = 8
N_EXPERTS = N_GROUPS * EXPERTS_PER_GROUP
TOP_K_INNER = 2
D_EXPERT = 512
EXPERT_BLOCK = 128
LN_EPS = 1e-5
DEEPNORM_ALPHA = (2.0 * DEPTH) ** 0.25
DEEPNORM_BETA = (8.0 * DEPTH) ** -0.25

kernel_name = "chunk_causal_hybrid_sb_sgu_hmoe_deepnorm"


def layer_norm(x, gain=None, bias=None):
    xf = x.astype(jnp.float32)
    mu = jnp.mean(xf, axis=-1, keepdims=True)
    var = jnp.mean(jnp.square(xf - mu), axis=-1, keepdims=True)
    y = (xf - mu) * lax.rsqrt(var + LN_EPS)
    if gain is not None:
        y = y * gain.astype(jnp.float32) + bias.astype(jnp.float32)
    return y.astype(x.dtype)


def modulate(x, shift, scale):
    return layer_norm(x) * (1 + scale[:, None, :]) + shift[:, None, :]


def stick_breaking_attention(q, k, v):
    b, h, s, dh = q.shape
    n_qb = s // Q_BLOCK
    kf = k.astype(jnp.float32)
    vf = v.astype(jnp.float32)
    qb = q.astype(jnp.float32).reshape(b, h, n_qb, Q_BLOCK, dh).transpose(2, 0, 1, 3, 4)
    key_pos = jnp.arange(s)
    scale = dh ** -0.5

    def one_block(args):
        q_i, blk = args
        z = jnp.einsum('bhqd,bhkd->bhqk', q_i, kf) * scale
        q_pos = blk * Q_BLOCK + jnp.arange(Q_BLOCK)
        causal = key_pos[None, :] < q_pos[:, None]
        log_keep = jnp.where(causal, jax.nn.log_sigmoid(-z), 0.0)
        between = lax.cumsum(log_keep, axis=3, reverse=True) - log_keep
        w = jnp.where(causal, jnp.exp(jax.nn.log_sigmoid(z) + between), 0.0)
        return jnp.einsum('bhqk,bhkd->bhqd', w, vf)

    out = lax.map(one_block, (qb, jnp.arange(n_qb)))
    return out.transpose(1, 0, 3, 2, 4).reshape(b, s, h * dh).astype(q.dtype)


def spatial_gating(u, v, w_s, b_s, ln_g, ln_b):
    b, s, _ = v.shape
    n_blk = s // SGU_BLOCK
    v = layer_norm(v, ln_g, ln_b)
    pos = jnp.arange(SGU_BLOCK)
    mask = (pos[None, :] // CHUNK) <= (pos[:, None] // CHUNK)
    w = jnp.where(mask[None], w_s, 0.0).astype(v.dtype)
    vb = v.reshape(b, n_blk, SGU_BLOCK, SGU_GROUPS, SGU_GROUP_DIM)
    mixed = jnp.einsum('gts,bnsgd->bntgd', w, vb) + b_s.T[None, None, :, :, None]
    return u * mixed.reshape(b, s, SGU_WIDTH)


def hierarchical_moe(h, w_group, b_group, w_router, b_router, w_gate, w_up, w_down):
    b, s, d = h.shape
    n = b * s
    xf = h.reshape(n, d)
    group_logits = (xf @ w_group).astype(jnp.float32) + b_group.astype(jnp.float32)
    group_prob = jax.nn.softmax(group_logits, axis=-1)
    _, g_sel = lax.top_k(group_logits, 1)
    p_group = jnp.take_along_axis(group_prob, g_sel, axis=1)
    expert_logits = ((xf @ w_router).astype(jnp.float32) + b_router.astype(jnp.float32)
                     ).reshape(n, N_GROUPS, EXPERTS_PER_GROUP)
    in_group = jnp.take_along_axis(expert_logits, g_sel[:, :, None], axis=1)[:, 0]
    top_val, top_idx = lax.top_k(in_group, TOP_K_INNER)
    weights = p_group * jax.nn.softmax(top_val, axis=-1)
    expert_id = g_sel * EXPERTS_PER_GROUP + top_idx

    a = n * TOP_K_INNER
    exp_flat = expert_id.reshape(a)
    tok_flat = jnp.arange(a) // TOP_K_INNER
    w_flat = weights.reshape(a)
    order = jnp.argsort(exp_flat)
    exp_sorted = exp_flat[order]
    tok_sorted = tok_flat[order]
    w_sorted = w_flat[order]
    counts = jax.ops.segment_sum(jnp.ones((a,), jnp.int32), exp_flat, num_segments=N_EXPERTS)
    padded = (counts + EXPERT_BLOCK - 1) // EXPERT_BLOCK * EXPERT_BLOCK
    starts = jnp.cumsum(counts) - counts
    pad_ends = jnp.cumsum(padded)
    pad_starts = pad_ends - padded
    dest = pad_starts[exp_sorted] + jnp.arange(a) - starts[exp_sorted]
    n_rows = a + N_EXPERTS * EXPERT_BLOCK
    n_blocks = n_rows // EXPERT_BLOCK
    row_tok = jnp.zeros((n_rows,), jnp.int32).at[dest].set(tok_sorted)
    row_w = jnp.zeros((n_rows,), w_sorted.dtype).at[dest].set(w_sorted)
    block_expert = jnp.minimum(
        jnp.searchsorted(pad_ends, jnp.arange(n_blocks) * EXPERT_BLOCK, side='right'),
        N_EXPERTS - 1)
    xs = xf[row_tok].reshape(n_blocks, EXPERT_BLOCK, d)

    def expert_block(args):
        xb, e = args
        hb = jax.nn.silu(xb @ w_gate[e]) * (xb @ w_up[e])
        return hb @ w_down[e]

    ys = lax.map(expert_block, (xs, block_expert)).reshape(n_rows, d)
    out = jax.ops.segment_sum(ys * row_w[:, None].astype(ys.dtype), row_tok, num_segments=n)
    return out.reshape(b, s, d)


def setup_inputs(seed: int = 0) -> dict:
    key = jax.random.key(seed)
    ks = jax.random.split(key, 24)
    f32 = jnp.float32
    d = D_MODEL

    def nrm(k, shape, scale):
        return jax.random.normal(k, shape, f32) * scale

    return {
        "x": nrm(ks[0], (BATCH, SEQ, d), 1.0),
        "c": nrm(ks[1], (BATCH, d), 1.0),
        "w_ada": nrm(ks[2], (DEPTH, d, 6 * d), d ** -0.5),
        "b_ada": nrm(ks[3], (DEPTH, 6 * d), 0.01),
        "w_in": nrm(ks[4], (DEPTH, d, IN_COLS), d ** -0.5),
        "sgu_w": nrm(ks[5], (DEPTH, SGU_GROUPS, SGU_BLOCK, SGU_BLOCK), 0.5 * SGU_BLOCK ** -0.5),
        "sgu_b": 1.0 + nrm(ks[6], (DEPTH, SGU_GROUPS, SGU_BLOCK), 0.02),
        "sgu_ln_g": 1.0 + nrm(ks[7], (DEPTH, SGU_WIDTH), 0.02),
        "sgu_ln_b": nrm(ks[8], (DEPTH, SGU_WIDTH), 0.02),
        "w_proj_a": nrm(ks[9], (DEPTH, SB_WIDTH, d), SB_WIDTH ** -0.5),
        "w_proj_b": nrm(ks[10], (DEPTH, SGU_WIDTH, d), SGU_WIDTH ** -0.5),
        "w_out": nrm(ks[11], (DEPTH, d, d), DEEPNORM_BETA * d ** -0.5),
        "ln1_g": 1.0 + nrm(ks[12], (DEPTH, d), 0.02),
        "ln1_b": nrm(ks[13], (DEPTH, d), 0.02),
        "w_group": nrm(ks[14], (DEPTH, d, N_GROUPS), d ** -0.5),
        "b_group": nrm(ks[15], (DEPTH, N_GROUPS), 0.01),
        "w_router": nrm(ks[16], (DEPTH, d, N_EXPERTS), d ** -0.5),
        "b_router": nrm(ks[17], (DEPTH, N_EXPERTS), 0.01),
        "w_gate": nrm(ks[18], (DEPTH, N_EXPERTS, d, D_EXPERT), d ** -0.5),
        "w_up": nrm(ks[19], (DEPTH, N_EXPERTS, d, D_EXPERT), d ** -0.5),
        "w_down": nrm(ks[20], (DEPTH, N_EXPERTS, D_EXPERT, d), DEEPNORM_BETA * D_EXPERT ** -0.5),
        "ln2_g": 1.0 + nrm(ks[21], (DEPTH, d), 0.02),
        "ln2_b": nrm(ks[22], (DEPTH, d), 0.02),
    }


def reference(x, c, w_ada, b_ada, w_in, sgu_w, sgu_b, sgu_ln_g, sgu_ln_b, w_proj_a, w_proj_b,
              w_out, ln1_g, ln1_b, w_group, b_group, w_router, b_router, w_gate, w_up, w_down,
              ln2_g, ln2_b):
    b, s, d = x.shape
    split_at = [SB_WIDTH, 2 * SB_WIDTH, 3 * SB_WIDTH, 3 * SB_WIDTH + SGU_WIDTH,
                3 * SB_WIDTH + 2 * SGU_WIDTH, 3 * SB_WIDTH + 2 * SGU_WIDTH + d]
    c_act = jax.nn.silu(c)
    for l in range(DEPTH):
        mod = c_act @ w_ada[l] + b_ada[l]
        shift1, scale1, gate1, shift2, scale2, gate2 = jnp.split(mod, 6, axis=-1)

        h = modulate(x, shift1, scale1)
        proj = h @ w_in[l]
        q, k, v, su, sv, ga, gb = jnp.split(proj, split_at, axis=-1)
        to_heads = lambda t: t.reshape(b, s, SB_HEADS, SB_HEAD_DIM).transpose(0, 2, 1, 3)
        out_a = stick_breaking_attention(to_heads(q), to_heads(k), to_heads(v))
        out_b = spatial_gating(jax.nn.gelu(su, approximate=False), jax.nn.gelu(sv, approximate=False),
                               sgu_w[l], sgu_b[l], sgu_ln_g[l], sgu_ln_b[l])
        merged = jax.nn.sigmoid(ga) * (out_a @ w_proj_a[l]) + jax.nn.sigmoid(gb) * (out_b @ w_proj_b[l])
        y = merged @ w_out[l]
        x = layer_norm(DEEPNORM_ALPHA * x + gate1[:, None, :] * y, ln1_g[l], ln1_b[l])

        h = modulate(x, shift2, scale2)
        y = hierarchical_moe(h, w_group[l], b_group[l], w_router[l], b_router[l],
                             w_gate[l], w_up[l], w_down[l])
        x = layer_norm(DEEPNORM_ALPHA * x + gate2[:, None, :] * y, ln2_g[l], ln2_b[l])
    return x
```

```python
import contextlib
import os
import numpy as np
import ml_dtypes
import concourse.bass as bass
import concourse.mybir as mybir
from concourse.bass_utils import run_bass_kernel_spmd

F32 = mybir.dt.float32
BF16 = mybir.dt.bfloat16
U32 = mybir.dt.uint32
AF = mybir.ActivationFunctionType
ALU = mybir.AluOpType

D = 2048
S = 8192
NKC = 16
OWN = 2048
LN_EPS = 1e-5
ALPHA = 2.0 ** 0.25
NEXP = 32
DEXP = 512


class Prog:
    LIMIT = 30000
    NDMA = 6

    def __init__(self, nc, stack):
        self.nc = nc
        self.stack = stack
        self.names = ['pe', 'act', 'dve', 'pool', 'sp']
        self.sems = []
        self.ops = {e: [] for e in self.names}
        self.cur = {}
        self.cnt = {}
        for e in self.names:
            self.cur[e] = self._new_sem("c_" + e)
            self.cnt[e] = 0
        self.waited = {e: {} for e in self.names}
        self.lastw = {}
        self.readers = {}
        self.latest = {}
        self.dq = {}
        self.dn = {}
        for q in ['sp', 'act', 'pool']:
            self.dq[q] = [self._new_sem("d_%s%d" % (q, i)) for i in range(self.NDMA)]
            self.dn[q] = 0
        self.n_instr = 0

    def _new_sem(self, name):
        h = self.stack.enter_context(self.nc.semaphore(name + "_%d" % len(self.sems)))
        self.sems.append(h)
        return len(self.sems) - 1

    def _deps(self, e, reads, writes, excl=()):
        deps = []
        for k in excl:
            t = self.lastw.get(k)
            if t is not None and t[0] != self.cur.get(e, -1):
                deps.append(t)
        for k in reads:
            t = self.lastw.get(k)
            if t is not None:
                deps.append(t)
        for k in writes:
            t = self.lastw.get(k)
            if t is not None:
                deps.append(t)
            deps.extend(self.readers.get(k, ()))
        waits = {}
        for (s, v) in deps:
            if e == 'pe' and s == self.cur['pe']:
                continue
            if self.waited[e].get(s, 0) >= v:
                continue
            if waits.get(s, 0) < v:
                waits[s] = v
        for s, v in waits.items():
            self.waited[e][s] = v
        return list(waits.items())

    def _commit(self, tok, reads, writes):
        for k in writes:
            self.lastw[k] = tok
            self.readers[k] = []
        for k in reads:
            if k in writes:
                continue
            self.readers.setdefault(k, []).append(tok)
        self.latest[tok[0]] = tok[1]

    def op(self, e, fn, reads=(), writes=(), excl=()):
        waits = self._deps(e, reads, writes, excl)
        if self.cnt[e] >= self.LIMIT:
            self.cur[e] = self._new_sem("c_" + e)
            self.cnt[e] = 0
        self.cnt[e] += 1
        tok = (self.cur[e], self.cnt[e])
        sems = self.sems

        def emit(eng, waits=waits, fn=fn, tok=tok):
            for (s, v) in waits:
                eng.wait_ge(sems[s], v)
            fn(eng).then_inc(sems[tok[0]], 1)
        self.ops[e].append(emit)
        self._commit(tok, reads, writes)
        for k in excl:
            self.lastw[k] = tok
        self.n_instr += 1
        return tok

    def dma(self, q, out, in_, reads=(), writes=(), **kw):
        waits = self._deps(q, reads, writes)
        i = self.dn[q]
        self.dn[q] += 1
        k = i % self.NDMA
        val = 16 * (i // self.NDMA + 1)
        s = self.dq[q][k]
        if i >= self.NDMA and self.waited[q].get(s, 0) < val - 16:
            waits.append((s, val - 16))
            self.waited[q][s] = val - 16
        tok = (s, val)
        sems = self.sems

        def emit(eng, waits=waits, tok=tok, out=out, in_=in_, kw=kw):
            for (ss, v) in waits:
                eng.wait_ge(sems[ss], v)
            eng.dma_start(out=out, in_=in_, **kw).then_inc(sems[tok[0]], 16)
        self.ops[q].append(emit)
        self._commit(tok, reads, writes)
        self.n_instr += 1
        return tok

    def barrier(self):
        for e in self.names:
            waits = []
            for s, v in self.latest.items():
                if self.waited[e].get(s, 0) < v:
                    waits.append((s, v))
                    self.waited[e][s] = v
            sems = self.sems

            def emit(eng, waits=waits):
                for (s, v) in waits:
                    eng.wait_ge(sems[s], v)
            self.ops[e].append(emit)
        self.lastw.clear()
        self.readers.clear()

    def flush(self):
        nc = self.nc
        ops = self.ops
        with nc.Block() as block:
            @block.tensor
            def _(eng):
                for f in ops['pe']:
                    f(eng)

            @block.scalar
            def _(eng):
                for f in ops['act']:
                    f(eng)

            @block.vector
            def _(eng):
                for f in ops['dve']:
                    f(eng)

            @block.gpsimd
            def _(eng):
                for f in ops['pool']:
                    f(eng)

            @block.sync
            def _(eng):
                for f in ops['sp']:
                    f(eng)
        self.ops = {e: [] for e in self.names}


def build(upto=99, debug=False, lite=False):
    nc = bass.Bass("TRN2", target_bir_lowering=False)
    dk = "ExternalOutput"

    def din(name, shape, dt=F32):
        return nc.dram_tensor(name, list(shape), dt, kind="ExternalInput").ap()

    xb = din("xb", [S, D])
    xo = din("xo", [OWN, D])
    maskd = din("maskd", [128, 4, 128])
    cT = din("cT", [128, NKC])
    w_ada = din("w_ada", [D, 6 * D])
    b_ada = din("b_ada", [1, 6 * D])
    w_in = din("w_in", [D, 9216])
    sgu_wT = din("sgu_wT", [128, 8, 128])
    sgu_b = din("sgu_b", [1, 8 * 128])
    sgu_ln_g = din("sgu_ln_g", [1, 1024])
    sgu_ln_b = din("sgu_ln_b", [1, 1024])
    w_pa = din("w_proj_a", [1024, D])
    w_pb = din("w_proj_b", [1024, D])
    w_out = din("w_out", [D, D])
    ln1_g = din("ln1_g", [1, D])
    ln1_b = din("ln1_b", [1, D])
    w_rt = din("w_rt", [D, 36])
    b_rt = din("b_rt", [1, 36])
    if not lite:
        w_gate = din("w_gate", [NEXP, D, DEXP])
        w_up = din("w_up", [NEXP, D, DEXP])
        w_down = din("w_down", [NEXP, DEXP, D])
    ln2_g = din("ln2_g", [1, D])
    ln2_b = din("ln2_b", [1, D])
    out = nc.dram_tensor("out", [OWN, D], F32, kind="ExternalOutput").ap()

    def dscr(name, shape, dt):
        return nc.dram_tensor(name, list(shape), dt, kind=dk).ap()

    modv = dscr("modv", [1, 6 * D], F32)
    KT = dscr("KT", [8, 128, S], BF16)
    VV = dscr("VV", [64, 128, 1024], BF16)
    QTd = dscr("QTd", [8, 128, OWN], BF16)
    obTd = dscr("obTd", [8, 128, OWN], BF16)
    gaTd = dscr("gaTd", [NKC, 128, OWN], BF16)
    gbTd = dscr("gbTd", [NKC, 128, OWN], BF16)
    oaTd = dscr("oaTd", [8, 128, OWN], BF16)
    x1d = dscr("x1d", [OWN, D], F32)
    h2Td = dscr("h2Td", [NKC, 128, OWN], BF16)
    wdd = dscr("wdd", [OWN, NEXP], F32)

    with contextlib.ExitStack() as top:
        P = Prog(nc, top)
        banks = [top.enter_context(nc.psum_tensor("bank%d" % i, [128, 512], F32)) for i in range(8)]
        ident = top.enter_context(nc.sbuf_tensor("ident", [128, 128], F32))
        identb = top.enter_context(nc.sbuf_tensor("identb", [128, 128], BF16))
        ones_r = top.enter_context(nc.sbuf_tensor("ones_r", [1, 128], F32))

        uniq = [0]

        def sb(ph, name, shape, dt):
            uniq[0] += 1
            return ph.enter_context(nc.sbuf_tensor("%s_%d" % (name, uniq[0]), list(shape), dt))

        P.op('pool', lambda g: g.memset(ident[:], 1.0), writes=['ident'])
        P.op('pool', lambda g: g.affine_select(out=ident[:], in_=ident[:], pattern=[[-1, 128]],
                                               compare_op=ALU.is_equal, fill=0.0, base=0,
                                               channel_multiplier=1),
             reads=['ident'], writes=['ident'])
        P.op('pool', lambda g: g.tensor_copy(out=identb[:], in_=ident[:]), reads=['ident'], writes=['identb'])
        P.op('pool', lambda g: g.memset(ones_r[:], 1.0), writes=['ones_r'])

        if upto >= 0:
            with contextlib.ExitStack() as ph:
                cact = sb(ph, "cact", [128, NKC], F32)
                craw = sb(ph, "craw", [128, NKC], F32)
                bada = sb(ph, "bada", [1, 6 * D], F32)
                wst = [sb(ph, "wst%d" % i, [128, 2048], F32) for i in range(3)]
                mrow = sb(ph, "mrow", [1, 2048], F32)
                P.dma('sp', craw[:], cT, writes=['craw'])
                P.dma('sp', bada[:], b_ada, writes=['bada'])
                P.op('act', lambda a: a.activation(out=cact[:], in_=craw[:], func=AF.Silu),
                     reads=['craw'], writes=['cact'])
                n = 0
                for g in range(6):
                    for kc in range(NKC):
                        w = wst[n % 3]
                        wk = 'wst%d' % (n % 3)
                        n += 1
                        P.dma('sp', w[:], w_ada[kc * 128:(kc + 1) * 128, g * 2048:(g + 1) * 2048], writes=[wk])
                        for jj in range(4):
                            P.op('pe', lambda pe, w=w, jj=jj, kc=kc: pe.matmul(
                                banks[jj][0:1, :], lhsT=cact[:, kc:kc + 1], rhs=w[:, jj * 512:(jj + 1) * 512],
                                start=(kc == 0), stop=(kc == NKC - 1)),
                                reads=[wk, 'cact'], writes=['b%d' % jj])
                    for jj in range(4):
                        P.op('dve', lambda v, jj=jj, g=g: v.tensor_tensor(
                            out=mrow[0:1, jj * 512:(jj + 1) * 512], in0=banks[jj][0:1, :],
                            in1=bada[0:1, g * 2048 + jj * 512: g * 2048 + (jj + 1) * 512], op=ALU.add),
                            reads=['b%d' % jj, 'bada'], writes=['mrow%d' % jj])
                    P.dma('sp', modv[0:1, g * 2048:(g + 1) * 2048], mrow[:],
                          reads=['mrow%d' % jj for jj in range(4)], writes=['modv'])
                P.barrier()
                P.flush()

        mfm = top.enter_context(nc.sbuf_tensor("mfm", [128, 6, NKC], F32))
        mview = modv.rearrange("o (s c p) -> p (o s) c", p=128, c=NKC)
        for s6 in range(6):
            P.dma('sp', mfm[:, s6, :], mview[:, s6, :], reads=['modv'], writes=['mfm'],
                  allow_slow_non_contiguous=True)
        P.op('dve', lambda v: v.tensor_scalar(out=mfm[:, 1, :], in0=mfm[:, 1, :], scalar1=1.0, scalar2=None,
                                              op0=ALU.add), reads=['mfm'], writes=['mfm'])
        P.op('dve', lambda v: v.tensor_scalar(out=mfm[:, 4, :], in0=mfm[:, 4, :], scalar1=1.0, scalar2=None,
                                              op0=ALU.add), reads=['mfm'], writes=['mfm'])

        def layer_norm_tile(xt, xk, xn, xnk, st, mv, sd, eps_t):
            for q in range(4):
                P.op('dve', lambda v, q=q: v.bn_stats(out=st[:, q, :], in_=xt[:, q * 512:(q + 1) * 512]),
                     reads=[xk], writes=['st%d' % q])
            P.op('dve', lambda v: v.bn_aggr(out=mv[:], in_=st[:].rearrange("p a b -> p (a b)")),
                 reads=['st%d' % q for q in range(4)], writes=['mv'])
            P.op('act', lambda a: a.activation(out=sd[:, 0:1], in_=mv[:, 1:2], func=AF.Sqrt, bias=eps_t[:, 0:1],
                                               scale=1.0),
                 reads=['mv', 'eps'], writes=['sd0'])
            P.op('dve', lambda v: v.reciprocal(out=sd[:, 1:2], in_=sd[:, 0:1]), reads=['sd0'], writes=['sd1'])
            P.op('dve', lambda v: v.tensor_scalar(out=sd[:, 2:3], in0=mv[:, 0:1], scalar1=sd[:, 1:2], scalar2=-1.0,
                                                  op0=ALU.mult, op1=ALU.mult),
                 reads=['mv', 'sd1'], writes=['sd2'])
            P.op('act', lambda a: a.activation(out=xn[:], in_=xt[:], func=AF.Identity, bias=sd[:, 2:3],
                                               scale=sd[:, 1:2]),
                 reads=[xk, 'sd1', 'sd2'], writes=[xnk])

        eps_t = top.enter_context(nc.sbuf_tensor("eps_t", [128, 1], F32))
        P.op('pool', lambda g: g.memset(eps_t[:], LN_EPS), writes=['eps'])

        def B(bk):
            return ['B%d' % bk]

        def ln_T_tile(ph_bufs, src_rows, dst, dkey, col0, ms, fdst=None, bo=0, xt_in=None, xk_in=None):
            xin, xn, st, mv, sd, cnt = ph_bufs
            xt = xin[cnt[0] % 2]
            xk = 'xin%d' % (cnt[0] % 2)
            cnt[0] += 1
            if src_rows is not None:
                P.dma('act', xt[:], src_rows, writes=[xk])
            else:
                xt, xk = xt_in, xk_in
            layer_norm_tile(xt, xk, xn, 'xn', st, mv, sd, eps_t)
            for kc in range(NKC):
                bk = bo + kc // 4
                P.op('pe', lambda pe, kc=kc, bk=bk: pe.transpose(
                    banks[bk][:, (kc % 4) * 128:(kc % 4 + 1) * 128], xn[:, kc * 128:(kc + 1) * 128], ident[:]),
                    reads=['xn', 'ident'], writes=[('bq', bk, kc % 4)], excl=B(bk))
            for kc in range(NKC):
                bk = bo + kc // 4
                q = kc % 4
                if bk - bo < 2:
                    P.op('dve', lambda v, kc=kc, bk=bk, q=q: v.tensor_scalar(
                        out=dst[:, kc, col0:col0 + 128], in0=banks[bk][:, q * 128:(q + 1) * 128],
                        scalar1=mfm[:, ms + 1, kc:kc + 1], scalar2=mfm[:, ms, kc:kc + 1],
                        op0=ALU.mult, op1=ALU.add),
                        reads=[('bq', bk, q), 'mfm'], writes=[(dkey, kc, col0)], excl=B(bk))
                else:
                    P.op('act', lambda a, kc=kc, bk=bk, q=q: a.activation(
                        out=dst[:, kc, col0:col0 + 128], in_=banks[bk][:, q * 128:(q + 1) * 128],
                        func=AF.Identity, scale=mfm[:, ms + 1, kc:kc + 1], bias=mfm[:, ms, kc:kc + 1]),
                        reads=[('bq', bk, q), 'mfm'], writes=[(dkey, kc, col0)], excl=B(bk))
                if fdst is not None:
                    P.op('dve', lambda v, kc=kc, bk=bk, q=q: v.tensor_scalar(
                        out=fdst[:, kc, :], in0=banks[bk][:, q * 128:(q + 1) * 128],
                        scalar1=mfm[:, ms + 1, kc:kc + 1], scalar2=mfm[:, ms, kc:kc + 1],
                        op0=ALU.mult, op1=ALU.add),
                        reads=[('bq', bk, q), 'mfm'], writes=[('fdst', kc)], excl=B(bk))

        def ln_bufs(ph):
            xin = [sb(ph, "xin%d" % i, [128, 2048], F32) for i in range(2)]
            xn = sb(ph, "xn", [128, 2048], F32)
            st = sb(ph, "st", [128, 4, 6], F32)
            mv = sb(ph, "mv", [128, 2], F32)
            sd = sb(ph, "sd", [128, 4], F32)
            return (xin, xn, st, mv, sd, [0])

        evc = [0]

        def evac_copy(dst_ap, bk, wkeys, src=None):
            src = banks[bk][:, :] if src is None else src
            evc[0] += 1
            if evc[0] % 2 == 0:
                P.op('dve', lambda v: v.tensor_copy(out=dst_ap, in_=src), reads=['b%d' % bk], writes=wkeys, excl=B(bk))
            else:
                P.op('act', lambda a: a.copy(out=dst_ap, in_=src), reads=['b%d' % bk], writes=wkeys, excl=B(bk))

        if upto >= 1:
            with contextlib.ExitStack() as ph:
                Wkv = sb(ph, "Wkv", [128, NKC, 2048], BF16)
                wst = [sb(ph, "wstb%d" % i, [128, 2048], F32) for i in range(2)]
                lb = ln_bufs(ph)
                hT = [sb(ph, "hT%d" % i, [128, NKC, 512], BF16) for i in range(2)]
                kst = [sb(ph, "kst%d" % i, [128, 8, 512], BF16) for i in range(2)]
                vst = [sb(ph, "vst%d" % i, [128, 1024], BF16) for i in range(2)]
                for kc in range(NKC):
                    w = wst[kc % 2]
                    wk = 'wstb%d' % (kc % 2)
                    P.dma('sp', w[:], w_in[kc * 128:(kc + 1) * 128, 1024:3072], writes=[wk])
                    P.op('pool', lambda g, w=w, kc=kc: g.tensor_copy(out=Wkv[:, kc, :], in_=w[:]),
                         reads=[wk], writes=[('Wkv', kc)])
                nk = 0
                nv = 0
                for gc in range(16):
                    hb = hT[gc % 2]
                    hk = 'hT%d' % (gc % 2)
                    for tt in range(4):
                        r0 = gc * 512 + tt * 128
                        ln_T_tile(lb, xb[r0:r0 + 128, :], hb, hk, tt * 128, 0)
                    ks = kst[gc % 2]
                    kk = 'kst%d' % (gc % 2)
                    for h in range(8):
                        bk = 4 + (nk % 4)
                        nk += 1
                        for kc in range(NKC):
                            P.op('pe', lambda pe, kc=kc, h=h, bk=bk, hb=hb: pe.matmul(
                                banks[bk][:, :], lhsT=Wkv[:, kc, h * 128:(h + 1) * 128], rhs=hb[:, kc, :],
                                start=(kc == 0), stop=(kc == NKC - 1)),
                                reads=[('Wkv', kc)] + [(hk, kc, t4 * 128) for t4 in range(4)],
                                writes=['b%d' % bk], excl=B(bk))
                        evac_copy(ks[:, h, :], bk, [(kk, h)])
                    P.dma('sp', KT[:, :, gc * 512:(gc + 1) * 512].rearrange("h p t -> p h t"), ks[:],
                          reads=[(kk, h) for h in range(8)], writes=[('KT', gc)])
                    for tt in range(4):
                        vs = vst[nv % 2]
                        vk = 'vst%d' % (nv % 2)
                        nv += 1
                        for half in range(2):
                            bk = 4 + (nk % 4)
                            nk += 1
                            for kc in range(NKC):
                                P.op('pe', lambda pe, kc=kc, half=half, bk=bk, hb=hb, tt=tt: pe.matmul(
                                    banks[bk][:, :], lhsT=hb[:, kc, tt * 128:(tt + 1) * 128],
                                    rhs=Wkv[:, kc, 1024 + half * 512:1024 + (half + 1) * 512],
                                    start=(kc == 0), stop=(kc == NKC - 1)),
                                    reads=[('Wkv', kc), (hk, kc, tt * 128)], writes=['b%d' % bk], excl=B(bk))
                            evac_copy(vs[:, half * 512:(half + 1) * 512], bk, [(vk, half)])
                        P.dma('sp', VV[gc * 4 + tt], vs[:], reads=[(vk, 0), (vk, 1)], writes=[('VV', gc * 4 + tt)])
                P.barrier()
                P.flush()

        if upto >= 2:
            with contextlib.ExitStack() as ph:
                hO = sb(ph, "hO", [128, NKC, OWN], BF16)
                lb = ln_bufs(ph)
                wsh = [sb(ph, "wsh%d" % i, [128, 8, 512], F32) for i in range(2)]
                wbf = [sb(ph, "wbf%d" % i, [128, NKC, 512], BF16) for i in range(2)]
                ost = [sb(ph, "ost%d" % i, [128, 4, 512], BF16) for i in range(2)]
                for t in range(16):
                    ln_T_tile(lb, xo[t * 128:(t + 1) * 128, :], hO, 'hO', t * 128, 0)
                hO_keys = lambda kc, c0, n: [('hO', kc, c0 + 128 * q) for q in range(n)]
                nld = [0]

                def load_wblock(src2d, c0, wb, wbk, nkc=NKC):
                    for half in range(nkc // 8):
                        w = wsh[nld[0] % 2]
                        wk = 'wsh%d' % (nld[0] % 2)
                        nld[0] += 1
                        P.dma('sp', w[:], src2d[half * 1024:(half + 1) * 1024, c0:c0 + 512].rearrange(
                            "(c p) n -> p c n", p=128), writes=[wk])
                        P.op('pool', lambda g, w=w, half=half, wb=wb: g.tensor_copy(
                            out=wb[:, half * 8:(half + 1) * 8, :], in_=w[:]),
                            reads=[wk], writes=[(wbk, half)])

                blocks = []
                for i in range(2):
                    blocks.append((i * 512, 'copy', QTd, i * 4))
                for i in range(2):
                    blocks.append((3072 + i * 512, 'gelu', obTd, i * 4))
                for i in range(4):
                    blocks.append((5120 + i * 512, 'sig', gaTd, i * 4))
                for i in range(4):
                    blocks.append((7168 + i * 512, 'sig', gbTd, i * 4))
                nb = 0
                no = 0
                nbank = 0
                for (c0, fn, dstd, ch0) in blocks:
                    wb = wbf[nb % 2]
                    wbk = 'wbf%d' % (nb % 2)
                    nb += 1
                    load_wblock(w_in, c0, wb, wbk)
                    for oc in range(4):
                        os_ = ost[no % 2]
                        ok = 'ost%d' % (no % 2)
                        no += 1
                        for sub in range(4):
                            bk = 4 + (nbank % 4)
                            nbank += 1
                            for kc in range(NKC):
                                P.op('pe', lambda pe, kc=kc, sub=sub, bk=bk, wb=wb, oc=oc: pe.matmul(
                                    banks[bk][:, :], lhsT=wb[:, kc, sub * 128:(sub + 1) * 128],
                                    rhs=hO[:, kc, oc * 512:(oc + 1) * 512],
                                    start=(kc == 0), stop=(kc == NKC - 1)),
                                    reads=[(wbk, kc // 8)] + hO_keys(kc, oc * 512, 4), writes=['b%d' % bk], excl=B(bk))
                            if fn == 'copy':
                                evac_copy(os_[:, sub, :], bk, [(ok, sub)])
                            else:
                                f = AF.Gelu if fn == 'gelu' else AF.Sigmoid
                                P.op('act', lambda a, sub=sub, bk=bk, os_=os_, f=f: a.activation(
                                    out=os_[:, sub, :], in_=banks[bk][:, :], func=f),
                                    reads=['b%d' % bk], writes=[(ok, sub)], excl=B(bk))
                        P.dma('sp', dstd[ch0:ch0 + 4, :, oc * 512:(oc + 1) * 512].rearrange("c p t -> p c t"), os_[:],
                              reads=[(ok, q) for q in range(4)], writes=[(dstd.tensor.name, ch0, oc)])
                load_wblock(w_in, 4096, wbf[0], 'wbf0')
                load_wblock(w_in, 4608, wbf[1], 'wbf1')
                wsf = sb(ph, "wsf", [128, 8, 128], F32)
                wsb = sb(ph, "wsb", [128, 8, 128], BF16)
                bsb = sb(ph, "bsb", [128, 1024], F32)
                lnG = sb(ph, "lnG", [128, 1024], F32)
                lnB = sb(ph, "lnB", [128, 1024], F32)
                gv = sb(ph, "gv", [128, 1024], F32)
                vn0 = sb(ph, "vn0", [128, 1024], F32)
                vnb = sb(ph, "vnb", [128, 1024], BF16)
                uT = [sb(ph, "uT%d" % i, [128, 8, 128], BF16) for i in range(2)]
                tmpm = sb(ph, "tmpm", [128, 1024], F32)
                obs = [sb(ph, "obs%d" % i, [128, 8, 128], BF16) for i in range(2)]
                st2 = sb(ph, "st2", [128, 2, 6], F32)
                mv2 = sb(ph, "mv2", [128, 2], F32)
                sd2 = sb(ph, "sd2", [128, 4], F32)
                P.dma('sp', wsf[:], sgu_wT, writes=['wsf'])
                P.op('pool', lambda g: g.memset(wsf[64:128, :, 0:64], 0.0), reads=['wsf'], writes=['wsf'])
                P.op('pool', lambda g: g.tensor_copy(out=wsb[:], in_=wsf[:]), reads=['wsf'], writes=['wsb'])
                P.dma('sp', bsb[:], sgu_b.partition_broadcast(128).rearrange("p o f -> p (o f)"), writes=['bsb'])
                P.dma('sp', lnG[:], sgu_ln_g.partition_broadcast(128).rearrange("p o f -> p (o f)"), writes=['lnG'])
                P.dma('sp', lnB[:], sgu_ln_b.partition_broadcast(128).rearrange("p o f -> p (o f)"), writes=['lnB'])
                for t in range(16):
                    u = uT[t % 2]
                    uk = 'uT%d' % (t % 2)
                    ob = obs[t % 2]
                    obk = 'obs%d' % (t % 2)
                    P.dma('act', u[:], obTd[:, :, t * 128:(t + 1) * 128].rearrange("c p t -> p c t"),
                          reads=[('obTd', 0, t // 4), ('obTd', 4, t // 4)], writes=[uk])
                    for half in range(2):
                        bk = half
                        for kc in range(NKC):
                            P.op('pe', lambda pe, kc=kc, half=half, bk=bk, t=t: pe.matmul(
                                banks[bk][:, :], lhsT=hO[:, kc, t * 128:(t + 1) * 128], rhs=wbf[half][:, kc, :],
                                start=(kc == 0), stop=(kc == NKC - 1)),
                                reads=[('wbf%d' % half, kc // 8), ('hO', kc, t * 128)], writes=['b%d' % bk], excl=B(bk))
                        P.op('act', lambda a, half=half, bk=bk: a.activation(
                            out=gv[:, half * 512:(half + 1) * 512], in_=banks[bk][:, :], func=AF.Gelu),
                            reads=['b%d' % bk], writes=[('gv', half)], excl=B(bk))
                        P.op('dve', lambda v, half=half: v.bn_stats(out=st2[:, half, :], in_=gv[:, half * 512:(half + 1) * 512]),
                             reads=[('gv', half)], writes=[('st2', half)])
                    P.op('dve', lambda v: v.bn_aggr(out=mv2[:], in_=st2[:].rearrange("p a b -> p (a b)")),
                         reads=[('st2', 0), ('st2', 1)], writes=['mv2'])
                    P.op('act', lambda a: a.activation(out=sd2[:, 0:1], in_=mv2[:, 1:2], func=AF.Sqrt, bias=eps_t[:, 0:1], scale=1.0),
                         reads=['mv2', 'eps'], writes=['sd20'])
                    P.op('dve', lambda v: v.reciprocal(out=sd2[:, 1:2], in_=sd2[:, 0:1]), reads=['sd20'], writes=['sd21'])
                    P.op('dve', lambda v: v.tensor_scalar(out=sd2[:, 2:3], in0=mv2[:, 0:1], scalar1=sd2[:, 1:2], scalar2=-1.0,
                                                          op0=ALU.mult, op1=ALU.mult), reads=['mv2', 'sd21'], writes=['sd22'])
                    P.op('act', lambda a: a.activation(out=vn0[:], in_=gv[:], func=AF.Identity, bias=sd2[:, 2:3], scale=sd2[:, 1:2]),
                         reads=[('gv', 0), ('gv', 1), 'sd21', 'sd22'], writes=['vn0'])
                    P.op('dve', lambda v: v.tensor_tensor(out=vn0[:], in0=vn0[:], in1=lnG[:], op=ALU.mult),
                         reads=['vn0', 'lnG'], writes=['vn0'])
                    P.op('dve', lambda v: v.tensor_tensor(out=vnb[:], in0=vn0[:], in1=lnB[:], op=ALU.add),
                         reads=['vn0', 'lnB'], writes=['vnb'])
                    for g in range(8):
                        bk = 2 + g // 4
                        P.op('pe', lambda pe, g=g, bk=bk: pe.matmul(
                            banks[bk][:, (g % 4) * 128:(g % 4 + 1) * 128], lhsT=vnb[:, g * 128:(g + 1) * 128],
                            rhs=wsb[:, g, :], start=True, stop=True),
                            reads=['vnb', 'wsb'], writes=[('bq', bk, g % 4)], excl=B(bk))
                    for hh in range(2):
                        bk = 2 + hh
                        P.op('dve', lambda v, hh=hh, bk=bk: v.tensor_tensor(
                            out=tmpm[:, hh * 512:(hh + 1) * 512], in0=banks[bk][:, :], in1=bsb[:, hh * 512:(hh + 1) * 512],
                            op=ALU.add), reads=[('bq', bk, q) for q in range(4)] + ['bsb'], writes=[('tmpm', hh)], excl=B(bk))
                    P.op('pool', lambda gp, u=u, ob=ob: gp.tensor_tensor(
                        out=ob[:].rearrange("p c t -> p (c t)"), in0=tmpm[:], in1=u[:].rearrange("p c t -> p (c t)"), op=ALU.mult),
                        reads=[('tmpm', 0), ('tmpm', 1), uk], writes=[obk])
                    P.dma('sp', obTd[:, :, t * 128:(t + 1) * 128].rearrange("c p t -> p c t"), ob[:],
                          reads=[obk], writes=[('obTd2', t)])
                P.barrier()
                P.flush()

        if upto >= 3:
            with contextlib.ExitStack() as ph:
                KTh = sb(ph, "KTh", [128, 4, S], BF16)
                Vh = sb(ph, "Vh", [128, 64, 512], BF16)
                QTh = sb(ph, "QTh", [128, 4, OWN], BF16)
                mkf = sb(ph, "mkf", [128, 4, 128], F32)
                mk4 = sb(ph, "mk4", [128, 4, 4, 128], BF16)
                triI = sb(ph, "triI", [128, 128], BF16)
                triC = sb(ph, "triC", [128, 128], BF16)
                trif = sb(ph, "trif", [128, 128], F32)
                eb = [sb(ph, "eb%d" % i, [128, 512], F32) for i in range(2)]
                spb = [sb(ph, "spb%d" % i, [128, 512], BF16) for i in range(2)]
                gb_ = [sb(ph, "gb%d" % i, [128, 512], F32) for i in range(2)]
                wb_ = [sb(ph, "wb%d" % i, [128, 512], BF16) for i in range(2)]
                w32 = sb(ph, "w32", [128, 512], F32)
                oas = [sb(ph, "oas%d" % i, [128, 4, 128], BF16) for i in range(2)]
                P.dma('sp', mkf[:], maskd, writes=['mkf'])
                for h in range(4):
                    P.op('pool', lambda g, h=h: g.tensor_copy(out=mk4[:, :, h, :], in_=mkf[:]), reads=['mkf'], writes=[('mk4', h)])
                mk4k = [('mk4', h) for h in range(4)]
                P.op('pool', lambda g: g.memset(trif[:], 1.0), writes=['trif'])
                P.op('pool', lambda g: g.affine_select(out=trif[:], in_=trif[:], pattern=[[-1, 128]], compare_op=ALU.is_ge,
                                                       fill=0.0, base=0, channel_multiplier=1), reads=['trif'], writes=['trif'])
                P.op('pool', lambda g: g.tensor_copy(out=triI[:], in_=trif[:]), reads=['trif'], writes=['triI'])
                P.op('pool', lambda g: g.memset(trif[:], 1.0), reads=['trif'], writes=['trif'])
                P.op('pool', lambda g: g.affine_select(out=trif[:], in_=trif[:], pattern=[[1, 128]], compare_op=ALU.is_gt,
                                                       fill=0.0, base=0, channel_multiplier=-1), reads=['trif'], writes=['trif'])
                P.op('pool', lambda g: g.tensor_copy(out=triC[:], in_=trif[:]), reads=['trif'], writes=['triC'])
                sc = 128.0 ** -0.5
                for hg in range(2):
                    for h in range(4):
                        P.dma('sp', KTh[:, h, :], KT[hg * 4 + h], writes=[('KTh', h)])
                    for q4 in range(4):
                        P.dma('act', Vh[:, q4 * 16:(q4 + 1) * 16, :],
                              VV[q4 * 16:(q4 + 1) * 16, :, hg * 512:(hg + 1) * 512].rearrange("b p f -> p b f"),
                              writes=[('Vh', q4)])
                    P.dma('sp', QTh[:], QTd[hg * 4:(hg + 1) * 4].rearrange("c p t -> p c t"), writes=['QTh'])
                    tiles = [(i, kb) for i in range(16) for kb in range(4 * i + 3, -1, -1)]
                    nT = len(tiles)

                    def stageA(n):
                        i, kb = tiles[n]
                        zb = n % 2
                        e = eb[n % 2]
                        sp_ = spb[n % 2]
                        for h in range(4):
                            P.op('pe', lambda pe, h=h, i=i, kb=kb, zb=zb: pe.matmul(
                                banks[zb][:, h * 128:(h + 1) * 128], lhsT=KTh[:, h, kb * 128:(kb + 1) * 128],
                                rhs=QTh[:, h, i * 128:(i + 1) * 128], start=True, stop=True),
                                reads=[('KTh', h), 'QTh'], writes=['b%d' % zb], excl=B(zb))
                        P.op('act', lambda a, zb=zb, e=e: a.activation(out=e[:], in_=banks[zb][:, :], func=AF.Exp, scale=sc),
                             reads=['b%d' % zb], writes=['e%d' % (n % 2)], excl=B(zb))
                        P.op('act', lambda a, e=e, sp_=sp_: a.activation(out=sp_[:], in_=e[:], func=AF.Ln, bias=1.0, scale=1.0),
                             reads=['e%d' % (n % 2)], writes=['sp%d' % (n % 2)])
                        if kb >= 4 * i:
                            m = kb - 4 * i
                            P.op('pool', lambda g, sp_=sp_, m=m: g.tensor_tensor(
                                out=sp_[:], in0=sp_[:], in1=mk4[:, m, :, :].rearrange("p h t -> p (h t)"), op=ALU.mult),
                                reads=['sp%d' % (n % 2)] + mk4k, writes=['sp%d' % (n % 2)])

                    def stageB(n):
                        i, kb = tiles[n]
                        e = eb[n % 2]
                        sp_ = spb[n % 2]
                        g_ = gb_[n % 2]
                        w_ = wb_[n % 2]
                        btb = 2 + (i % 2)
                        otb = 4 + (i % 2)
                        first = (kb == 4 * i + 3)
                        last = (kb == 0)
                        P.op('pe', lambda pe: pe.matmul(banks[btb][:, :], lhsT=triI[:], rhs=sp_[:], start=first, stop=False),
                             reads=['triI', 'sp%d' % (n % 2)], writes=['b%d' % btb], excl=B(btb))
                        P.op('act', lambda a: a.activation(out=g_[:], in_=banks[btb][:, :], func=AF.Exp, scale=-1.0),
                             reads=['b%d' % btb], writes=['g%d' % (n % 2)], excl=B(btb))
                        P.op('pe', lambda pe: pe.matmul(banks[btb][:, :], lhsT=triC[:], rhs=sp_[:], start=False, stop=last),
                             reads=['triC', 'sp%d' % (n % 2)], writes=['b%d' % btb], excl=B(btb))
                        if kb >= 4 * i:
                            m = kb - 4 * i
                            P.op('dve', lambda v: v.tensor_tensor(out=w32[:], in0=e[:], in1=g_[:], op=ALU.mult),
                                 reads=['e%d' % (n % 2), 'g%d' % (n % 2)], writes=['w32'])
                            P.op('dve', lambda v: v.tensor_tensor(
                                out=w_[:], in0=w32[:], in1=mk4[:, m, :, :].rearrange("p h t -> p (h t)"), op=ALU.mult),
                                reads=['w32'] + mk4k, writes=['w%d' % (n % 2)])
                        else:
                            P.op('dve', lambda v: v.tensor_tensor(out=w_[:], in0=e[:], in1=g_[:], op=ALU.mult),
                                 reads=['e%d' % (n % 2), 'g%d' % (n % 2)], writes=['w%d' % (n % 2)])
                        for h in range(4):
                            P.op('pe', lambda pe, h=h: pe.matmul(
                                banks[otb][:, h * 128:(h + 1) * 128], lhsT=Vh[:, kb, h * 128:(h + 1) * 128],
                                rhs=w_[:, h * 128:(h + 1) * 128], start=(first and h == 0), stop=last),
                                reads=[('Vh', kb // 16), 'w%d' % (n % 2)], writes=['b%d' % otb], excl=B(otb))
                        if last:
                            oa = oas[i % 2]
                            evac_copy(oa[:].rearrange("p c t -> p (c t)"), otb, ['oas%d' % (i % 2)])
                            P.dma('sp', oaTd[hg * 4:(hg + 1) * 4, :, i * 128:(i + 1) * 128].rearrange("c p t -> p c t"), oa[:],
                                  reads=['oas%d' % (i % 2)], writes=[('oaTd', hg, i)])

                    stageA(0)
                    for n in range(nT):
                        if n + 1 < nT:
                            stageA(n + 1)
                        stageB(n)
                P.barrier()
                P.flush()
        mTd = dscr("mTd", [NKC, 128, OWN], BF16)
        y2d = dscr("y2d", [OWN, D], F32) if debug else None
        if debug:
            dbg_wdh = dscr("dbg_wdh", [2, 128, 8 * NEXP], F32)
            dbg_hact = dscr("dbg_hact", [2, 2, 128, 4 * 512], BF16)
            dbg_h2h = dscr("dbg_h2h", [2, 128, NKC * 1024], BF16)
            dbg_y0 = dscr("dbg_y0", [2, 128, 8 * D], F32)
            dbg_pad = dscr("dbg_pad", [2, 128, 1024], F32)

        def bcast_load(ph, name, src_row):
            t = sb(ph, name, [128, src_row.shape[-1]], F32)
            P.dma('sp', t[:], src_row.partition_broadcast(128).rearrange("p o f -> p (o f)"), writes=[name])
            return t

        if upto >= 4:
            with contextlib.ExitStack() as ph:
                oaT = sb(ph, "oaT", [128, 8, OWN], BF16)
                obT = sb(ph, "obT", [128, 8, OWN], BF16)
                P.dma('sp', oaT[:], oaTd.rearrange("c p t -> p c t"), writes=['oaT'])
                P.dma('act', obT[:], obTd.rearrange("c p t -> p c t"), writes=['obT'])
                was = [sb(ph, "was%d" % i, [128, 8, 128], F32) for i in range(2)]
                wbs = [sb(ph, "wbs%d" % i, [128, 8, 128], F32) for i in range(2)]
                wab = [sb(ph, "wab%d" % i, [128, 8, 128], BF16) for i in range(2)]
                wbb = [sb(ph, "wbb%d" % i, [128, 8, 128], BF16) for i in range(2)]
                gas = [sb(ph, "gas%d" % i, [128, OWN], BF16) for i in range(2)]
                gbs = [sb(ph, "gbs%d" % i, [128, OWN], BF16) for i in range(2)]
                t1 = [sb(ph, "t1%d" % i, [128, 512], F32) for i in range(2)]
                t2 = [sb(ph, "t2%d" % i, [128, 512], F32) for i in range(2)]
                mst = [sb(ph, "mst%d" % i, [128, OWN], BF16) for i in range(2)]
                nn = 0
                for nb in range(16):
                    p2 = nb % 2
                    P.dma('sp', was[p2][:], w_pa[:, nb * 128:(nb + 1) * 128].rearrange("(c p) n -> p c n", p=128), writes=['was%d' % p2])
                    P.dma('sp', wbs[p2][:], w_pb[:, nb * 128:(nb + 1) * 128].rearrange("(c p) n -> p c n", p=128), writes=['wbs%d' % p2])
                    P.op('pool', lambda g, p2=p2: g.tensor_copy(out=wab[p2][:], in_=was[p2][:]), reads=['was%d' % p2], writes=['wab%d' % p2])
                    P.op('pool', lambda g, p2=p2: g.tensor_copy(out=wbb[p2][:], in_=wbs[p2][:]), reads=['wbs%d' % p2], writes=['wbb%d' % p2])
                    P.dma('act', gas[p2][:], gaTd[nb], writes=['gas%d' % p2])
                    P.dma('act', gbs[p2][:], gbTd[nb], writes=['gbs%d' % p2])
                    for oc in range(4):
                        q2 = nn % 2
                        nn += 1
                        ba = 0 + q2
                        bb = 2 + q2
                        for kc in range(8):
                            P.op('pe', lambda pe, kc=kc, p2=p2, oc=oc, ba=ba: pe.matmul(
                                banks[ba][:, :], lhsT=wab[p2][:, kc, :], rhs=oaT[:, kc, oc * 512:(oc + 1) * 512],
                                start=(kc == 0), stop=(kc == 7)), reads=['wab%d' % p2, 'oaT'], writes=['b%d' % ba], excl=B(ba))
                        for kc in range(8):
                            P.op('pe', lambda pe, kc=kc, p2=p2, oc=oc, bb=bb: pe.matmul(
                                banks[bb][:, :], lhsT=wbb[p2][:, kc, :], rhs=obT[:, kc, oc * 512:(oc + 1) * 512],
                                start=(kc == 0), stop=(kc == 7)), reads=['wbb%d' % p2, 'obT'], writes=['b%d' % bb], excl=B(bb))
                        P.op('dve', lambda v, p2=p2, oc=oc, ba=ba, q2=q2: v.tensor_tensor(
                            out=t1[q2][:], in0=banks[ba][:, :], in1=gas[p2][:, oc * 512:(oc + 1) * 512], op=ALU.mult),
                            reads=['b%d' % ba, 'gas%d' % p2], writes=['t1%d' % q2], excl=B(ba))
                        P.op('dve', lambda v, p2=p2, oc=oc, bb=bb, q2=q2: v.tensor_tensor(
                            out=t2[q2][:], in0=banks[bb][:, :], in1=gbs[p2][:, oc * 512:(oc + 1) * 512], op=ALU.mult),
                            reads=['b%d' % bb, 'gbs%d' % p2], writes=['t2%d' % q2], excl=B(bb))
                        P.op('pool', lambda g, p2=p2, oc=oc, q2=q2: g.tensor_tensor(
                            out=mst[p2][:, oc * 512:(oc + 1) * 512], in0=t1[q2][:], in1=t2[q2][:], op=ALU.add),
                            reads=['t1%d' % q2, 't2%d' % q2], writes=[('mst%d' % p2, oc)])
                    P.dma('sp', mTd[nb], mst[p2][:], reads=[('mst%d' % p2, oc) for oc in range(4)], writes=[('mTd', nb)])
                P.barrier()
                P.flush()

        if upto >= 5:
            with contextlib.ExitStack() as ph:
                Wo = sb(ph, "Wo", [128, NKC, D], BF16)
                wst = [sb(ph, "wsto%d" % i, [128, 2048], F32) for i in range(2)]
                for kc in range(NKC):
                    w = wst[kc % 2]
                    wk = 'wsto%d' % (kc % 2)
                    P.dma('sp', w[:], w_out[kc * 128:(kc + 1) * 128, :], writes=[wk])
                    P.op('pool', lambda g, w=w, kc=kc: g.tensor_copy(out=Wo[:, kc, :], in_=w[:]), reads=[wk], writes=[('Wo', kc)])
                G1 = bcast_load(ph, "G1", modv[0:1, 2 * D:3 * D])
                L1g = bcast_load(ph, "L1g", ln1_g)
                L1b = bcast_load(ph, "L1b", ln1_b)
                brt = bcast_load(ph, "brt", b_rt)
                Wrt = sb(ph, "Wrt", [128, NKC, 36], F32)
                P.dma('sp', Wrt[:], w_rt.rearrange("(c p) n -> p c n", p=128), writes=['Wrt'])
                lb = ln_bufs(ph)
                xin, xn, st, mv, sd, cnt = lb
                mts = [sb(ph, "mts%d" % i, [128, NKC, 128], BF16) for i in range(2)]
                tg = sb(ph, "tg", [128, D], F32)
                rt = sb(ph, "rt", [128, D], F32)
                x1t = sb(ph, "x1t", [128, D], F32)
                h2t = [sb(ph, "h2t%d" % i, [128, NKC, 128], BF16) for i in range(2)]
                h2f = sb(ph, "h2f", [128, NKC, 128], F32)
                lg = sb(ph, "lg", [128, 36], F32)
                rs = sb(ph, "rs", [128, 64], F32)
                wdt = [sb(ph, "wdt%d" % i, [128, 32], F32) for i in range(2)]
                for t in range(16):
                    p2 = t % 2
                    mt = mts[p2]
                    P.dma('sp', mt[:], mTd[:, :, t * 128:(t + 1) * 128].rearrange("c p t -> p c t"), writes=['mts%d' % p2])
                    xt = xin[t % 2]
                    xk = 'xin%d' % (t % 2)
                    P.dma('act', xt[:], xo[t * 128:(t + 1) * 128, :], writes=[xk])
                    for fb in range(4):
                        for kc in range(NKC):
                            P.op('pe', lambda pe, kc=kc, fb=fb, mt=mt: pe.matmul(
                                banks[fb][:, :], lhsT=mt[:, kc, :], rhs=Wo[:, kc, fb * 512:(fb + 1) * 512],
                                start=(kc == 0), stop=(kc == NKC - 1)),
                                reads=['mts%d' % p2, ('Wo', kc)], writes=['b%d' % fb], excl=B(fb))
                        P.op('dve', lambda v, fb=fb: v.tensor_tensor(
                            out=tg[:, fb * 512:(fb + 1) * 512], in0=banks[fb][:, :], in1=G1[:, fb * 512:(fb + 1) * 512], op=ALU.mult),
                            reads=['b%d' % fb, 'G1'], writes=[('tg', fb)], excl=B(fb))
                    P.op('dve', lambda v, xt=xt: v.scalar_tensor_tensor(out=rt[:], in0=xt[:], scalar=ALPHA, in1=tg[:],
                                                                       op0=ALU.mult, op1=ALU.add),
                         reads=[xk] + [('tg', fb) for fb in range(4)], writes=['rt'])
                    layer_norm_tile(rt, 'rt', xn, 'xn', st, mv, sd, eps_t)
                    P.op('dve', lambda v: v.tensor_tensor(out=xn[:], in0=xn[:], in1=L1g[:], op=ALU.mult), reads=['xn', 'L1g'], writes=['xn'])
                    P.op('pool', lambda g: g.tensor_tensor(out=x1t[:], in0=xn[:], in1=L1b[:], op=ALU.add), reads=['xn', 'L1b'], writes=['x1t'])
                    P.dma('sp', x1d[t * 128:(t + 1) * 128, :], x1t[:], reads=['x1t'], writes=[('x1d', t)])
                    h2 = h2t[p2]
                    ln_T_tile(lb, None, h2, 'h2t%d' % p2, 0, 3, fdst=h2f, bo=4, xt_in=x1t, xk_in='x1t')
                    P.dma('sp', h2Td[:, :, t * 128:(t + 1) * 128].rearrange("c p t -> p c t"), h2[:],
                          reads=[('h2t%d' % p2, kc, 0) for kc in range(NKC)], writes=[('h2Td', t)])
                    for kc in range(NKC):
                        P.op('pe', lambda pe, kc=kc: pe.matmul(banks[0][:, 0:36], lhsT=h2f[:, kc, :], rhs=Wrt[:, kc, :],
                                                                start=(kc == 0), stop=(kc == NKC - 1)),
                             reads=[('fdst', kc), 'Wrt'], writes=['b0'], excl=B(0))
                    P.op('dve', lambda v: v.tensor_tensor(out=lg[:], in0=banks[0][:, 0:36], in1=brt[:], op=ALU.add),
                         reads=['b0', 'brt'], writes=['lg'], excl=B(0))
                    wd_ = wdt[p2]
                    V = lambda f, r, w: P.op('dve', f, reads=r, writes=w)
                    V(lambda v: v.reduce_max(out=rs[:, 0:1], in_=lg[:, 0:4], axis=mybir.AxisListType.X), ['lg'], ['r0'])
                    V(lambda v: v.tensor_scalar(out=rs[:, 1:2], in0=rs[:, 0:1], scalar1=-1.0, scalar2=None, op0=ALU.mult), ['r0'], ['r1'])
                    P.op('act', lambda a: a.activation(out=rs[:, 4:8], in_=lg[:, 0:4], func=AF.Exp, bias=rs[:, 1:2], scale=1.0,
                                                       accum_out=rs[:, 2:3]), reads=['lg', 'r1'], writes=['r2'])
                    V(lambda v: v.reciprocal(out=rs[:, 3:4], in_=rs[:, 2:3]), ['r2'], ['r3'])
                    V(lambda v: v.tensor_scalar(out=rs[:, 8:12], in0=lg[:, 0:4], scalar1=rs[:, 0:1], scalar2=None, op0=ALU.is_equal),
                      ['lg', 'r0'], ['gm'])
                    V(lambda v: v.tensor_scalar(out=rs[:, 12:20], in0=lg[:, 4:12], scalar1=rs[:, 8:9], scalar2=None, op0=ALU.mult),
                      ['lg', 'gm'], ['sel'])
                    for g in range(1, 4):
                        V(lambda v, g=g: v.scalar_tensor_tensor(out=rs[:, 12:20], in0=lg[:, 4 + 8 * g:12 + 8 * g], scalar=rs[:, 8 + g:9 + g],
                                                                in1=rs[:, 12:20], op0=ALU.mult, op1=ALU.add), ['lg', 'gm', 'sel'], ['sel'])
                    V(lambda v: v.max(out=rs[:, 20:28], in_=rs[:, 12:20]), ['sel'], ['top8'])
                    V(lambda v: v.tensor_scalar(out=rs[:, 28:36], in0=rs[:, 12:20], scalar1=rs[:, 20:21], scalar2=None, op0=ALU.is_equal),
                      ['sel', 'top8'], ['m1'])
                    V(lambda v: v.tensor_scalar(out=rs[:, 36:44], in0=rs[:, 12:20], scalar1=rs[:, 21:22], scalar2=None, op0=ALU.is_equal),
                      ['sel', 'top8'], ['m2'])
                    V(lambda v: v.tensor_tensor(out=rs[:, 44:45], in0=rs[:, 21:22], in1=rs[:, 20:21], op=ALU.subtract), ['top8'], ['rd'])
                    P.op('act', lambda a: a.activation(out=rs[:, 45:46], in_=rs[:, 44:45], func=AF.Exp), reads=['rd'], writes=['red'])
                    V(lambda v: v.tensor_scalar(out=rs[:, 46:47], in0=rs[:, 45:46], scalar1=1.0, scalar2=None, op0=ALU.add), ['red'], ['rden'])
                    V(lambda v: v.reciprocal(out=rs[:, 47:48], in_=rs[:, 46:47]), ['rden'], ['rw1'])
                    V(lambda v: v.tensor_tensor(out=rs[:, 48:49], in0=rs[:, 45:46], in1=rs[:, 47:48], op=ALU.mult), ['red', 'rw1'], ['rw2'])
                    V(lambda v: v.tensor_tensor(out=rs[:, 49:50], in0=rs[:, 47:48], in1=rs[:, 3:4], op=ALU.mult), ['rw1', 'r3'], ['rw1p'])
                    V(lambda v: v.tensor_tensor(out=rs[:, 50:51], in0=rs[:, 48:49], in1=rs[:, 3:4], op=ALU.mult), ['rw2', 'r3'], ['rw2p'])
                    V(lambda v: v.tensor_scalar(out=rs[:, 52:60], in0=rs[:, 28:36], scalar1=rs[:, 49:50], scalar2=None, op0=ALU.mult),
                      ['m1', 'rw1p'], ['cw'])
                    V(lambda v: v.scalar_tensor_tensor(out=rs[:, 52:60], in0=rs[:, 36:44], scalar=rs[:, 50:51], in1=rs[:, 52:60],
                                                       op0=ALU.mult, op1=ALU.add), ['m2', 'rw2p', 'cw'], ['cw'])
                    for g in range(4):
                        V(lambda v, g=g, wd_=wd_: v.tensor_scalar(out=wd_[:, g * 8:(g + 1) * 8], in0=rs[:, 52:60], scalar1=rs[:, 8 + g:9 + g],
                                                                  scalar2=None, op0=ALU.mult), ['cw', 'gm'], [('wdt%d' % p2, g)])
                    P.dma('sp', wdd[t * 128:(t + 1) * 128, :], wd_[:], reads=[('wdt%d' % p2, g) for g in range(4)], writes=[('wdd', t)])
                P.barrier()
                P.flush()

        if upto >= 6:
            for half in range(2):
                with contextlib.ExitStack() as ph:
                    y2 = sb(ph, "y2", [128, 8, D], F32)
                    wdh = sb(ph, "wdh", [128, 8, NEXP], F32)
                    h2h = sb(ph, "h2h", [128, NKC, 1024], BF16)
                    P.dma('sp', h2h[:], h2Td[:, :, half * 1024:(half + 1) * 1024].rearrange("c p t -> p c t"), writes=['h2h'])
                    P.dma('sp', wdh[:], wdd[half * 1024:(half + 1) * 1024, :].rearrange("(c p) e -> p c e", p=128), writes=['wdh'])
                    for ti in range(8):
                        P.op('pool', lambda g, ti=ti: g.memset(y2[:, ti, :], 0.0), writes=[('y2', ti, fb) for fb in range(4)])
                    if debug:
                        P.dma('sp', dbg_y0[half], y2[:].rearrange("p a b -> p (a b)"),
                              reads=[('y2', ti, fb) for ti in range(8) for fb in range(4)], writes=['dbg_y0'])
                        P.dma('sp', dbg_wdh[half], wdh[:].rearrange("p a b -> p (a b)"), reads=['wdh'], writes=['dbg_wdh'])
                        P.dma('sp', dbg_h2h[half], h2h[:].rearrange("p a b -> p (a b)"), reads=['h2h'], writes=['dbg_h2h'])
                    with contextlib.ExitStack() as ph2:
                        hact = [sb(ph2, "hact%d" % i, [128, 4, 512], BF16) for i in range(2)]
                        sil = [sb(ph2, "sil%d" % i, [128, 512], F32) for i in range(2)]
                        wg = sb(ph2, "wg", [128, NKC, DEXP], BF16)
                        wu = sb(ph2, "wu", [128, NKC, DEXP], BF16)
                        wdn = sb(ph2, "wdn", [128, 4, D], BF16)
                        NSTG = 3
                        stg = [sb(ph2, "stg%d" % i, [128, 1024], F32) for i in range(NSTG)]
                        pad = sb(ph2, "pad", [128, 1024], F32)
                        P.op('pool', lambda g: g.memset(pad[:], 0.0), writes=['pad'])
                        ns = 0
                        nsl = 0
                        nh = 0
                        nbk = 0
                        for e in range(NEXP):
                            for (src, dstw, dk_, nhalf, ceng) in ((w_gate[e], wg, 'wg', 2, 'pool'), (w_up[e], wu, 'wu', 2, 'act'),
                                                                  (w_down[e], wdn, 'wdn', 2, 'pool')):
                                for hf in range(8):
                                    sg = stg[ns % NSTG]
                                    sk = 'stg%d' % (ns % NSTG)
                                    ns += 1
                                    if dk_ == 'wdn':
                                        dcw, hh = hf // 2, hf % 2
                                        P.dma('sp', sg[:], src[dcw * 128:(dcw + 1) * 128, hh * 1024:(hh + 1) * 1024], writes=[sk])
                                        dst_ap = dstw[:, dcw, hh * 1024:(hh + 1) * 1024]
                                    else:
                                        P.dma('sp', sg[:].rearrange("p (c n) -> p c n", c=2),
                                              src[hf * 256:(hf + 1) * 256, :].rearrange("(c p) n -> p c n", p=128), writes=[sk])
                                        dst_ap = dstw[:, hf * 2:(hf + 1) * 2, :].rearrange("p c n -> p (c n)")
                                    if ceng == 'pool':
                                        P.op('pool', lambda g, sg=sg, dst_ap=dst_ap: g.tensor_copy(out=dst_ap, in_=sg[:]),
                                             reads=[sk], writes=[(dk_, hf)])
                                    else:
                                        P.op('act', lambda a, sg=sg, dst_ap=dst_ap: a.copy(out=dst_ap, in_=sg[:]),
                                             reads=[sk], writes=[(dk_, hf)])
                            for ch in range(2):
                                ha = hact[nh % 2]
                                hk = 'hact%d' % (nh % 2)
                                nh += 1
                                for dc in range(4):
                                    bg = nbk % 2
                                    bu = 2 + nbk % 2
                                    nbk += 1
                                    for kc in range(NKC):
                                        P.op('pe', lambda pe, kc=kc, dc=dc, ch=ch, bg=bg: pe.matmul(
                                            banks[bg][:, :], lhsT=wg[:, kc, dc * 128:(dc + 1) * 128], rhs=h2h[:, kc, ch * 512:(ch + 1) * 512],
                                            start=(kc == 0), stop=(kc == NKC - 1)), reads=[('wg', kc // 2), 'h2h'], writes=['b%d' % bg], excl=B(bg))
                                    for kc in range(NKC):
                                        P.op('pe', lambda pe, kc=kc, dc=dc, ch=ch, bu=bu: pe.matmul(
                                            banks[bu][:, :], lhsT=wu[:, kc, dc * 128:(dc + 1) * 128], rhs=h2h[:, kc, ch * 512:(ch + 1) * 512],
                                            start=(kc == 0), stop=(kc == NKC - 1)), reads=[('wu', kc // 2), 'h2h'], writes=['b%d' % bu], excl=B(bu))
                                    sl = sil[nsl % 2]
                                    slk = 'sil%d' % (nsl % 2)
                                    nsl += 1
                                    P.op('act', lambda a, sl=sl, bg=bg: a.activation(out=sl[:], in_=banks[bg][:, :], func=AF.Silu),
                                         reads=['b%d' % bg], writes=[slk], excl=B(bg))
                                    P.op('dve', lambda v, sl=sl, bu=bu, ha=ha, dc=dc: v.tensor_tensor(
                                        out=ha[:, dc, :], in0=banks[bu][:, :], in1=sl[:], op=ALU.mult),
                                        reads=['b%d' % bu, slk], writes=[(hk, dc)], excl=B(bu))
                                for tl in range(4):
                                    ti = ch * 4 + tl
                                    for fb in range(4):
                                        bd = 4 + fb
                                        for dc in range(4):
                                            P.op('pe', lambda pe, dc=dc, tl=tl, fb=fb, bd=bd, ha=ha: pe.matmul(
                                                banks[bd][:, :], lhsT=ha[:, dc, tl * 128:(tl + 1) * 128], rhs=wdn[:, dc, fb * 512:(fb + 1) * 512],
                                                start=(dc == 0), stop=(dc == 3)), reads=[(hk, dc), ('wdn', dc * 2 + fb // 2)], writes=['b%d' % bd], excl=B(bd))
                                        P.op('dve', lambda v, ti=ti, fb=fb, bd=bd, e=e: v.scalar_tensor_tensor(
                                            out=y2[:, ti, fb * 512:(fb + 1) * 512], in0=banks[bd][:, :], scalar=wdh[:, ti, e:e + 1],
                                            in1=y2[:, ti, fb * 512:(fb + 1) * 512], op0=ALU.mult, op1=ALU.add),
                                            reads=['b%d' % bd, 'wdh', ('y2', ti, fb)], writes=[('y2', ti, fb)], excl=B(bd))
                        if debug:
                            P.dma('sp', dbg_pad[half], pad[:], reads=['pad'], writes=['dbg_pad'])
                            for q in range(2):
                                P.dma('sp', dbg_hact[half, q], hact[q][:].rearrange("p a b -> p (a b)"),
                                      reads=[('hact%d' % q, dc) for dc in range(4)], writes=[('dbg_hact', q)])
                    P.barrier()
                    if debug:
                        for ti in range(8):
                            t = half * 8 + ti
                            P.dma('sp', y2d[t * 128:(t + 1) * 128, :], y2[:, ti, :],
                                  reads=[('y2', ti, fb) for fb in range(4)], writes=[('y2d', t)])
                    with contextlib.ExitStack() as ph3:
                        G2 = bcast_load(ph3, "G2", modv[0:1, 5 * D:6 * D])
                        L2g = bcast_load(ph3, "L2g", ln2_g)
                        L2b = bcast_load(ph3, "L2b", ln2_b)
                        lb = ln_bufs(ph3)
                        xin, xn, st, mv, sd, cnt = lb
                        tg2 = sb(ph3, "tg2", [128, D], F32)
                        rt2 = sb(ph3, "rt2", [128, D], F32)
                        ot = [sb(ph3, "ot%d" % i, [128, D], F32) for i in range(2)]
                        for ti in range(8):
                            t = half * 8 + ti
                            xt = xin[ti % 2]
                            xk = 'xin%d' % (ti % 2)
                            P.dma('act', xt[:], x1d[t * 128:(t + 1) * 128, :], writes=[xk])
                            P.op('pool', lambda g, ti=ti: g.tensor_tensor(out=tg2[:], in0=y2[:, ti, :], in1=G2[:], op=ALU.mult),
                                 reads=[('y2', ti, fb) for fb in range(4)] + ['G2'], writes=['tg2'])
                            P.op('dve', lambda v, xt=xt: v.scalar_tensor_tensor(out=rt2[:], in0=xt[:], scalar=ALPHA, in1=tg2[:],
                                                                               op0=ALU.mult, op1=ALU.add), reads=[xk, 'tg2'], writes=['rt2'])
                            layer_norm_tile(rt2, 'rt2', xn, 'xn', st, mv, sd, eps_t)
                            o = ot[ti % 2]
                            ok_ = 'ot%d' % (ti % 2)
                            P.op('dve', lambda v: v.tensor_tensor(out=xn[:], in0=xn[:], in1=L2g[:], op=ALU.mult), reads=['xn', 'L2g'], writes=['xn'])
                            P.op('pool', lambda g, o=o: g.tensor_tensor(out=o[:], in0=xn[:], in1=L2b[:], op=ALU.add), reads=['xn', 'L2b'], writes=[ok_])
                            P.dma('sp', out[t * 128:(t + 1) * 128, :], o[:], reads=[ok_], writes=[('out', t)])
                    P.barrier()
                    P.flush()
        P.barrier()
        P.flush()
    return nc


def make_in_maps(inputs):
    x = np.asarray(inputs["x"], dtype=np.float32)
    c = np.asarray(inputs["c"], dtype=np.float32)
    g = lambda k: np.ascontiguousarray(np.asarray(inputs[k], dtype=np.float32)[0])
    w_rt = np.ascontiguousarray(np.concatenate([g("w_group"), g("w_router")], axis=1))
    b_rt = np.ascontiguousarray(np.concatenate([g("b_group"), g("b_router")], axis=0)[None, :])
    sgu_wT = np.ascontiguousarray(g("sgu_w").transpose(2, 0, 1))
    shared = {
        "w_ada": g("w_ada"), "b_ada": g("b_ada")[None, :], "w_in": g("w_in"),
        "sgu_wT": sgu_wT, "sgu_b": g("sgu_b").reshape(1, -1),
        "sgu_ln_g": g("sgu_ln_g")[None, :], "sgu_ln_b": g("sgu_ln_b")[None, :],
        "w_proj_a": g("w_proj_a"), "w_proj_b": g("w_proj_b"), "w_out": g("w_out"),
        "ln1_g": g("ln1_g")[None, :], "ln1_b": g("ln1_b")[None, :],
        "w_rt": w_rt, "b_rt": b_rt,
        "w_gate": g("w_gate"), "w_up": g("w_up"), "w_down": g("w_down"),
        "ln2_g": g("ln2_g")[None, :], "ln2_b": g("ln2_b")[None, :],
    }
    maps = []
    for core in range(8):
        b = core // 4
        m = dict(shared)
        j = core % 4
        m["xb"] = np.ascontiguousarray(x[b])
        m["xo"] = np.ascontiguousarray(x[b].reshape(16, 4, 128, D)[:, j].reshape(OWN, D))
        mk = np.zeros((128, 4, 128), np.float32)
        for mm in range(4):
            if mm < j:
                mk[:, mm, :] = 1.0
            elif mm == j:
                mk[:, mm, :] = np.triu(np.ones((128, 128), np.float32), k=1)
        m["maskd"] = mk
        m["cT"] = np.ascontiguousarray(c[b].reshape(NKC, 128).T)
        maps.append(m)
    return maps


_NC_CACHE = {}


def kernel(**inputs):
    if "nc" not in _NC_CACHE:
        _NC_CACHE["nc"] = build()
    nc = _NC_CACHE["nc"]
    maps = make_in_maps(inputs)
    res = run_bass_kernel_spmd(nc, maps, core_ids=list(range(8)))
    full = np.zeros((2, S, D), np.float32)
    for core in range(8):
        b, j = core // 4, core % 4
        o = np.asarray(res.results[core]["out"], dtype=np.float32).reshape(16, 128, D)
        full[b].reshape(16, 4, 128, D)[:, j] = o
    return full
```

```python
import contextlib
import os
import numpy as np
import ml_dtypes
import concourse.bass as bass
import concourse.mybir as mybir
from concourse.bass_utils import run_bass_kernel_spmd

F32 = mybir.dt.float32
BF16 = mybir.dt.bfloat16
U32 = mybir.dt.uint32
AF = mybir.ActivationFunctionType
ALU = mybir.AluOpType

D = 2048
S = 8192
NKC = 16
OWN = 2048
LN_EPS = 1e-5
ALPHA = 2.0 ** 0.25
NEXP = 32
DEXP = 512


class Prog:
    LIMIT = 30000
    NDMA = 6

    def __init__(self, nc, stack):
        self.nc = nc
        self.stack = stack
        self.names = ['pe', 'act', 'dve', 'pool', 'sp']
        self.sems = []
        self.ops = {e: [] for e in self.names}
        self.cur = {}
        self.cnt = {}
        for e in self.names:
            self.cur[e] = self._new_sem("c_" + e)
            self.cnt[e] = 0
        self.waited = {e: {} for e in self.names}
        self.lastw = {}
        self.readers = {}
        self.latest = {}
        self.dq = {}
        self.dn = {}
        for q in ['sp', 'act', 'pool']:
            self.dq[q] = [self._new_sem("d_%s%d" % (q, i)) for i in range(self.NDMA)]
            self.dn[q] = 0
        self.n_instr = 0

    def _new_sem(self, name):
        h = self.stack.enter_context(self.nc.semaphore(name + "_%d" % len(self.sems)))
        self.sems.append(h)
        return len(self.sems) - 1

    def _deps(self, e, reads, writes, excl=()):
        deps = []
        for k in excl:
            t = self.lastw.get(k)
            if t is not None and t[0] != self.cur.get(e, -1):
                deps.append(t)
        for k in reads:
            t = self.lastw.get(k)
            if t is not None:
                deps.append(t)
        for k in writes:
            t = self.lastw.get(k)
            if t is not None:
                deps.append(t)
            deps.extend(self.readers.get(k, ()))
        waits = {}
        for (s, v) in deps:
            if e == 'pe' and s == self.cur['pe']:
                continue
            if self.waited[e].get(s, 0) >= v:
                continue
            if waits.get(s, 0) < v:
                waits[s] = v
        for s, v in waits.items():
            self.waited[e][s] = v
        return list(waits.items())

    def _commit(self, tok, reads, writes):
        for k in writes:
            self.lastw[k] = tok
            self.readers[k] = []
        for k in reads:
            if k in writes:
                continue
            self.readers.setdefault(k, []).append(tok)
        self.latest[tok[0]] = tok[1]

    def op(self, e, fn, reads=(), writes=(), excl=()):
        waits = self._deps(e, reads, writes, excl)
        if self.cnt[e] >= self.LIMIT:
            self.cur[e] = self._new_sem("c_" + e)
            self.cnt[e] = 0
        self.cnt[e] += 1
        tok = (self.cur[e], self.cnt[e])
        sems = self.sems

        def emit(eng, waits=waits, fn=fn, tok=tok):
            for (s, v) in waits:
                eng.wait_ge(sems[s], v)
            fn(eng).then_inc(sems[tok[0]], 1)
        self.ops[e].append(emit)
        self._commit(tok, reads, writes)
        for k in excl:
            self.lastw[k] = tok
        self.n_instr += 1
        return tok

    def dma(self, q, out, in_, reads=(), writes=(), **kw):
        waits = self._deps(q, reads, writes)
        i = self.dn[q]
        self.dn[q] += 1
        k = i % self.NDMA
        val = 16 * (i // self.NDMA + 1)
        s = self.dq[q][k]
        if i >= self.NDMA and self.waited[q].get(s, 0) < val - 16:
            waits.append((s, val - 16))
            self.waited[q][s] = val - 16
        tok = (s, val)
        sems = self.sems

        def emit(eng, waits=waits, tok=tok, out=out, in_=in_, kw=kw):
            for (ss, v) in waits:
                eng.wait_ge(sems[ss], v)
            eng.dma_start(out=out, in_=in_, **kw).then_inc(sems[tok[0]], 16)
        self.ops[q].append(emit)
        self._commit(tok, reads, writes)
        self.n_instr += 1
        return tok

    def barrier(self):
        for e in self.names:
            waits = []
            for s, v in self.latest.items():
                if self.waited[e].get(s, 0) < v:
                    waits.append((s, v))
                    self.waited[e][s] = v
            sems = self.sems

            def emit(eng, waits=waits):
                for (s, v) in waits:
                    eng.wait_ge(sems[s], v)
            self.ops[e].append(emit)
        self.lastw.clear()
        self.readers.clear()

    def flush(self):
        nc = self.nc
        ops = self.ops
        with nc.Block() as block:
            @block.tensor
            def _(eng):
                for f in ops['pe']:
                    f(eng)

            @block.scalar
            def _(eng):
                for f in ops['act']:
                    f(eng)

            @block.vector
            def _(eng):
                for f in ops['dve']:
                    f(eng)

            @block.gpsimd
            def _(eng):
                for f in ops['pool']:
                    f(eng)

            @block.sync
            def _(eng):
                for f in ops['sp']:
                    f(eng)
        self.ops = {e: [] for e in self.names}


def build(upto=99, debug=False, lite=False):
    nc = bass.Bass("TRN2", target_bir_lowering=False)
    dk = "ExternalOutput"

    def din(name, shape, dt=F32):
        return nc.dram_tensor(name, list(shape), dt, kind="ExternalInput").ap()

    xb = din("xb", [S, D])
    xo = din("xo", [OWN, D])
    maskd = din("maskd", [128, 4, 128])
    cT = din("cT", [128, NKC])
    w_ada = din("w_ada", [D, 6 * D])
    b_ada = din("b_ada", [1, 6 * D])
    w_in = din("w_in", [D, 9216])
    sgu_wT = din("sgu_wT", [128, 8, 128])
    sgu_b = din("sgu_b", [1, 8 * 128])
    sgu_ln_g = din("sgu_ln_g", [1, 1024])
    sgu_ln_b = din("sgu_ln_b", [1, 1024])
    w_pa = din("w_proj_a", [1024, D])
    w_pb = din("w_proj_b", [1024, D])
    w_out = din("w_out", [D, D])
    ln1_g = din("ln1_g", [1, D])
    ln1_b = din("ln1_b", [1, D])
    w_rt = din("w_rt", [D, 36])
    b_rt = din("b_rt", [1, 36])
    if not lite:
        w_gate = din("w_gate", [NEXP, D, DEXP])
        w_up = din("w_up", [NEXP, D, DEXP])
        w_down = din("w_down", [NEXP, DEXP, D])
    ln2_g = din("ln2_g", [1, D])
    ln2_b = din("ln2_b", [1, D])
    out = nc.dram_tensor("out", [OWN, D], F32, kind="ExternalOutput").ap()

    def dscr(name, shape, dt):
        return nc.dram_tensor(name, list(shape), dt, kind=dk).ap()

    modv = dscr("modv", [1, 6 * D], F32)
    KT = dscr("KT", [8, 128, S], BF16)
    VV = dscr("VV", [64, 128, 1024], BF16)
    QTd = dscr("QTd", [8, 128, OWN], BF16)
    obTd = dscr("obTd", [8, 128, OWN], BF16)
    gaTd = dscr("gaTd", [NKC, 128, OWN], BF16)
    gbTd = dscr("gbTd", [NKC, 128, OWN], BF16)
    oaTd = dscr("oaTd", [8, 128, OWN], BF16)
    x1d = dscr("x1d", [OWN, D], F32)
    h2Td = dscr("h2Td", [NKC, 128, OWN], BF16)
    wdd = dscr("wdd", [OWN, NEXP], F32)

    with contextlib.ExitStack() as top:
        P = Prog(nc, top)
        banks = [top.enter_context(nc.psum_tensor("bank%d" % i, [128, 512], F32)) for i in range(8)]
        ident = top.enter_context(nc.sbuf_tensor("ident", [128, 128], F32))
        identb = top.enter_context(nc.sbuf_tensor("identb", [128, 128], BF16))
        ones_r = top.enter_context(nc.sbuf_tensor("ones_r", [1, 128], F32))

        uniq = [0]

        def sb(ph, name, shape, dt):
            uniq[0] += 1
            return ph.enter_context(nc.sbuf_tensor("%s_%d" % (name, uniq[0]), list(shape), dt))

        P.op('pool', lambda g: g.memset(ident[:], 1.0), writes=['ident'])
        P.op('pool', lambda g: g.affine_select(out=ident[:], in_=ident[:], pattern=[[-1, 128]],
                                               compare_op=ALU.is_equal, fill=0.0, base=0,
                                               channel_multiplier=1),
             reads=['ident'], writes=['ident'])
        P.op('pool', lambda g: g.tensor_copy(out=identb[:], in_=ident[:]), reads=['ident'], writes=['identb'])
        P.op('pool', lambda g: g.memset(ones_r[:], 1.0), writes=['ones_r'])

        if upto >= 0:
            with contextlib.ExitStack() as ph:
                cact = sb(ph, "cact", [128, NKC], F32)
                craw = sb(ph, "craw", [128, NKC], F32)
                bada = sb(ph, "bada", [1, 6 * D], F32)
                wst = [sb(ph, "wst%d" % i, [128, 2048], F32) for i in range(3)]
                mrow = sb(ph, "mrow", [1, 2048], F32)
                P.dma('sp', craw[:], cT, writes=['craw'])
                P.dma('sp', bada[:], b_ada, writes=['bada'])
                P.op('act', lambda a: a.activation(out=cact[:], in_=craw[:], func=AF.Silu),
                     reads=['craw'], writes=['cact'])
                n = 0
                for g in range(6):
                    for kc in range(NKC):
                        w = wst[n % 3]
                        wk = 'wst%d' % (n % 3)
                        n += 1
                        P.dma('sp', w[:], w_ada[kc * 128:(kc + 1) * 128, g * 2048:(g + 1) * 2048], writes=[wk])
                        for jj in range(4):
                            P.op('pe', lambda pe, w=w, jj=jj, kc=kc: pe.matmul(
                                banks[jj][0:1, :], lhsT=cact[:, kc:kc + 1], rhs=w[:, jj * 512:(jj + 1) * 512],
                                start=(kc == 0), stop=(kc == NKC - 1)),
                                reads=[wk, 'cact'], writes=['b%d' % jj])
                    for jj in range(4):
                        P.op('dve', lambda v, jj=jj, g=g: v.tensor_tensor(
                            out=mrow[0:1, jj * 512:(jj + 1) * 512], in0=banks[jj][0:1, :],
                            in1=bada[0:1, g * 2048 + jj * 512: g * 2048 + (jj + 1) * 512], op=ALU.add),
                            reads=['b%d' % jj, 'bada'], writes=['mrow%d' % jj])
                    P.dma('sp', modv[0:1, g * 2048:(g + 1) * 2048], mrow[:],
                          reads=['mrow%d' % jj for jj in range(4)], writes=['modv'])
                P.barrier()
                P.flush()

        mfm = top.enter_context(nc.sbuf_tensor("mfm", [128, 6, NKC], F32))
        mview = modv.rearrange("o (s c p) -> p (o s) c", p=128, c=NKC)
        for s6 in range(6):
            P.dma('sp', mfm[:, s6, :], mview[:, s6, :], reads=['modv'], writes=['mfm'],
                  allow_slow_non_contiguous=True)
        P.op('dve', lambda v: v.tensor_scalar(out=mfm[:, 1, :], in0=mfm[:, 1, :], scalar1=1.0, scalar2=None,
                                              op0=ALU.add), reads=['mfm'], writes=['mfm'])
        P.op('dve', lambda v: v.tensor_scalar(out=mfm[:, 4, :], in0=mfm[:, 4, :], scalar1=1.0, scalar2=None,
                                              op0=ALU.add), reads=['mfm'], writes=['mfm'])

        def layer_norm_tile(xt, xk, xn, xnk, st, mv, sd, eps_t):
            for q in range(4):
                P.op('dve', lambda v, q=q: v.bn_stats(out=st[:, q, :], in_=xt[:, q * 512:(q + 1) * 512]),
                     reads=[xk], writes=['st%d' % q])
            P.op('dve', lambda v: v.bn_aggr(out=mv[:], in_=st[:].rearrange("p a b -> p (a b)")),
                 reads=['st%d' % q for q in range(4)], writes=['mv'])
            P.op('act', lambda a: a.activation(out=sd[:, 0:1], in_=mv[:, 1:2], func=AF.Sqrt, bias=eps_t[:, 0:1],
                                               scale=1.0),
                 reads=['mv', 'eps'], writes=['sd0'])
            P.op('dve', lambda v: v.reciprocal(out=sd[:, 1:2], in_=sd[:, 0:1]), reads=['sd0'], writes=['sd1'])
            P.op('dve', lambda v: v.tensor_scalar(out=sd[:, 2:3], in0=mv[:, 0:1], scalar1=sd[:, 1:2], scalar2=-1.0,
                                                  op0=ALU.mult, op1=ALU.mult),
                 reads=['mv', 'sd1'], writes=['sd2'])
            P.op('act', lambda a: a.activation(out=xn[:], in_=xt[:], func=AF.Identity, bias=sd[:, 2:3],
                                               scale=sd[:, 1:2]),
                 reads=[xk, 'sd1', 'sd2'], writes=[xnk])

        eps_t = top.enter_context(nc.sbuf_tensor("eps_t", [128, 1], F32))
        P.op('pool', lambda g: g.memset(eps_t[:], LN_EPS), writes=['eps'])

        def B(bk):
            return ['B%d' % bk]

        def ln_T_tile(ph_bufs, src_rows, dst, dkey, col0, ms, fdst=None, bo=0, xt_in=None, xk_in=None):
            xin, xn, st, mv, sd, cnt = ph_bufs
            xt = xin[cnt[0] % 2]
            xk = 'xin%d' % (cnt[0] % 2)
            cnt[0] += 1
            if src_rows is not None:
                P.dma('act', xt[:], src_rows, writes=[xk])
            else:
                xt, xk = xt_in, xk_in
            layer_norm_tile(xt, xk, xn, 'xn', st, mv, sd, eps_t)
            for kc in range(NKC):
                bk = bo + kc // 4
                P.op('pe', lambda pe, kc=kc, bk=bk: pe.transpose(
                    banks[bk][:, (kc % 4) * 128:(kc % 4 + 1) * 128], xn[:, kc * 128:(kc + 1) * 128], ident[:]),
                    reads=['xn', 'ident'], writes=[('bq', bk, kc % 4)], excl=B(bk))
            for kc in range(NKC):
                bk = bo + kc // 4
                q = kc % 4
                if bk - bo < 2:
                    P.op('dve', lambda v, kc=kc, bk=bk, q=q: v.tensor_scalar(
                        out=dst[:, kc, col0:col0 + 128], in0=banks[bk][:, q * 128:(q + 1) * 128],
                        scalar1=mfm[:, ms + 1, kc:kc + 1], scalar2=mfm[:, ms, kc:kc + 1],
                        op0=ALU.mult, op1=ALU.add),
                        reads=[('bq', bk, q), 'mfm'], writes=[(dkey, kc, col0)], excl=B(bk))
                else:
                    P.op('act', lambda a, kc=kc, bk=bk, q=q: a.activation(
                        out=dst[:, kc, col0:col0 + 128], in_=banks[bk][:, q * 128:(q + 1) * 128],
                        func=AF.Identity, scale=mfm[:, ms + 1, kc:kc + 1], bias=mfm[:, ms, kc:kc + 1]),
                        reads=[('bq', bk, q), 'mfm'], writes=[(dkey, kc, col0)], excl=B(bk))
                if fdst is not None:
                    P.op('dve', lambda v, kc=kc, bk=bk, q=q: v.tensor_scalar(
                        out=fdst[:, kc, :], in0=banks[bk][:, q * 128:(q + 1) * 128],
                        scalar1=mfm[:, ms + 1, kc:kc + 1], scalar2=mfm[:, ms, kc:kc + 1],
                        op0=ALU.mult, op1=ALU.add),
                        reads=[('bq', bk, q), 'mfm'], writes=[('fdst', kc)], excl=B(bk))

        def ln_bufs(ph):
            xin = [sb(ph, "xin%d" % i, [128, 2048], F32) for i in range(2)]
            xn = sb(ph, "xn", [128, 2048], F32)
            st = sb(ph, "st", [128, 4, 6], F32)
            mv = sb(ph, "mv", [128, 2], F32)
            sd = sb(ph, "sd", [128, 4], F32)
            return (xin, xn, st, mv, sd, [0])

        evc = [0]

        def evac_copy(dst_ap, bk, wkeys, src=None):
            src = banks[bk][:, :] if src is None else src
            evc[0] += 1
            if evc[0] % 2 == 0:
                P.op('dve', lambda v: v.tensor_copy(out=dst_ap, in_=src), reads=['b%d' % bk], writes=wkeys, excl=B(bk))
            else:
                P.op('act', lambda a: a.copy(out=dst_ap, in_=src), reads=['b%d' % bk], writes=wkeys, excl=B(bk))

        if upto >= 1:
            with contextlib.ExitStack() as ph:
                Wkv = sb(ph, "Wkv", [128, NKC, 2048], BF16)
                wst = [sb(ph, "wstb%d" % i, [128, 2048], F32) for i in range(2)]
                lb = ln_bufs(ph)
                hT = [sb(ph, "hT%d" % i, [128, NKC, 512], BF16) for i in range(2)]
                kst = [sb(ph, "kst%d" % i, [128, 8, 512], BF16) for i in range(2)]
                vst = [sb(ph, "vst%d" % i, [128, 1024], BF16) for i in range(2)]
                for kc in range(NKC):
                    w = wst[kc % 2]
                    wk = 'wstb%d' % (kc % 2)
                    P.dma('sp', w[:], w_in[kc * 128:(kc + 1) * 128, 1024:3072], writes=[wk])
                    P.op('pool', lambda g, w=w, kc=kc: g.tensor_copy(out=Wkv[:, kc, :], in_=w[:]),
                         reads=[wk], writes=[('Wkv', kc)])
                nk = 0
                nv = 0
                for gc in range(16):
                    hb = hT[gc % 2]
                    hk = 'hT%d' % (gc % 2)
                    for tt in range(4):
                        r0 = gc * 512 + tt * 128
                        ln_T_tile(lb, xb[r0:r0 + 128, :], hb, hk, tt * 128, 0)
                    ks = kst[gc % 2]
                    kk = 'kst%d' % (gc % 2)
                    for h in range(8):
                        bk = 4 + (nk % 4)
                        nk += 1
                        for kc in range(NKC):
                            P.op('pe', lambda pe, kc=kc, h=h, bk=bk, hb=hb: pe.matmul(
                                banks[bk][:, :], lhsT=Wkv[:, kc, h * 128:(h + 1) * 128], rhs=hb[:, kc, :],
                                start=(kc == 0), stop=(kc == NKC - 1)),
                                reads=[('Wkv', kc)] + [(hk, kc, t4 * 128) for t4 in range(4)],
                                writes=['b%d' % bk], excl=B(bk))
                        evac_copy(ks[:, h, :], bk, [(kk, h)])
                    P.dma('sp', KT[:, :, gc * 512:(gc + 1) * 512].rearrange("h p t -> p h t"), ks[:],
                          reads=[(kk, h) for h in range(8)], writes=[('KT', gc)])
                    for tt in range(4):
                        vs = vst[nv % 2]
                        vk = 'vst%d' % (nv % 2)
                        nv += 1
                        for half in range(2):
                            bk = 4 + (nk % 4)
                            nk += 1
                            for kc in range(NKC):
                                P.op('pe', lambda pe, kc=kc, half=half, bk=bk, hb=hb, tt=tt: pe.matmul(
                                    banks[bk][:, :], lhsT=hb[:, kc, tt * 128:(tt + 1) * 128],
                                    rhs=Wkv[:, kc, 1024 + half * 512:1024 + (half + 1) * 512],
                                    start=(kc == 0), stop=(kc == NKC - 1)),
                                    reads=[('Wkv', kc), (hk, kc, tt * 128)], writes=['b%d' % bk], excl=B(bk))
                            evac_copy(vs[:, half * 512:(half + 1) * 512], bk, [(vk, half)])
                        P.dma('sp', VV[gc * 4 + tt], vs[:], reads=[(vk, 0), (vk, 1)], writes=[('VV', gc * 4 + tt)])
                P.barrier()
                P.flush()

        if upto >= 2:
            with contextlib.ExitStack() as ph:
                hO = sb(ph, "hO", [128, NKC, OWN], BF16)
                lb = ln_bufs(ph)
                wsh = [sb(ph, "wsh%d" % i, [128, 8, 512], F32) for i in range(2)]
                wbf = [sb(ph, "wbf%d" % i, [128, NKC, 512], BF16) for i in range(2)]
                ost = [sb(ph, "ost%d" % i, [128, 4, 512], BF16) for i in range(2)]
                for t in range(16):
                    ln_T_tile(lb, xo[t * 128:(t + 1) * 128, :], hO, 'hO', t * 128, 0)
                hO_keys = lambda kc, c0, n: [('hO', kc, c0 + 128 * q) for q in range(n)]
                nld = [0]

                def load_wblock(src2d, c0, wb, wbk, nkc=NKC):
                    for half in range(nkc // 8):
                        w = wsh[nld[0] % 2]
                        wk = 'wsh%d' % (nld[0] % 2)
                        nld[0] += 1
                        P.dma('sp', w[:], src2d[half * 1024:(half + 1) * 1024, c0:c0 + 512].rearrange(
                            "(c p) n -> p c n", p=128), writes=[wk])
                        P.op('pool', lambda g, w=w, half=half, wb=wb: g.tensor_copy(
                            out=wb[:, half * 8:(half + 1) * 8, :], in_=w[:]),
                            reads=[wk], writes=[(wbk, half)])

                blocks = []
                for i in range(2):
                    blocks.append((i * 512, 'copy', QTd, i * 4))
                for i in range(2):
                    blocks.append((3072 + i * 512, 'gelu', obTd, i * 4))
                for i in range(4):
                    blocks.append((5120 + i * 512, 'sig', gaTd, i * 4))
                for i in range(4):
                    blocks.append((7168 + i * 512, 'sig', gbTd, i * 4))
                nb = 0
                no = 0
                nbank = 0
                for (c0, fn, dstd, ch0) in blocks:
                    wb = wbf[nb % 2]
                    wbk = 'wbf%d' % (nb % 2)
                    nb += 1
                    load_wblock(w_in, c0, wb, wbk)
                    for oc in range(4):
                        os_ = ost[no % 2]
                        ok = 'ost%d' % (no % 2)
                        no += 1
                        for sub in range(4):
                            bk = 4 + (nbank % 4)
                            nbank += 1
                            for kc in range(NKC):
                                P.op('pe', lambda pe, kc=kc, sub=sub, bk=bk, wb=wb, oc=oc: pe.matmul(
                                    banks[bk][:, :], lhsT=wb[:, kc, sub * 128:(sub + 1) * 128],
                                    rhs=hO[:, kc, oc * 512:(oc + 1) * 512],
                                    start=(kc == 0), stop=(kc == NKC - 1)),
                                    reads=[(wbk, kc // 8)] + hO_keys(kc, oc * 512, 4), writes=['b%d' % bk], excl=B(bk))
                            if fn == 'copy':
                                evac_copy(os_[:, sub, :], bk, [(ok, sub)])
                            else:
                                f = AF.Gelu if fn == 'gelu' else AF.Sigmoid
                                P.op('act', lambda a, sub=sub, bk=bk, os_=os_, f=f: a.activation(
                                    out=os_[:, sub, :], in_=banks[bk][:, :], func=f),
                                    reads=['b%d' % bk], writes=[(ok, sub)], excl=B(bk))
                        P.dma('sp', dstd[ch0:ch0 + 4, :, oc * 512:(oc + 1) * 512].rearrange("c p t -> p c t"), os_[:],
                              reads=[(ok, q) for q in range(4)], writes=[(dstd.tensor.name, ch0, oc)])
                load_wblock(w_in, 4096, wbf[0], 'wbf0')
                load_wblock(w_in, 4608, wbf[1], 'wbf1')
                wsf = sb(ph, "wsf", [128, 8, 128], F32)
                wsb = sb(ph, "wsb", [128, 8, 128], BF16)
                bsb = sb(ph, "bsb", [128, 1024], F32)
                lnG = sb(ph, "lnG", [128, 1024], F32)
                lnB = sb(ph, "lnB", [128, 1024], F32)
                gv = sb(ph, "gv", [128, 1024], F32)
                vn0 = sb(ph, "vn0", [128, 1024], F32)
                vnb = sb(ph, "vnb", [128, 1024], BF16)
                uT = [sb(ph, "uT%d" % i, [128, 8, 128], BF16) for i in range(2)]
                tmpm = sb(ph, "tmpm", [128, 1024], F32)
                obs = [sb(ph, "obs%d" % i, [128, 8, 128], BF16) for i in range(2)]
                st2 = sb(ph, "st2", [128, 2, 6], F32)
                mv2 = sb(ph, "mv2", [128, 2], F32)
                sd2 = sb(ph, "sd2", [128, 4], F32)
                P.dma('sp', wsf[:], sgu_wT, writes=['wsf'])
                P.op('pool', lambda g: g.memset(wsf[64:128, :, 0:64], 0.0), reads=['wsf'], writes=['wsf'])
                P.op('pool', lambda g: g.tensor_copy(out=wsb[:], in_=wsf[:]), reads=['wsf'], writes=['wsb'])
                P.dma('sp', bsb[:], sgu_b.partition_broadcast(128).rearrange("p o f -> p (o f)"), writes=['bsb'])
                P.dma('sp', lnG[:], sgu_ln_g.partition_broadcast(128).rearrange("p o f -> p (o f)"), writes=['lnG'])
                P.dma('sp', lnB[:], sgu_ln_b.partition_broadcast(128).rearrange("p o f -> p (o f)"), writes=['lnB'])
                for t in range(16):
                    u = uT[t % 2]
                    uk = 'uT%d' % (t % 2)
                    ob = obs[t % 2]
                    obk = 'obs%d' % (t % 2)
                    P.dma('act', u[:], obTd[:, :, t * 128:(t + 1) * 128].rearrange("c p t -> p c t"),
                          reads=[('obTd', 0, t // 4), ('obTd', 4, t // 4)], writes=[uk])
                    for half in range(2):
                        bk = half
                        for kc in range(NKC):
                            P.op('pe', lambda pe, kc=kc, half=half, bk=bk, t=t: pe.matmul(
                                banks[bk][:, :], lhsT=hO[:, kc, t * 128:(t + 1) * 128], rhs=wbf[half][:, kc, :],
                                start=(kc == 0), stop=(kc == NKC - 1)),
                                reads=[('wbf%d' % half, kc // 8), ('hO', kc, t * 128)], writes=['b%d' % bk], excl=B(bk))
                        P.op('act', lambda a, half=half, bk=bk: a.activation(
                            out=gv[:, half * 512:(half + 1) * 512], in_=banks[bk][:, :], func=AF.Gelu),
                            reads=['b%d' % bk], writes=[('gv', half)], excl=B(bk))
                        P.op('dve', lambda v, half=half: v.bn_stats(out=st2[:, half, :], in_=gv[:, half * 512:(half + 1) * 512]),
                             reads=[('gv', half)], writes=[('st2', half)])
                    P.op('dve', lambda v: v.bn_aggr(out=mv2[:], in_=st2[:].rearrange("p a b -> p (a b)")),
                         reads=[('st2', 0), ('st2', 1)], writes=['mv2'])
                    P.op('act', lambda a: a.activation(out=sd2[:, 0:1], in_=mv2[:, 1:2], func=AF.Sqrt, bias=eps_t[:, 0:1], scale=1.0),
                         reads=['mv2', 'eps'], writes=['sd20'])
                    P.op('dve', lambda v: v.reciprocal(out=sd2[:, 1:2], in_=sd2[:, 0:1]), reads=['sd20'], writes=['sd21'])
                    P.op('dve', lambda v: v.tensor_scalar(out=sd2[:, 2:3], in0=mv2[:, 0:1], scalar1=sd2[:, 1:2], scalar2=-1.0,
                                                          op0=ALU.mult, op1=ALU.mult), reads=['mv2', 'sd21'], writes=['sd22'])
                    P.op('act', lambda a: a.activation(out=vn0[:], in_=gv[:], func=AF.Identity, bias=sd2[:, 2:3], scale=sd2[:, 1:2]),
                         reads=[('gv', 0), ('gv', 1), 'sd21', 'sd22'], writes=['vn0'])
                    P.op('dve', lambda v: v.tensor_tensor(out=vn0[:], in0=vn0[:], in1=lnG[:], op=ALU.mult),
                         reads=['vn0', 'lnG'], writes=['vn0'])
                    P.op('dve', lambda v: v.tensor_tensor(out=vnb[:], in0=vn0[:], in1=lnB[:], op=ALU.add),
                         reads=['vn0', 'lnB'], writes=['vnb'])
                    for g in range(8):
                        bk = 2 + g // 4
                        P.op('pe', lambda pe, g=g, bk=bk: pe.matmul(
                            banks[bk][:, (g % 4) * 128:(g % 4 + 1) * 128], lhsT=vnb[:, g * 128:(g + 1) * 128],
                            rhs=wsb[:, g, :], start=True, stop=True),
                            reads=['vnb', 'wsb'], writes=[('bq', bk, g % 4)], excl=B(bk))
                    for hh in range(2):
                        bk = 2 + hh
                        P.op('dve', lambda v, hh=hh, bk=bk: v.tensor_tensor(
                            out=tmpm[:, hh * 512:(hh + 1) * 512], in0=banks[bk][:, :], in1=bsb[:, hh * 512:(hh + 1) * 512],
                            op=ALU.add), reads=[('bq', bk, q) for q in range(4)] + ['bsb'], writes=[('tmpm', hh)], excl=B(bk))
                    P.op('pool', lambda gp, u=u, ob=ob: gp.tensor_tensor(
                        out=ob[:].rearrange("p c t -> p (c t)"), in0=tmpm[:], in1=u[:].rearrange("p c t -> p (c t)"), op=ALU.mult),
                        reads=[('tmpm', 0), ('tmpm', 1), uk], writes=[obk])
                    P.dma('sp', obTd[:, :, t * 128:(t + 1) * 128].rearrange("c p t -> p c t"), ob[:],
                          reads=[obk], writes=[('obTd2', t)])
                P.barrier()
                P.flush()

        if upto >= 3:
            with contextlib.ExitStack() as ph:
                KTh = sb(ph, "KTh", [128, 4, S], BF16)
                Vh = sb(ph, "Vh", [128, 64, 512], BF16)
                QTh = sb(ph, "QTh", [128, 4, OWN], BF16)
                mkf = sb(ph, "mkf", [128, 4, 128], F32)
                mk4 = sb(ph, "mk4", [128, 4, 4, 128], BF16)
                triI = sb(ph, "triI", [128, 128], BF16)
                triC = sb(ph, "triC", [128, 128], BF16)
                trif = sb(ph, "trif", [128, 128], F32)
                NS_ = 2
                eb = [[sb(ph, "eb%d_%d" % (q, i), [128, 512], F32) for i in range(2)] for q in range(NS_)]
                spb = [[sb(ph, "spb%d_%d" % (q, i), [128, 512], BF16) for i in range(2)] for q in range(NS_)]
                gb_ = [[sb(ph, "gb%d_%d" % (q, i), [128, 512], F32) for i in range(2)] for q in range(NS_)]
                wb_ = [[sb(ph, "wb%d_%d" % (q, i), [128, 512], BF16) for i in range(2)] for q in range(NS_)]
                w32 = [sb(ph, "w32_%d" % q, [128, 512], F32) for q in range(NS_)]
                oas = [[sb(ph, "oas%d_%d" % (q, i), [128, 4, 128], BF16) for i in range(2)] for q in range(NS_)]
                P.dma('sp', mkf[:], maskd, writes=['mkf'])
                for h in range(4):
                    P.op('pool', lambda g, h=h: g.tensor_copy(out=mk4[:, :, h, :], in_=mkf[:]), reads=['mkf'], writes=[('mk4', h)])
                mk4k = [('mk4', h) for h in range(4)]
                P.op('pool', lambda g: g.memset(trif[:], 1.0), writes=['trif'])
                P.op('pool', lambda g: g.affine_select(out=trif[:], in_=trif[:], pattern=[[-1, 128]], compare_op=ALU.is_ge,
                                                       fill=0.0, base=0, channel_multiplier=1), reads=['trif'], writes=['trif'])
                P.op('pool', lambda g: g.tensor_copy(out=triI[:], in_=trif[:]), reads=['trif'], writes=['triI'])
                P.op('pool', lambda g: g.memset(trif[:], 1.0), reads=['trif'], writes=['trif'])
                P.op('pool', lambda g: g.affine_select(out=trif[:], in_=trif[:], pattern=[[1, 128]], compare_op=ALU.is_gt,
                                                       fill=0.0, base=0, channel_multiplier=-1), reads=['trif'], writes=['trif'])
                P.op('pool', lambda g: g.tensor_copy(out=triC[:], in_=trif[:]), reads=['trif'], writes=['triC'])
                sc = 128.0 ** -0.5
                for hg in range(2):
                    for h in range(4):
                        P.dma('sp', KTh[:, h, :], KT[hg * 4 + h], writes=[('KTh', h)])
                    for q4 in range(4):
                        P.dma('act', Vh[:, q4 * 16:(q4 + 1) * 16, :],
                              VV[q4 * 16:(q4 + 1) * 16, :, hg * 512:(hg + 1) * 512].rearrange("b p f -> p b f"),
                              writes=[('Vh', q4)])
                    P.dma('sp', QTh[:], QTd[hg * 4:(hg + 1) * 4].rearrange("c p t -> p c t"), writes=['QTh'])
                    streams = [[(i, kb) for i in range(q, 16, NS_) for kb in range(4 * i + 3, -1, -1)] for q in range(NS_)]

                    def stageA(q, n):
                        i, kb = streams[q][n]
                        zb = 2 * q + (n % 2)
                        p = n % 2
                        e = eb[q][p]
                        sp_ = spb[q][p]
                        ek = 'e%d_%d' % (q, p)
                        sk = 'sp%d_%d' % (q, p)
                        for h in range(4):
                            P.op('pe', lambda pe, h=h, i=i, kb=kb, zb=zb: pe.matmul(
                                banks[zb][:, h * 128:(h + 1) * 128], lhsT=KTh[:, h, kb * 128:(kb + 1) * 128],
                                rhs=QTh[:, h, i * 128:(i + 1) * 128], start=True, stop=True),
                                reads=[('KTh', h), 'QTh'], writes=['b%d' % zb], excl=B(zb))
                        P.op('act', lambda a, zb=zb, e=e: a.activation(out=e[:], in_=banks[zb][:, :], func=AF.Exp, scale=sc),
                             reads=['b%d' % zb], writes=[ek], excl=B(zb))
                        P.op('act', lambda a, e=e, sp_=sp_: a.activation(out=sp_[:], in_=e[:], func=AF.Ln, bias=1.0, scale=1.0),
                             reads=[ek], writes=[sk])
                        if kb >= 4 * i:
                            m = kb - 4 * i
                            P.op('pool', lambda g, sp_=sp_, m=m: g.tensor_tensor(
                                out=sp_[:], in0=sp_[:], in1=mk4[:, m, :, :].rearrange("p h t -> p (h t)"), op=ALU.mult),
                                reads=[sk] + mk4k, writes=[sk])

                    def stageB(q, n):
                        i, kb = streams[q][n]
                        p = n % 2
                        e = eb[q][p]
                        sp_ = spb[q][p]
                        g_ = gb_[q][p]
                        w_ = wb_[q][p]
                        ek = 'e%d_%d' % (q, p)
                        sk = 'sp%d_%d' % (q, p)
                        gk = 'g%d_%d' % (q, p)
                        wk = 'w%d_%d' % (q, p)
                        btb = 4 + q
                        otb = 6 + q
                        first = (kb == 4 * i + 3)
                        last = (kb == 0)
                        P.op('pe', lambda pe: pe.matmul(banks[btb][:, :], lhsT=triI[:], rhs=sp_[:], start=first, stop=False),
                             reads=['triI', sk], writes=['b%d' % btb], excl=B(btb))
                        P.op('act', lambda a: a.activation(out=g_[:], in_=banks[btb][:, :], func=AF.Exp, scale=-1.0),
                             reads=['b%d' % btb], writes=[gk], excl=B(btb))
                        P.op('pe', lambda pe: pe.matmul(banks[btb][:, :], lhsT=triC[:], rhs=sp_[:], start=False, stop=last),
                             reads=['triC', sk], writes=['b%d' % btb], excl=B(btb))
                        if kb >= 4 * i:
                            m = kb - 4 * i
                            P.op('dve', lambda v: v.tensor_tensor(out=w32[q][:], in0=e[:], in1=g_[:], op=ALU.mult),
                                 reads=[ek, gk], writes=['w32_%d' % q])
                            P.op('dve', lambda v: v.tensor_tensor(
                                out=w_[:], in0=w32[q][:], in1=mk4[:, m, :, :].rearrange("p h t -> p (h t)"), op=ALU.mult),
                                reads=['w32_%d' % q] + mk4k, writes=[wk])
                        else:
                            P.op('dve', lambda v: v.tensor_tensor(out=w_[:], in0=e[:], in1=g_[:], op=ALU.mult),
                                 reads=[ek, gk], writes=[wk])
                        for h in range(4):
                            P.op('pe', lambda pe, h=h: pe.matmul(
                                banks[otb][:, h * 128:(h + 1) * 128], lhsT=Vh[:, kb, h * 128:(h + 1) * 128],
                                rhs=w_[:, h * 128:(h + 1) * 128], start=(first and h == 0), stop=last),
                                reads=[('Vh', kb // 16), wk], writes=['b%d' % otb], excl=B(otb))
                        if last:
                            oa = oas[q][(i // NS_) % 2]
                            ok_ = 'oas%d_%d' % (q, (i // NS_) % 2)
                            evac_copy(oa[:].rearrange("p c t -> p (c t)"), otb, [ok_])
                            P.dma('sp', oaTd[hg * 4:(hg + 1) * 4, :, i * 128:(i + 1) * 128].rearrange("c p t -> p c t"), oa[:],
                                  reads=[ok_], writes=[('oaTd', hg, i)])

                    lens = [len(st_) for st_ in streams]
                    for q in range(NS_):
                        stageA(q, 0)
                    for n in range(max(lens)):
                        for q in range(NS_):
                            if n + 1 < lens[q]:
                                stageA(q, n + 1)
                        for q in range(NS_):
                            if n < lens[q]:
                                stageB(q, n)
                P.barrier()
                P.flush()
        mTd = dscr("mTd", [NKC, 128, OWN], BF16)
        y2d = dscr("y2d", [OWN, D], F32) if debug else None
        if debug:
            dbg_wdh = dscr("dbg_wdh", [2, 128, 8 * NEXP], F32)
            dbg_hact = dscr("dbg_hact", [2, 2, 128, 4 * 512], BF16)
            dbg_h2h = dscr("dbg_h2h", [2, 128, NKC * 1024], BF16)
            dbg_y0 = dscr("dbg_y0", [2, 128, 8 * D], F32)
            dbg_pad = dscr("dbg_pad", [2, 128, 1024], F32)

        def bcast_load(ph, name, src_row):
            t = sb(ph, name, [128, src_row.shape[-1]], F32)
            P.dma('sp', t[:], src_row.partition_broadcast(128).rearrange("p o f -> p (o f)"), writes=[name])
            return t

        if upto >= 4:
            with contextlib.ExitStack() as ph:
                oaT = sb(ph, "oaT", [128, 8, OWN], BF16)
                obT = sb(ph, "obT", [128, 8, OWN], BF16)
                P.dma('sp', oaT[:], oaTd.rearrange("c p t -> p c t"), writes=['oaT'])
                P.dma('act', obT[:], obTd.rearrange("c p t -> p c t"), writes=['obT'])
                was = [sb(ph, "was%d" % i, [128, 8, 128], F32) for i in range(2)]
                wbs = [sb(ph, "wbs%d" % i, [128, 8, 128], F32) for i in range(2)]
                wab = [sb(ph, "wab%d" % i, [128, 8, 128], BF16) for i in range(2)]
                wbb = [sb(ph, "wbb%d" % i, [128, 8, 128], BF16) for i in range(2)]
                gas = [sb(ph, "gas%d" % i, [128, OWN], BF16) for i in range(2)]
                gbs = [sb(ph, "gbs%d" % i, [128, OWN], BF16) for i in range(2)]
                t1 = [sb(ph, "t1%d" % i, [128, 512], F32) for i in range(2)]
                t2 = [sb(ph, "t2%d" % i, [128, 512], F32) for i in range(2)]
                mst = [sb(ph, "mst%d" % i, [128, OWN], BF16) for i in range(2)]
                nn = 0
                for nb in range(16):
                    p2 = nb % 2
                    P.dma('sp', was[p2][:], w_pa[:, nb * 128:(nb + 1) * 128].rearrange("(c p) n -> p c n", p=128), writes=['was%d' % p2])
                    P.dma('sp', wbs[p2][:], w_pb[:, nb * 128:(nb + 1) * 128].rearrange("(c p) n -> p c n", p=128), writes=['wbs%d' % p2])
                    P.op('pool', lambda g, p2=p2: g.tensor_copy(out=wab[p2][:], in_=was[p2][:]), reads=['was%d' % p2], writes=['wab%d' % p2])
                    P.op('pool', lambda g, p2=p2: g.tensor_copy(out=wbb[p2][:], in_=wbs[p2][:]), reads=['wbs%d' % p2], writes=['wbb%d' % p2])
                    P.dma('act', gas[p2][:], gaTd[nb], writes=['gas%d' % p2])
                    P.dma('act', gbs[p2][:], gbTd[nb], writes=['gbs%d' % p2])
                    for oc in range(4):
                        q2 = nn % 2
                        nn += 1
                        ba = 0 + q2
                        bb = 2 + q2
                        for kc in range(8):
                            P.op('pe', lambda pe, kc=kc, p2=p2, oc=oc, ba=ba: pe.matmul(
                                banks[ba][:, :], lhsT=wab[p2][:, kc, :], rhs=oaT[:, kc, oc * 512:(oc + 1) * 512],
                                start=(kc == 0), stop=(kc == 7)), reads=['wab%d' % p2, 'oaT'], writes=['b%d' % ba], excl=B(ba))
                        for kc in range(8):
                            P.op('pe', lambda pe, kc=kc, p2=p2, oc=oc, bb=bb: pe.matmul(
                                banks[bb][:, :], lhsT=wbb[p2][:, kc, :], rhs=obT[:, kc, oc * 512:(oc + 1) * 512],
                                start=(kc == 0), stop=(kc == 7)), reads=['wbb%d' % p2, 'obT'], writes=['b%d' % bb], excl=B(bb))
                        P.op('dve', lambda v, p2=p2, oc=oc, ba=ba, q2=q2: v.tensor_tensor(
                            out=t1[q2][:], in0=banks[ba][:, :], in1=gas[p2][:, oc * 512:(oc + 1) * 512], op=ALU.mult),
                            reads=['b%d' % ba, 'gas%d' % p2], writes=['t1%d' % q2], excl=B(ba))
                        P.op('dve', lambda v, p2=p2, oc=oc, bb=bb, q2=q2: v.tensor_tensor(
                            out=t2[q2][:], in0=banks[bb][:, :], in1=gbs[p2][:, oc * 512:(oc + 1) * 512], op=ALU.mult),
                            reads=['b%d' % bb, 'gbs%d' % p2], writes=['t2%d' % q2], excl=B(bb))
                        P.op('pool', lambda g, p2=p2, oc=oc, q2=q2: g.tensor_tensor(
                            out=mst[p2][:, oc * 512:(oc + 1) * 512], in0=t1[q2][:], in1=t2[q2][:], op=ALU.add),
                            reads=['t1%d' % q2, 't2%d' % q2], writes=[('mst%d' % p2, oc)])
                    P.dma('sp', mTd[nb], mst[p2][:], reads=[('mst%d' % p2, oc) for oc in range(4)], writes=[('mTd', nb)])
                P.barrier()
                P.flush()

        if upto >= 5:
            with contextlib.ExitStack() as ph:
                Wo = sb(ph, "Wo", [128, NKC, D], BF16)
                wst = [sb(ph, "wsto%d" % i, [128, 2048], F32) for i in range(2)]
                for kc in range(NKC):
                    w = wst[kc % 2]
                    wk = 'wsto%d' % (kc % 2)
                    P.dma('sp', w[:], w_out[kc * 128:(kc + 1) * 128, :], writes=[wk])
                    P.op('pool', lambda g, w=w, kc=kc: g.tensor_copy(out=Wo[:, kc, :], in_=w[:]), reads=[wk], writes=[('Wo', kc)])
                G1 = bcast_load(ph, "G1", modv[0:1, 2 * D:3 * D])
                L1g = bcast_load(ph, "L1g", ln1_g)
                L1b = bcast_load(ph, "L1b", ln1_b)
                brt = bcast_load(ph, "brt", b_rt)
                Wrt = sb(ph, "Wrt", [128, NKC, 36], F32)
                P.dma('sp', Wrt[:], w_rt.rearrange("(c p) n -> p c n", p=128), writes=['Wrt'])
                lb = ln_bufs(ph)
                xin, xn, st, mv, sd, cnt = lb
                mts = [sb(ph, "mts%d" % i, [128, NKC, 128], BF16) for i in range(2)]
                tg = sb(ph, "tg", [128, D], F32)
                rt = sb(ph, "rt", [128, D], F32)
                x1t = sb(ph, "x1t", [128, D], F32)
                h2t = [sb(ph, "h2t%d" % i, [128, NKC, 128], BF16) for i in range(2)]
                h2f = sb(ph, "h2f", [128, NKC, 128], F32)
                lg = sb(ph, "lg", [128, 36], F32)
                rs = sb(ph, "rs", [128, 64], F32)
                wdt = [sb(ph, "wdt%d" % i, [128, 32], F32) for i in range(2)]
                for t in range(16):
                    p2 = t % 2
                    mt = mts[p2]
                    P.dma('sp', mt[:], mTd[:, :, t * 128:(t + 1) * 128].rearrange("c p t -> p c t"), writes=['mts%d' % p2])
                    xt = xin[t % 2]
                    xk = 'xin%d' % (t % 2)
                    P.dma('act', xt[:], xo[t * 128:(t + 1) * 128, :], writes=[xk])
                    for fb in range(4):
                        for kc in range(NKC):
                            P.op('pe', lambda pe, kc=kc, fb=fb, mt=mt: pe.matmul(
                                banks[fb][:, :], lhsT=mt[:, kc, :], rhs=Wo[:, kc, fb * 512:(fb + 1) * 512],
                                start=(kc == 0), stop=(kc == NKC - 1)),
                                reads=['mts%d' % p2, ('Wo', kc)], writes=['b%d' % fb], excl=B(fb))
                        P.op('dve', lambda v, fb=fb: v.tensor_tensor(
                            out=tg[:, fb * 512:(fb + 1) * 512], in0=banks[fb][:, :], in1=G1[:, fb * 512:(fb + 1) * 512], op=ALU.mult),
                            reads=['b%d' % fb, 'G1'], writes=[('tg', fb)], excl=B(fb))
                    P.op('dve', lambda v, xt=xt: v.scalar_tensor_tensor(out=rt[:], in0=xt[:], scalar=ALPHA, in1=tg[:],
                                                                       op0=ALU.mult, op1=ALU.add),
                         reads=[xk] + [('tg', fb) for fb in range(4)], writes=['rt'])
                    layer_norm_tile(rt, 'rt', xn, 'xn', st, mv, sd, eps_t)
                    P.op('dve', lambda v: v.tensor_tensor(out=xn[:], in0=xn[:], in1=L1g[:], op=ALU.mult), reads=['xn', 'L1g'], writes=['xn'])
                    P.op('pool', lambda g: g.tensor_tensor(out=x1t[:], in0=xn[:], in1=L1b[:], op=ALU.add), reads=['xn', 'L1b'], writes=['x1t'])
                    P.dma('sp', x1d[t * 128:(t + 1) * 128, :], x1t[:], reads=['x1t'], writes=[('x1d', t)])
                    h2 = h2t[p2]
                    ln_T_tile(lb, None, h2, 'h2t%d' % p2, 0, 3, fdst=h2f, bo=4, xt_in=x1t, xk_in='x1t')
                    P.dma('sp', h2Td[:, :, t * 128:(t + 1) * 128].rearrange("c p t -> p c t"), h2[:],
                          reads=[('h2t%d' % p2, kc, 0) for kc in range(NKC)], writes=[('h2Td', t)])
                    for kc in range(NKC):
                        P.op('pe', lambda pe, kc=kc: pe.matmul(banks[0][:, 0:36], lhsT=h2f[:, kc, :], rhs=Wrt[:, kc, :],
                                                                start=(kc == 0), stop=(kc == NKC - 1)),
                             reads=[('fdst', kc), 'Wrt'], writes=['b0'], excl=B(0))
                    P.op('dve', lambda v: v.tensor_tensor(out=lg[:], in0=banks[0][:, 0:36], in1=brt[:], op=ALU.add),
                         reads=['b0', 'brt'], writes=['lg'], excl=B(0))
                    wd_ = wdt[p2]
                    V = lambda f, r, w: P.op('dve', f, reads=r, writes=w)
                    V(lambda v: v.reduce_max(out=rs[:, 0:1], in_=lg[:, 0:4], axis=mybir.AxisListType.X), ['lg'], ['r0'])
                    V(lambda v: v.tensor_scalar(out=rs[:, 1:2], in0=rs[:, 0:1], scalar1=-1.0, scalar2=None, op0=ALU.mult), ['r0'], ['r1'])
                    P.op('act', lambda a: a.activation(out=rs[:, 4:8], in_=lg[:, 0:4], func=AF.Exp, bias=rs[:, 1:2], scale=1.0,
                                                       accum_out=rs[:, 2:3]), reads=['lg', 'r1'], writes=['r2'])
                    V(lambda v: v.reciprocal(out=rs[:, 3:4], in_=rs[:, 2:3]), ['r2'], ['r3'])
                    V(lambda v: v.tensor_scalar(out=rs[:, 8:12], in0=lg[:, 0:4], scalar1=rs[:, 0:1], scalar2=None, op0=ALU.is_equal),
                      ['lg', 'r0'], ['gm'])
                    V(lambda v: v.tensor_scalar(out=rs[:, 12:20], in0=lg[:, 4:12], scalar1=rs[:, 8:9], scalar2=None, op0=ALU.mult),
                      ['lg', 'gm'], ['sel'])
                    for g in range(1, 4):
                        V(lambda v, g=g: v.scalar_tensor_tensor(out=rs[:, 12:20], in0=lg[:, 4 + 8 * g:12 + 8 * g], scalar=rs[:, 8 + g:9 + g],
                                                                in1=rs[:, 12:20], op0=ALU.mult, op1=ALU.add), ['lg', 'gm', 'sel'], ['sel'])
                    V(lambda v: v.max(out=rs[:, 20:28], in_=rs[:, 12:20]), ['sel'], ['top8'])
                    V(lambda v: v.tensor_scalar(out=rs[:, 28:36], in0=rs[:, 12:20], scalar1=rs[:, 20:21], scalar2=None, op0=ALU.is_equal),
                      ['sel', 'top8'], ['m1'])
                    V(lambda v: v.tensor_scalar(out=rs[:, 36:44], in0=rs[:, 12:20], scalar1=rs[:, 21:22], scalar2=None, op0=ALU.is_equal),
                      ['sel', 'top8'], ['m2'])
                    V(lambda v: v.tensor_tensor(out=rs[:, 44:45], in0=rs[:, 21:22], in1=rs[:, 20:21], op=ALU.subtract), ['top8'], ['rd'])
                    P.op('act', lambda a: a.activation(out=rs[:, 45:46], in_=rs[:, 44:45], func=AF.Exp), reads=['rd'], writes=['red'])
                    V(lambda v: v.tensor_scalar(out=rs[:, 46:47], in0=rs[:, 45:46], scalar1=1.0, scalar2=None, op0=ALU.add), ['red'], ['rden'])
                    V(lambda v: v.reciprocal(out=rs[:, 47:48], in_=rs[:, 46:47]), ['rden'], ['rw1'])
                    V(lambda v: v.tensor_tensor(out=rs[:, 48:49], in0=rs[:, 45:46], in1=rs[:, 47:48], op=ALU.mult), ['red', 'rw1'], ['rw2'])
                    V(lambda v: v.tensor_tensor(out=rs[:, 49:50], in0=rs[:, 47:48], in1=rs[:, 3:4], op=ALU.mult), ['rw1', 'r3'], ['rw1p'])
                    V(lambda v: v.tensor_tensor(out=rs[:, 50:51], in0=rs[:, 48:49], in1=rs[:, 3:4], op=ALU.mult), ['rw2', 'r3'], ['rw2p'])
                    V(lambda v: v.tensor_scalar(out=rs[:, 52:60], in0=rs[:, 28:36], scalar1=rs[:, 49:50], scalar2=None, op0=ALU.mult),
                      ['m1', 'rw1p'], ['cw'])
                    V(lambda v: v.scalar_tensor_tensor(out=rs[:, 52:60], in0=rs[:, 36:44], scalar=rs[:, 50:51], in1=rs[:, 52:60],
                                                       op0=ALU.mult, op1=ALU.add), ['m2', 'rw2p', 'cw'], ['cw'])
                    for g in range(4):
                        V(lambda v, g=g, wd_=wd_: v.tensor_scalar(out=wd_[:, g * 8:(g + 1) * 8], in0=rs[:, 52:60], scalar1=rs[:, 8 + g:9 + g],
                                                                  scalar2=None, op0=ALU.mult), ['cw', 'gm'], [('wdt%d' % p2, g)])
                    P.dma('sp', wdd[t * 128:(t + 1) * 128, :], wd_[:], reads=[('wdt%d' % p2, g) for g in range(4)], writes=[('wdd', t)])
                P.barrier()
                P.flush()

        if upto >= 6:
            for half in range(2):
                with contextlib.ExitStack() as ph:
                    y2 = sb(ph, "y2", [128, 8, D], F32)
                    wdh = sb(ph, "wdh", [128, 8, NEXP], F32)
                    h2h = sb(ph, "h2h", [128, NKC, 1024], BF16)
                    P.dma('sp', h2h[:], h2Td[:, :, half * 1024:(half + 1) * 1024].rearrange("c p t -> p c t"), writes=['h2h'])
                    P.dma('sp', wdh[:], wdd[half * 1024:(half + 1) * 1024, :].rearrange("(c p) e -> p c e", p=128), writes=['wdh'])
                    for ti in range(8):
                        P.op('pool', lambda g, ti=ti: g.memset(y2[:, ti, :], 0.0), writes=[('y2', ti, fb) for fb in range(4)])
                    if debug:
                        P.dma('sp', dbg_y0[half], y2[:].rearrange("p a b -> p (a b)"),
                              reads=[('y2', ti, fb) for ti in range(8) for fb in range(4)], writes=['dbg_y0'])
                        P.dma('sp', dbg_wdh[half], wdh[:].rearrange("p a b -> p (a b)"), reads=['wdh'], writes=['dbg_wdh'])
                        P.dma('sp', dbg_h2h[half], h2h[:].rearrange("p a b -> p (a b)"), reads=['h2h'], writes=['dbg_h2h'])
                    with contextlib.ExitStack() as ph2:
                        hact = [sb(ph2, "hact%d" % i, [128, 4, 512], BF16) for i in range(2)]
                        sil = [sb(ph2, "sil%d" % i, [128, 512], F32) for i in range(2)]
                        wg = sb(ph2, "wg", [128, NKC, DEXP], BF16)
                        wu = sb(ph2, "wu", [128, NKC, DEXP], BF16)
                        wdn = sb(ph2, "wdn", [128, 4, D], BF16)
                        NSTG = 3
                        stg = [sb(ph2, "stg%d" % i, [128, 1024], F32) for i in range(NSTG)]
                        pad = sb(ph2, "pad", [128, 1024], F32)
                        P.op('pool', lambda g: g.memset(pad[:], 0.0), writes=['pad'])
                        ns = 0
                        nsl = 0
                        nh = 0
                        nbk = 0
                        for e in range(NEXP):
                            for (src, dstw, dk_, nhalf, ceng) in ((w_gate[e], wg, 'wg', 2, 'pool'), (w_up[e], wu, 'wu', 2, 'act'),
                                                                  (w_down[e], wdn, 'wdn', 2, 'pool')):
                                for hf in range(8):
                                    sg = stg[ns % NSTG]
                                    sk = 'stg%d' % (ns % NSTG)
                                    ns += 1
                                    if dk_ == 'wdn':
                                        dcw, hh = hf // 2, hf % 2
                                        P.dma('sp', sg[:], src[dcw * 128:(dcw + 1) * 128, hh * 1024:(hh + 1) * 1024], writes=[sk])
                                        dst_ap = dstw[:, dcw, hh * 1024:(hh + 1) * 1024]
                                    else:
                                        P.dma('sp', sg[:].rearrange("p (c n) -> p c n", c=2),
                                              src[hf * 256:(hf + 1) * 256, :].rearrange("(c p) n -> p c n", p=128), writes=[sk])
                                        dst_ap = dstw[:, hf * 2:(hf + 1) * 2, :].rearrange("p c n -> p (c n)")
                                    if ceng == 'pool':
                                        P.op('pool', lambda g, sg=sg, dst_ap=dst_ap: g.tensor_copy(out=dst_ap, in_=sg[:]),
                                             reads=[sk], writes=[(dk_, hf)])
                                    else:
                                        P.op('act', lambda a, sg=sg, dst_ap=dst_ap: a.copy(out=dst_ap, in_=sg[:]),
                                             reads=[sk], writes=[(dk_, hf)])
                            for ch in range(2):
                                ha = hact[nh % 2]
                                hk = 'hact%d' % (nh % 2)
                                nh += 1
                                for dc in range(4):
                                    bg = nbk % 2
                                    bu = 2 + nbk % 2
                                    nbk += 1
                                    for kc in range(NKC):
                                        P.op('pe', lambda pe, kc=kc, dc=dc, ch=ch, bg=bg: pe.matmul(
                                            banks[bg][:, :], lhsT=wg[:, kc, dc * 128:(dc + 1) * 128], rhs=h2h[:, kc, ch * 512:(ch + 1) * 512],
                                            start=(kc == 0), stop=(kc == NKC - 1)), reads=[('wg', kc // 2), 'h2h'], writes=['b%d' % bg], excl=B(bg))
                                    for kc in range(NKC):
                                        P.op('pe', lambda pe, kc=kc, dc=dc, ch=ch, bu=bu: pe.matmul(
                                            banks[bu][:, :], lhsT=wu[:, kc, dc * 128:(dc + 1) * 128], rhs=h2h[:, kc, ch * 512:(ch + 1) * 512],
                                            start=(kc == 0), stop=(kc == NKC - 1)), reads=[('wu', kc // 2), 'h2h'], writes=['b%d' % bu], excl=B(bu))
                                    sl = sil[nsl % 2]
                                    slk = 'sil%d' % (nsl % 2)
                                    nsl += 1
                                    P.op('act', lambda a, sl=sl, bg=bg: a.activation(out=sl[:], in_=banks[bg][:, :], func=AF.Silu),
                                         reads=['b%d' % bg], writes=[slk], excl=B(bg))
                                    P.op('dve', lambda v, sl=sl, bu=bu, ha=ha, dc=dc: v.tensor_tensor(
                                        out=ha[:, dc, :], in0=banks[bu][:, :], in1=sl[:], op=ALU.mult),
                                        reads=['b%d' % bu, slk], writes=[(hk, dc)], excl=B(bu))
                                for tl in range(4):
                                    ti = ch * 4 + tl
                                    for fb in range(4):
                                        bd = 4 + fb
                                        for dc in range(4):
                                            P.op('pe', lambda pe, dc=dc, tl=tl, fb=fb, bd=bd, ha=ha: pe.matmul(
                                                banks[bd][:, :], lhsT=ha[:, dc, tl * 128:(tl + 1) * 128], rhs=wdn[:, dc, fb * 512:(fb + 1) * 512],
                                                start=(dc == 0), stop=(dc == 3)), reads=[(hk, dc), ('wdn', dc * 2 + fb // 2)], writes=['b%d' % bd], excl=B(bd))
                                        P.op('dve', lambda v, ti=ti, fb=fb, bd=bd, e=e: v.scalar_tensor_tensor(
                                            out=y2[:, ti, fb * 512:(fb + 1) * 512], in0=banks[bd][:, :], scalar=wdh[:, ti, e:e + 1],
                                            in1=y2[:, ti, fb * 512:(fb + 1) * 512], op0=ALU.mult, op1=ALU.add),
                                            reads=['b%d' % bd, 'wdh', ('y2', ti, fb)], writes=[('y2', ti, fb)], excl=B(bd))
                        if debug:
                            P.dma('sp', dbg_pad[half], pad[:], reads=['pad'], writes=['dbg_pad'])
                            for q in range(2):
                                P.dma('sp', dbg_hact[half, q], hact[q][:].rearrange("p a b -> p (a b)"),
                                      reads=[('hact%d' % q, dc) for dc in range(4)], writes=[('dbg_hact', q)])
                    P.barrier()
                    if debug:
                        for ti in range(8):
                            t = half * 8 + ti
                            P.dma('sp', y2d[t * 128:(t + 1) * 128, :], y2[:, ti, :],
                                  reads=[('y2', ti, fb) for fb in range(4)], writes=[('y2d', t)])
                    with contextlib.ExitStack() as ph3:
                        G2 = bcast_load(ph3, "G2", modv[0:1, 5 * D:6 * D])
                        L2g = bcast_load(ph3, "L2g", ln2_g)
                        L2b = bcast_load(ph3, "L2b", ln2_b)
                        lb = ln_bufs(ph3)
                        xin, xn, st, mv, sd, cnt = lb
                        tg2 = sb(ph3, "tg2", [128, D], F32)
                        rt2 = sb(ph3, "rt2", [128, D], F32)
                        ot = [sb(ph3, "ot%d" % i, [128, D], F32) for i in range(2)]
                        for ti in range(8):
                            t = half * 8 + ti
                            xt = xin[ti % 2]
                            xk = 'xin%d' % (ti % 2)
                            P.dma('act', xt[:], x1d[t * 128:(t + 1) * 128, :], writes=[xk])
                            P.op('pool', lambda g, ti=ti: g.tensor_tensor(out=tg2[:], in0=y2[:, ti, :], in1=G2[:], op=ALU.mult),
                                 reads=[('y2', ti, fb) for fb in range(4)] + ['G2'], writes=['tg2'])
                            P.op('dve', lambda v, xt=xt: v.scalar_tensor_tensor(out=rt2[:], in0=xt[:], scalar=ALPHA, in1=tg2[:],
                                                                               op0=ALU.mult, op1=ALU.add), reads=[xk, 'tg2'], writes=['rt2'])
                            layer_norm_tile(rt2, 'rt2', xn, 'xn', st, mv, sd, eps_t)
                            o = ot[ti % 2]
                            ok_ = 'ot%d' % (ti % 2)
                            P.op('dve', lambda v: v.tensor_tensor(out=xn[:], in0=xn[:], in1=L2g[:], op=ALU.mult), reads=['xn', 'L2g'], writes=['xn'])
                            P.op('pool', lambda g, o=o: g.tensor_tensor(out=o[:], in0=xn[:], in1=L2b[:], op=ALU.add), reads=['xn', 'L2b'], writes=[ok_])
                            P.dma('sp', out[t * 128:(t + 1) * 128, :], o[:], reads=[ok_], writes=[('out', t)])
                    P.barrier()
                    P.flush()
        P.barrier()
        P.flush()
    return nc


def make_in_maps(inputs):
    x = np.asarray(inputs["x"], dtype=np.float32)
    c = np.asarray(inputs["c"], dtype=np.float32)
    g = lambda k: np.ascontiguousarray(np.asarray(inputs[k], dtype=np.float32)[0])
    w_rt = np.ascontiguousarray(np.concatenate([g("w_group"), g("w_router")], axis=1))
    b_rt = np.ascontiguousarray(np.concatenate([g("b_group"), g("b_router")], axis=0)[None, :])
    sgu_wT = np.ascontiguousarray(g("sgu_w").transpose(2, 0, 1))
    shared = {
        "w_ada": g("w_ada"), "b_ada": g("b_ada")[None, :], "w_in": g("w_in"),
        "sgu_wT": sgu_wT, "sgu_b": g("sgu_b").reshape(1, -1),
        "sgu_ln_g": g("sgu_ln_g")[None, :], "sgu_ln_b": g("sgu_ln_b")[None, :],
        "w_proj_a": g("w_proj_a"), "w_proj_b": g("w_proj_b"), "w_out": g("w_out"),
        "ln1_g": g("ln1_g")[None, :], "ln1_b": g("ln1_b")[None, :],
        "w_rt": w_rt, "b_rt": b_rt,
        "w_gate": g("w_gate"), "w_up": g("w_up"), "w_down": g("w_down"),
        "ln2_g": g("ln2_g")[None, :], "ln2_b": g("ln2_b")[None, :],
    }
    maps = []
    for core in range(8):
        b = core // 4
        m = dict(shared)
        j = core % 4
        m["xb"] = np.ascontiguousarray(x[b])
        m["xo"] = np.ascontiguousarray(x[b].reshape(16, 4, 128, D)[:, j].reshape(OWN, D))
        mk = np.zeros((128, 4, 128), np.float32)
        for mm in range(4):
            if mm < j:
                mk[:, mm, :] = 1.0
            elif mm == j:
                mk[:, mm, :] = np.triu(np.ones((128, 128), np.float32), k=1)
        m["maskd"] = mk
        m["cT"] = np.ascontiguousarray(c[b].reshape(NKC, 128).T)
        maps.append(m)
    return maps


_NC_CACHE = {}


def kernel(**inputs):
    if "nc" not in _NC_CACHE:
        _NC_CACHE["nc"] = build()
    nc = _NC_CACHE["nc"]
    maps = make_in_maps(inputs)
    res = run_bass_kernel_spmd(nc, maps, core_ids=list(range(8)))
    full = np.zeros((2, S, D), np.float32)
    for core in range(8):
        b, j = core // 4, core % 4
        o = np.asarray(res.results[core]["out"], dtype=np.float32).reshape(16, 128, D)
        full[b].reshape(16, 4, 128, D)[:, j] = o
    return full
```

```python
import contextlib
import os
import numpy as np
import ml_dtypes
import concourse.bass as bass
import concourse.mybir as mybir
from concourse.bass_utils import run_bass_kernel_spmd

F32 = mybir.dt.float32
BF16 = mybir.dt.bfloat16
U32 = mybir.dt.uint32
AF = mybir.ActivationFunctionType
ALU = mybir.AluOpType

D = 2048
S = 8192
NKC = 16
OWN = 2048
LN_EPS = 1e-5
ALPHA = 2.0 ** 0.25
NEXP = 32
DEXP = 512


class Prog:
    LIMIT = 30000
    NDMA = 6

    def __init__(self, nc, stack):
        self.nc = nc
        self.stack = stack
        self.names = ['pe', 'act', 'dve', 'pool', 'sp']
        self.sems = []
        self.ops = {e: [] for e in self.names}
        self.cur = {}
        self.cnt = {}
        for e in self.names:
            self.cur[e] = self._new_sem("c_" + e)
            self.cnt[e] = 0
        self.waited = {e: {} for e in self.names}
        self.lastw = {}
        self.readers = {}
        self.latest = {}
        self.dq = {}
        self.dn = {}
        for q in ['sp', 'act', 'pool']:
            self.dq[q] = [self._new_sem("d_%s%d" % (q, i)) for i in range(self.NDMA)]
            self.dn[q] = 0
        self.n_instr = 0

    def _new_sem(self, name):
        h = self.stack.enter_context(self.nc.semaphore(name + "_%d" % len(self.sems)))
        self.sems.append(h)
        return len(self.sems) - 1

    def _deps(self, e, reads, writes, excl=()):
        deps = []
        for k in excl:
            t = self.lastw.get(k)
            if t is not None and t[0] != self.cur.get(e, -1):
                deps.append(t)
        for k in reads:
            t = self.lastw.get(k)
            if t is not None:
                deps.append(t)
        for k in writes:
            t = self.lastw.get(k)
            if t is not None:
                deps.append(t)
            deps.extend(self.readers.get(k, ()))
        waits = {}
        for (s, v) in deps:
            if e == 'pe' and s == self.cur['pe']:
                continue
            if self.waited[e].get(s, 0) >= v:
                continue
            if waits.get(s, 0) < v:
                waits[s] = v
        for s, v in waits.items():
            self.waited[e][s] = v
        return list(waits.items())

    def _commit(self, tok, reads, writes):
        for k in writes:
            self.lastw[k] = tok
            self.readers[k] = []
        for k in reads:
            if k in writes:
                continue
            self.readers.setdefault(k, []).append(tok)
        self.latest[tok[0]] = tok[1]

    def op(self, e, fn, reads=(), writes=(), excl=()):
        waits = self._deps(e, reads, writes, excl)
        if self.cnt[e] >= self.LIMIT:
            self.cur[e] = self._new_sem("c_" + e)
            self.cnt[e] = 0
        self.cnt[e] += 1
        tok = (self.cur[e], self.cnt[e])
        sems = self.sems

        def emit(eng, waits=waits, fn=fn, tok=tok):
            for (s, v) in waits:
                eng.wait_ge(sems[s], v)
            fn(eng).then_inc(sems[tok[0]], 1)
        self.ops[e].append(emit)
        self._commit(tok, reads, writes)
        for k in excl:
            self.lastw[k] = tok
        self.n_instr += 1
        return tok

    def dma(self, q, out, in_, reads=(), writes=(), **kw):
        waits = self._deps(q, reads, writes)
        i = self.dn[q]
        self.dn[q] += 1
        k = i % self.NDMA
        val = 16 * (i // self.NDMA + 1)
        s = self.dq[q][k]
        if i >= self.NDMA and self.waited[q].get(s, 0) < val - 16:
            waits.append((s, val - 16))
            self.waited[q][s] = val - 16
        tok = (s, val)
        sems = self.sems

        def emit(eng, waits=waits, tok=tok, out=out, in_=in_, kw=kw):
            for (ss, v) in waits:
                eng.wait_ge(sems[ss], v)
            eng.dma_start(out=out, in_=in_, **kw).then_inc(sems[tok[0]], 16)
        self.ops[q].append(emit)
        self._commit(tok, reads, writes)
        self.n_instr += 1
        return tok

    def barrier(self):
        for e in self.names:
            waits = []
            for s, v in self.latest.items():
                if self.waited[e].get(s, 0) < v:
                    waits.append((s, v))
                    self.waited[e][s] = v
            sems = self.sems

            def emit(eng, waits=waits):
                for (s, v) in waits:
                    eng.wait_ge(sems[s], v)
            self.ops[e].append(emit)
        self.lastw.clear()
        self.readers.clear()

    def flush(self):
        nc = self.nc
        ops = self.ops
        with nc.Block() as block:
            @block.tensor
            def _(eng):
                for f in ops['pe']:
                    f(eng)

            @block.scalar
            def _(eng):
                for f in ops['act']:
                    f(eng)

            @block.vector
            def _(eng):
                for f in ops['dve']:
                    f(eng)

            @block.gpsimd
            def _(eng):
                for f in ops['pool']:
                    f(eng)

            @block.sync
            def _(eng):
                for f in ops['sp']:
                    f(eng)
        self.ops = {e: [] for e in self.names}


def build(upto=99, debug=False, lite=False):
    nc = bass.Bass("TRN2", target_bir_lowering=False)
    dk = "ExternalOutput"

    def din(name, shape, dt=F32):
        return nc.dram_tensor(name, list(shape), dt, kind="ExternalInput").ap()

    xb = din("xb", [S, D])
    xo = din("xo", [OWN, D])
    maskd = din("maskd", [128, 4, 128])
    cT = din("cT", [128, NKC])
    w_ada = din("w_ada", [D, 6 * D])
    b_ada = din("b_ada", [1, 6 * D])
    w_in = din("w_in", [D, 9216])
    sgu_wT = din("sgu_wT", [128, 8, 128])
    sgu_b = din("sgu_b", [1, 8 * 128])
    sgu_ln_g = din("sgu_ln_g", [1, 1024])
    sgu_ln_b = din("sgu_ln_b", [1, 1024])
    w_pa = din("w_proj_a", [1024, D])
    w_pb = din("w_proj_b", [1024, D])
    w_out = din("w_out", [D, D])
    ln1_g = din("ln1_g", [1, D])
    ln1_b = din("ln1_b", [1, D])
    w_rt = din("w_rt", [D, 36])
    b_rt = din("b_rt", [1, 36])
    if not lite:
        w_gate = din("w_gate", [NEXP, D, DEXP])
        w_up = din("w_up", [NEXP, D, DEXP])
        w_down = din("w_down", [NEXP, DEXP, D])
    ln2_g = din("ln2_g", [1, D])
    ln2_b = din("ln2_b", [1, D])
    out = nc.dram_tensor("out", [OWN, D], F32, kind="ExternalOutput").ap()

    def dscr(name, shape, dt):
        return nc.dram_tensor(name, list(shape), dt, kind=dk).ap()

    modv = dscr("modv", [1, 6 * D], F32)
    KT = dscr("KT", [8, 128, S], BF16)
    VV = dscr("VV", [64, 128, 1024], BF16)
    QTd = dscr("QTd", [8, 128, OWN], BF16)
    obTd = dscr("obTd", [8, 128, OWN], BF16)
    gaTd = dscr("gaTd", [NKC, 128, OWN], BF16)
    gbTd = dscr("gbTd", [NKC, 128, OWN], BF16)
    oaTd = dscr("oaTd", [8, 128, OWN], BF16)
    x1d = dscr("x1d", [OWN, D], F32)
    h2Td = dscr("h2Td", [NKC, 128, OWN], BF16)
    wdd = dscr("wdd", [OWN, NEXP], F32)

    with contextlib.ExitStack() as top:
        P = Prog(nc, top)
        banks = [top.enter_context(nc.psum_tensor("bank%d" % i, [128, 512], F32)) for i in range(8)]
        ident = top.enter_context(nc.sbuf_tensor("ident", [128, 128], F32))
        identb = top.enter_context(nc.sbuf_tensor("identb", [128, 128], BF16))
        ones_r = top.enter_context(nc.sbuf_tensor("ones_r", [1, 128], F32))

        uniq = [0]

        def sb(ph, name, shape, dt):
            uniq[0] += 1
            return ph.enter_context(nc.sbuf_tensor("%s_%d" % (name, uniq[0]), list(shape), dt))

        P.op('pool', lambda g: g.memset(ident[:], 1.0), writes=['ident'])
        P.op('pool', lambda g: g.affine_select(out=ident[:], in_=ident[:], pattern=[[-1, 128]],
                                               compare_op=ALU.is_equal, fill=0.0, base=0,
                                               channel_multiplier=1),
             reads=['ident'], writes=['ident'])
        P.op('pool', lambda g: g.tensor_copy(out=identb[:], in_=ident[:]), reads=['ident'], writes=['identb'])
        P.op('pool', lambda g: g.memset(ones_r[:], 1.0), writes=['ones_r'])

        if upto >= 0:
            with contextlib.ExitStack() as ph:
                cact = sb(ph, "cact", [128, NKC], F32)
                craw = sb(ph, "craw", [128, NKC], F32)
                bada = sb(ph, "bada", [1, 6 * D], F32)
                wst = [sb(ph, "wst%d" % i, [128, 2048], F32) for i in range(3)]
                mrow = sb(ph, "mrow", [1, 2048], F32)
                P.dma('sp', craw[:], cT, writes=['craw'])
                P.dma('sp', bada[:], b_ada, writes=['bada'])
                P.op('act', lambda a: a.activation(out=cact[:], in_=craw[:], func=AF.Silu),
                     reads=['craw'], writes=['cact'])
                n = 0
                for g in range(6):
                    for kc in range(NKC):
                        w = wst[n % 3]
                        wk = 'wst%d' % (n % 3)
                        n += 1
                        P.dma('sp', w[:], w_ada[kc * 128:(kc + 1) * 128, g * 2048:(g + 1) * 2048], writes=[wk])
                        for jj in range(4):
                            P.op('pe', lambda pe, w=w, jj=jj, kc=kc: pe.matmul(
                                banks[jj][0:1, :], lhsT=cact[:, kc:kc + 1], rhs=w[:, jj * 512:(jj + 1) * 512],
                                start=(kc == 0), stop=(kc == NKC - 1)),
                                reads=[wk, 'cact'], writes=['b%d' % jj])
                    for jj in range(4):
                        P.op('dve', lambda v, jj=jj, g=g: v.tensor_tensor(
                            out=mrow[0:1, jj * 512:(jj + 1) * 512], in0=banks[jj][0:1, :],
                            in1=bada[0:1, g * 2048 + jj * 512: g * 2048 + (jj + 1) * 512], op=ALU.add),
                            reads=['b%d' % jj, 'bada'], writes=['mrow%d' % jj])
                    P.dma('sp', modv[0:1, g * 2048:(g + 1) * 2048], mrow[:],
                          reads=['mrow%d' % jj for jj in range(4)], writes=['modv'])
                P.barrier()
                P.flush()

        mfm = top.enter_context(nc.sbuf_tensor("mfm", [128, 6, NKC], F32))
        mview = modv.rearrange("o (s c p) -> p (o s) c", p=128, c=NKC)
        for s6 in range(6):
            P.dma('sp', mfm[:, s6, :], mview[:, s6, :], reads=['modv'], writes=['mfm'],
                  allow_slow_non_contiguous=True)
        P.op('dve', lambda v: v.tensor_scalar(out=mfm[:, 1, :], in0=mfm[:, 1, :], scalar1=1.0, scalar2=None,
                                              op0=ALU.add), reads=['mfm'], writes=['mfm'])
        P.op('dve', lambda v: v.tensor_scalar(out=mfm[:, 4, :], in0=mfm[:, 4, :], scalar1=1.0, scalar2=None,
                                              op0=ALU.add), reads=['mfm'], writes=['mfm'])

        def layer_norm_tile(xt, xk, xn, xnk, st, mv, sd, eps_t):
            for q in range(4):
                P.op('dve', lambda v, q=q: v.bn_stats(out=st[:, q, :], in_=xt[:, q * 512:(q + 1) * 512]),
                     reads=[xk], writes=['st%d' % q])
            P.op('dve', lambda v: v.bn_aggr(out=mv[:], in_=st[:].rearrange("p a b -> p (a b)")),
                 reads=['st%d' % q for q in range(4)], writes=['mv'])
            P.op('act', lambda a: a.activation(out=sd[:, 0:1], in_=mv[:, 1:2], func=AF.Sqrt, bias=eps_t[:, 0:1],
                                               scale=1.0),
                 reads=['mv', 'eps'], writes=['sd0'])
            P.op('dve', lambda v: v.reciprocal(out=sd[:, 1:2], in_=sd[:, 0:1]), reads=['sd0'], writes=['sd1'])
            P.op('dve', lambda v: v.tensor_scalar(out=sd[:, 2:3], in0=mv[:, 0:1], scalar1=sd[:, 1:2], scalar2=-1.0,
                                                  op0=ALU.mult, op1=ALU.mult),
                 reads=['mv', 'sd1'], writes=['sd2'])
            P.op('act', lambda a: a.activation(out=xn[:], in_=xt[:], func=AF.Identity, bias=sd[:, 2:3],
                                               scale=sd[:, 1:2]),
                 reads=[xk, 'sd1', 'sd2'], writes=[xnk])

        eps_t = top.enter_context(nc.sbuf_tensor("eps_t", [128, 1], F32))
        P.op('pool', lambda g: g.memset(eps_t[:], LN_EPS), writes=['eps'])

        def B(bk):
            return ['B%d' % bk]

        def ln_T_tile(ph_bufs, src_rows, dst, dkey, col0, ms, fdst=None, bo=0, xt_in=None, xk_in=None):
            xin, xn, st, mv, sd, cnt = ph_bufs
            xt = xin[cnt[0] % 2]
            xk = 'xin%d' % (cnt[0] % 2)
            cnt[0] += 1
            if src_rows is not None:
                P.dma('act', xt[:], src_rows, writes=[xk])
            else:
                xt, xk = xt_in, xk_in
            layer_norm_tile(xt, xk, xn, 'xn', st, mv, sd, eps_t)
            for kc in range(NKC):
                bk = bo + kc // 4
                P.op('pe', lambda pe, kc=kc, bk=bk: pe.transpose(
                    banks[bk][:, (kc % 4) * 128:(kc % 4 + 1) * 128], xn[:, kc * 128:(kc + 1) * 128], ident[:]),
                    reads=['xn', 'ident'], writes=[('bq', bk, kc % 4)], excl=B(bk))
            for kc in range(NKC):
                bk = bo + kc // 4
                q = kc % 4
                if bk - bo < 2:
                    P.op('dve', lambda v, kc=kc, bk=bk, q=q: v.tensor_scalar(
                        out=dst[:, kc, col0:col0 + 128], in0=banks[bk][:, q * 128:(q + 1) * 128],
                        scalar1=mfm[:, ms + 1, kc:kc + 1], scalar2=mfm[:, ms, kc:kc + 1],
                        op0=ALU.mult, op1=ALU.add),
                        reads=[('bq', bk, q), 'mfm'], writes=[(dkey, kc, col0)], excl=B(bk))
                else:
                    P.op('act', lambda a, kc=kc, bk=bk, q=q: a.activation(
                        out=dst[:, kc, col0:col0 + 128], in_=banks[bk][:, q * 128:(q + 1) * 128],
                        func=AF.Identity, scale=mfm[:, ms + 1, kc:kc + 1], bias=mfm[:, ms, kc:kc + 1]),
                        reads=[('bq', bk, q), 'mfm'], writes=[(dkey, kc, col0)], excl=B(bk))
                if fdst is not None:
                    P.op('dve', lambda v, kc=kc, bk=bk, q=q: v.tensor_scalar(
                        out=fdst[:, kc, :], in0=banks[bk][:, q * 128:(q + 1) * 128],
                        scalar1=mfm[:, ms + 1, kc:kc + 1], scalar2=mfm[:, ms, kc:kc + 1],
                        op0=ALU.mult, op1=ALU.add),
                        reads=[('bq', bk, q), 'mfm'], writes=[('fdst', kc)], excl=B(bk))

        def ln_bufs(ph):
            xin = [sb(ph, "xin%d" % i, [128, 2048], F32) for i in range(2)]
            xn = sb(ph, "xn", [128, 2048], F32)
            st = sb(ph, "st", [128, 4, 6], F32)
            mv = sb(ph, "mv", [128, 2], F32)
            sd = sb(ph, "sd", [128, 4], F32)
            return (xin, xn, st, mv, sd, [0])

        evc = [0]

        def evac_copy(dst_ap, bk, wkeys, src=None):
            src = banks[bk][:, :] if src is None else src
            evc[0] += 1
            if evc[0] % 2 == 0:
                P.op('dve', lambda v: v.tensor_copy(out=dst_ap, in_=src), reads=['b%d' % bk], writes=wkeys, excl=B(bk))
            else:
                P.op('act', lambda a: a.copy(out=dst_ap, in_=src), reads=['b%d' % bk], writes=wkeys, excl=B(bk))

        if upto >= 1:
            with contextlib.ExitStack() as ph:
                Wkv = sb(ph, "Wkv", [128, NKC, 2048], BF16)
                wst = [sb(ph, "wstb%d" % i, [128, 2048], F32) for i in range(2)]
                lb = ln_bufs(ph)
                hT = [sb(ph, "hT%d" % i, [128, NKC, 512], BF16) for i in range(2)]
                kst = [sb(ph, "kst%d" % i, [128, 8, 512], BF16) for i in range(2)]
                vst = [sb(ph, "vst%d" % i, [128, 1024], BF16) for i in range(2)]
                for kc in range(NKC):
                    w = wst[kc % 2]
                    wk = 'wstb%d' % (kc % 2)
                    P.dma('sp', w[:], w_in[kc * 128:(kc + 1) * 128, 1024:3072], writes=[wk])
                    P.op('pool', lambda g, w=w, kc=kc: g.tensor_copy(out=Wkv[:, kc, :], in_=w[:]),
                         reads=[wk], writes=[('Wkv', kc)])
                nk = 0
                nv = 0
                for gc in range(16):
                    hb = hT[gc % 2]
                    hk = 'hT%d' % (gc % 2)
                    for tt in range(4):
                        r0 = gc * 512 + tt * 128
                        ln_T_tile(lb, xb[r0:r0 + 128, :], hb, hk, tt * 128, 0)
                    ks = kst[gc % 2]
                    kk = 'kst%d' % (gc % 2)
                    for h in range(8):
                        bk = 4 + (nk % 4)
                        nk += 1
                        for kc in range(NKC):
                            P.op('pe', lambda pe, kc=kc, h=h, bk=bk, hb=hb: pe.matmul(
                                banks[bk][:, :], lhsT=Wkv[:, kc, h * 128:(h + 1) * 128], rhs=hb[:, kc, :],
                                start=(kc == 0), stop=(kc == NKC - 1)),
                                reads=[('Wkv', kc)] + [(hk, kc, t4 * 128) for t4 in range(4)],
                                writes=['b%d' % bk], excl=B(bk))
                        evac_copy(ks[:, h, :], bk, [(kk, h)])
                    P.dma('sp', KT[:, :, gc * 512:(gc + 1) * 512].rearrange("h p t -> p h t"), ks[:],
                          reads=[(kk, h) for h in range(8)], writes=[('KT', gc)])
                    for tt in range(4):
                        vs = vst[nv % 2]
                        vk = 'vst%d' % (nv % 2)
                        nv += 1
                        for half in range(2):
                            bk = 4 + (nk % 4)
                            nk += 1
                            for kc in range(NKC):
                                P.op('pe', lambda pe, kc=kc, half=half, bk=bk, hb=hb, tt=tt: pe.matmul(
                                    banks[bk][:, :], lhsT=hb[:, kc, tt * 128:(tt + 1) * 128],
                                    rhs=Wkv[:, kc, 1024 + half * 512:1024 + (half + 1) * 512],
                                    start=(kc == 0), stop=(kc == NKC - 1)),
                                    reads=[('Wkv', kc), (hk, kc, tt * 128)], writes=['b%d' % bk], excl=B(bk))
                            evac_copy(vs[:, half * 512:(half + 1) * 512], bk, [(vk, half)])
                        P.dma('sp', VV[gc * 4 + tt], vs[:], reads=[(vk, 0), (vk, 1)], writes=[('VV', gc * 4 + tt)])
                P.barrier()
                P.flush()

        if upto >= 2:
            with contextlib.ExitStack() as ph:
                hO = sb(ph, "hO", [128, NKC, OWN], BF16)
                lb = ln_bufs(ph)
                wsh = [sb(ph, "wsh%d" % i, [128, 8, 512], F32) for i in range(2)]
                wbf = [sb(ph, "wbf%d" % i, [128, NKC, 512], BF16) for i in range(2)]
                ost = [sb(ph, "ost%d" % i, [128, 4, 512], BF16) for i in range(2)]
                for t in range(16):
                    ln_T_tile(lb, xo[t * 128:(t + 1) * 128, :], hO, 'hO', t * 128, 0)
                hO_keys = lambda kc, c0, n: [('hO', kc, c0 + 128 * q) for q in range(n)]
                nld = [0]

                def load_wblock(src2d, c0, wb, wbk, nkc=NKC):
                    for half in range(nkc // 8):
                        w = wsh[nld[0] % 2]
                        wk = 'wsh%d' % (nld[0] % 2)
                        nld[0] += 1
                        P.dma('sp', w[:], src2d[half * 1024:(half + 1) * 1024, c0:c0 + 512].rearrange(
                            "(c p) n -> p c n", p=128), writes=[wk])
                        P.op('pool', lambda g, w=w, half=half, wb=wb: g.tensor_copy(
                            out=wb[:, half * 8:(half + 1) * 8, :], in_=w[:]),
                            reads=[wk], writes=[(wbk, half)])

                blocks = []
                for i in range(2):
                    blocks.append((i * 512, 'copy', QTd, i * 4))
                for i in range(2):
                    blocks.append((3072 + i * 512, 'gelu', obTd, i * 4))
                for i in range(4):
                    blocks.append((5120 + i * 512, 'sig', gaTd, i * 4))
                for i in range(4):
                    blocks.append((7168 + i * 512, 'sig', gbTd, i * 4))
                nb = 0
                no = 0
                nbank = 0
                for (c0, fn, dstd, ch0) in blocks:
                    wb = wbf[nb % 2]
                    wbk = 'wbf%d' % (nb % 2)
                    nb += 1
                    load_wblock(w_in, c0, wb, wbk)
                    for oc in range(4):
                        os_ = ost[no % 2]
                        ok = 'ost%d' % (no % 2)
                        no += 1
                        for sub in range(4):
                            bk = 4 + (nbank % 4)
                            nbank += 1
                            for kc in range(NKC):
                                P.op('pe', lambda pe, kc=kc, sub=sub, bk=bk, wb=wb, oc=oc: pe.matmul(
                                    banks[bk][:, :], lhsT=wb[:, kc, sub * 128:(sub + 1) * 128],
                                    rhs=hO[:, kc, oc * 512:(oc + 1) * 512],
                                    start=(kc == 0), stop=(kc == NKC - 1)),
                                    reads=[(wbk, kc // 8)] + hO_keys(kc, oc * 512, 4), writes=['b%d' % bk], excl=B(bk))
                            if fn == 'copy':
                                evac_copy(os_[:, sub, :], bk, [(ok, sub)])
                            else:
                                f = AF.Gelu if fn == 'gelu' else AF.Sigmoid
                                P.op('act', lambda a, sub=sub, bk=bk, os_=os_, f=f: a.activation(
                                    out=os_[:, sub, :], in_=banks[bk][:, :], func=f),
                                    reads=['b%d' % bk], writes=[(ok, sub)], excl=B(bk))
                        P.dma('sp', dstd[ch0:ch0 + 4, :, oc * 512:(oc + 1) * 512].rearrange("c p t -> p c t"), os_[:],
                              reads=[(ok, q) for q in range(4)], writes=[(dstd.tensor.name, ch0, oc)])
                load_wblock(w_in, 4096, wbf[0], 'wbf0')
                load_wblock(w_in, 4608, wbf[1], 'wbf1')
                wsf = sb(ph, "wsf", [128, 8, 128], F32)
                wsb = sb(ph, "wsb", [128, 8, 128], BF16)
                bsb = sb(ph, "bsb", [128, 1024], F32)
                lnG = sb(ph, "lnG", [128, 1024], F32)
                lnB = sb(ph, "lnB", [128, 1024], F32)
                gv = sb(ph, "gv", [128, 1024], F32)
                vn0 = sb(ph, "vn0", [128, 1024], F32)
                vnb = sb(ph, "vnb", [128, 1024], BF16)
                uT = [sb(ph, "uT%d" % i, [128, 8, 128], BF16) for i in range(2)]
                tmpm = sb(ph, "tmpm", [128, 1024], F32)
                obs = [sb(ph, "obs%d" % i, [128, 8, 128], BF16) for i in range(2)]
                st2 = sb(ph, "st2", [128, 2, 6], F32)
                mv2 = sb(ph, "mv2", [128, 2], F32)
                sd2 = sb(ph, "sd2", [128, 4], F32)
                P.dma('sp', wsf[:], sgu_wT, writes=['wsf'])
                P.op('pool', lambda g: g.memset(wsf[64:128, :, 0:64], 0.0), reads=['wsf'], writes=['wsf'])
                P.op('pool', lambda g: g.tensor_copy(out=wsb[:], in_=wsf[:]), reads=['wsf'], writes=['wsb'])
                P.dma('sp', bsb[:], sgu_b.partition_broadcast(128).rearrange("p o f -> p (o f)"), writes=['bsb'])
                P.dma('sp', lnG[:], sgu_ln_g.partition_broadcast(128).rearrange("p o f -> p (o f)"), writes=['lnG'])
                P.dma('sp', lnB[:], sgu_ln_b.partition_broadcast(128).rearrange("p o f -> p (o f)"), writes=['lnB'])
                for t in range(16):
                    u = uT[t % 2]
                    uk = 'uT%d' % (t % 2)
                    ob = obs[t % 2]
                    obk = 'obs%d' % (t % 2)
                    P.dma('act', u[:], obTd[:, :, t * 128:(t + 1) * 128].rearrange("c p t -> p c t"),
                          reads=[('obTd', 0, t // 4), ('obTd', 4, t // 4)], writes=[uk])
                    for half in range(2):
                        bk = half
                        for kc in range(NKC):
                            P.op('pe', lambda pe, kc=kc, half=half, bk=bk, t=t: pe.matmul(
                                banks[bk][:, :], lhsT=hO[:, kc, t * 128:(t + 1) * 128], rhs=wbf[half][:, kc, :],
                                start=(kc == 0), stop=(kc == NKC - 1)),
                                reads=[('wbf%d' % half, kc // 8), ('hO', kc, t * 128)], writes=['b%d' % bk], excl=B(bk))
                        P.op('act', lambda a, half=half, bk=bk: a.activation(
                            out=gv[:, half * 512:(half + 1) * 512], in_=banks[bk][:, :], func=AF.Gelu),
                            reads=['b%d' % bk], writes=[('gv', half)], excl=B(bk))
                        P.op('dve', lambda v, half=half: v.bn_stats(out=st2[:, half, :], in_=gv[:, half * 512:(half + 1) * 512]),
                             reads=[('gv', half)], writes=[('st2', half)])
                    P.op('dve', lambda v: v.bn_aggr(out=mv2[:], in_=st2[:].rearrange("p a b -> p (a b)")),
                         reads=[('st2', 0), ('st2', 1)], writes=['mv2'])
                    P.op('act', lambda a: a.activation(out=sd2[:, 0:1], in_=mv2[:, 1:2], func=AF.Sqrt, bias=eps_t[:, 0:1], scale=1.0),
                         reads=['mv2', 'eps'], writes=['sd20'])
                    P.op('dve', lambda v: v.reciprocal(out=sd2[:, 1:2], in_=sd2[:, 0:1]), reads=['sd20'], writes=['sd21'])
                    P.op('dve', lambda v: v.tensor_scalar(out=sd2[:, 2:3], in0=mv2[:, 0:1], scalar1=sd2[:, 1:2], scalar2=-1.0,
                                                          op0=ALU.mult, op1=ALU.mult), reads=['mv2', 'sd21'], writes=['sd22'])
                    P.op('act', lambda a: a.activation(out=vn0[:], in_=gv[:], func=AF.Identity, bias=sd2[:, 2:3], scale=sd2[:, 1:2]),
                         reads=[('gv', 0), ('gv', 1), 'sd21', 'sd22'], writes=['vn0'])
                    P.op('dve', lambda v: v.tensor_tensor(out=vn0[:], in0=vn0[:], in1=lnG[:], op=ALU.mult),
                         reads=['vn0', 'lnG'], writes=['vn0'])
                    P.op('dve', lambda v: v.tensor_tensor(out=vnb[:], in0=vn0[:], in1=lnB[:], op=ALU.add),
                         reads=['vn0', 'lnB'], writes=['vnb'])
                    for g in range(8):
                        bk = 2 + g // 4
                        P.op('pe', lambda pe, g=g, bk=bk: pe.matmul(
                            banks[bk][:, (g % 4) * 128:(g % 4 + 1) * 128], lhsT=vnb[:, g * 128:(g + 1) * 128],
                            rhs=wsb[:, g, :], start=True, stop=True),
                            reads=['vnb', 'wsb'], writes=[('bq', bk, g % 4)], excl=B(bk))
                    for hh in range(2):
                        bk = 2 + hh
                        P.op('dve', lambda v, hh=hh, bk=bk: v.tensor_tensor(
                            out=tmpm[:, hh * 512:(hh + 1) * 512], in0=banks[bk][:, :], in1=bsb[:, hh * 512:(hh + 1) * 512],
                            op=ALU.add), reads=[('bq', bk, q) for q in range(4)] + ['bsb'], writes=[('tmpm', hh)], excl=B(bk))
                    P.op('pool', lambda gp, u=u, ob=ob: gp.tensor_tensor(
                        out=ob[:].rearrange("p c t -> p (c t)"), in0=tmpm[:], in1=u[:].rearrange("p c t -> p (c t)"), op=ALU.mult),
                        reads=[('tmpm', 0), ('tmpm', 1), uk], writes=[obk])
                    P.dma('sp', obTd[:, :, t * 128:(t + 1) * 128].rearrange("c p t -> p c t"), ob[:],
                          reads=[obk], writes=[('obTd2', t)])
                P.barrier()
                P.flush()

        if upto >= 3:
            with contextlib.ExitStack() as ph:
                KTh = sb(ph, "KTh", [128, 4, S], BF16)
                Vh = sb(ph, "Vh", [128, 64, 512], BF16)
                QTh = sb(ph, "QTh", [128, 4, OWN], BF16)
                mkf = sb(ph, "mkf", [128, 4, 128], F32)
                mk4 = sb(ph, "mk4", [128, 4, 4, 128], BF16)
                triI = sb(ph, "triI", [128, 128], BF16)
                triC = sb(ph, "triC", [128, 128], BF16)
                trif = sb(ph, "trif", [128, 128], F32)
                NS_ = 2
                eb = [[sb(ph, "eb%d_%d" % (q, i), [128, 512], F32) for i in range(2)] for q in range(NS_)]
                spb = [[sb(ph, "spb%d_%d" % (q, i), [128, 512], BF16) for i in range(2)] for q in range(NS_)]
                gb_ = [[sb(ph, "gb%d_%d" % (q, i), [128, 512], F32) for i in range(2)] for q in range(NS_)]
                wb_ = [[sb(ph, "wb%d_%d" % (q, i), [128, 512], BF16) for i in range(2)] for q in range(NS_)]
                w32 = [sb(ph, "w32_%d" % q, [128, 512], F32) for q in range(NS_)]
                oas = [[sb(ph, "oas%d_%d" % (q, i), [128, 4, 128], BF16) for i in range(2)] for q in range(NS_)]
                P.dma('sp', mkf[:], maskd, writes=['mkf'])
                for h in range(4):
                    P.op('pool', lambda g, h=h: g.tensor_copy(out=mk4[:, :, h, :], in_=mkf[:]), reads=['mkf'], writes=[('mk4', h)])
                mk4k = [('mk4', h) for h in range(4)]
                P.op('pool', lambda g: g.memset(trif[:], 1.0), writes=['trif'])
                P.op('pool', lambda g: g.affine_select(out=trif[:], in_=trif[:], pattern=[[-1, 128]], compare_op=ALU.is_ge,
                                                       fill=0.0, base=0, channel_multiplier=1), reads=['trif'], writes=['trif'])
                P.op('pool', lambda g: g.tensor_copy(out=triI[:], in_=trif[:]), reads=['trif'], writes=['triI'])
                P.op('pool', lambda g: g.memset(trif[:], 1.0), reads=['trif'], writes=['trif'])
                P.op('pool', lambda g: g.affine_select(out=trif[:], in_=trif[:], pattern=[[1, 128]], compare_op=ALU.is_gt,
                                                       fill=0.0, base=0, channel_multiplier=-1), reads=['trif'], writes=['trif'])
                P.op('pool', lambda g: g.tensor_copy(out=triC[:], in_=trif[:]), reads=['trif'], writes=['triC'])
                sc = 128.0 ** -0.5
                for hg in range(2):
                    for h in range(4):
                        P.dma('sp', KTh[:, h, :], KT[hg * 4 + h], writes=[('KTh', h)])
                    for q4 in range(4):
                        P.dma('act', Vh[:, q4 * 16:(q4 + 1) * 16, :],
                              VV[q4 * 16:(q4 + 1) * 16, :, hg * 512:(hg + 1) * 512].rearrange("b p f -> p b f"),
                              writes=[('Vh', q4)])
                    P.dma('sp', QTh[:], QTd[hg * 4:(hg + 1) * 4].rearrange("c p t -> p c t"), writes=['QTh'])
                    streams = [[(i, kb) for i in range(q, 16, NS_) for kb in range(4 * i + 3, -1, -1)] for q in range(NS_)]

                    def stageA(q, n):
                        i, kb = streams[q][n]
                        zb = 2 * q + (n % 2)
                        p = n % 2
                        e = eb[q][p]
                        sp_ = spb[q][p]
                        ek = 'e%d_%d' % (q, p)
                        sk = 'sp%d_%d' % (q, p)
                        for h in range(4):
                            P.op('pe', lambda pe, h=h, i=i, kb=kb, zb=zb: pe.matmul(
                                banks[zb][:, h * 128:(h + 1) * 128], lhsT=KTh[:, h, kb * 128:(kb + 1) * 128],
                                rhs=QTh[:, h, i * 128:(i + 1) * 128], start=True, stop=True),
                                reads=[('KTh', h), 'QTh'], writes=['b%d' % zb], excl=B(zb))
                        P.op('act', lambda a, zb=zb, e=e: a.activation(out=e[:], in_=banks[zb][:, :], func=AF.Exp, scale=sc),
                             reads=['b%d' % zb], writes=[ek], excl=B(zb))
                        P.op('act', lambda a, e=e, sp_=sp_: a.activation(out=sp_[:], in_=e[:], func=AF.Ln, bias=1.0, scale=1.0),
                             reads=[ek], writes=[sk])
                        if kb >= 4 * i:
                            m = kb - 4 * i
                            P.op('pool', lambda g, sp_=sp_, m=m: g.tensor_tensor(
                                out=sp_[:], in0=sp_[:], in1=mk4[:, m, :, :].rearrange("p h t -> p (h t)"), op=ALU.mult),
                                reads=[sk] + mk4k, writes=[sk])

                    def stageB(q, n, part):
                        i, kb = streams[q][n]
                        p = n % 2
                        e = eb[q][p]
                        sp_ = spb[q][p]
                        g_ = gb_[q][p]
                        w_ = wb_[q][p]
                        ek = 'e%d_%d' % (q, p)
                        sk = 'sp%d_%d' % (q, p)
                        gk = 'g%d_%d' % (q, p)
                        wk = 'w%d_%d' % (q, p)
                        btb = 4 + q
                        otb = 6 + q
                        first = (kb == 4 * i + 3)
                        last = (kb == 0)
                        if part == 1:
                            P.op('pe', lambda pe: pe.matmul(banks[btb][:, :], lhsT=triI[:], rhs=sp_[:], start=first, stop=False),
                                 reads=['triI', sk], writes=['b%d' % btb], excl=B(btb))
                            P.op('act', lambda a: a.activation(out=g_[:], in_=banks[btb][:, :], func=AF.Exp, scale=-1.0),
                                 reads=['b%d' % btb], writes=[gk], excl=B(btb))
                            return
                        P.op('pe', lambda pe: pe.matmul(banks[btb][:, :], lhsT=triC[:], rhs=sp_[:], start=False, stop=last),
                             reads=['triC', sk], writes=['b%d' % btb], excl=B(btb))
                        if kb >= 4 * i:
                            m = kb - 4 * i
                            P.op('dve', lambda v: v.tensor_tensor(out=w32[q][:], in0=e[:], in1=g_[:], op=ALU.mult),
                                 reads=[ek, gk], writes=['w32_%d' % q])
                            P.op('dve', lambda v: v.tensor_tensor(
                                out=w_[:], in0=w32[q][:], in1=mk4[:, m, :, :].rearrange("p h t -> p (h t)"), op=ALU.mult),
                                reads=['w32_%d' % q] + mk4k, writes=[wk])
                        else:
                            P.op('dve', lambda v: v.tensor_tensor(out=w_[:], in0=e[:], in1=g_[:], op=ALU.mult),
                                 reads=[ek, gk], writes=[wk])
                        for h in range(4):
                            P.op('pe', lambda pe, h=h: pe.matmul(
                                banks[otb][:, h * 128:(h + 1) * 128], lhsT=Vh[:, kb, h * 128:(h + 1) * 128],
                                rhs=w_[:, h * 128:(h + 1) * 128], start=(first and h == 0), stop=last),
                                reads=[('Vh', kb // 16), wk], writes=['b%d' % otb], excl=B(otb))
                        if last:
                            oa = oas[q][(i // NS_) % 2]
                            ok_ = 'oas%d_%d' % (q, (i // NS_) % 2)
                            evac_copy(oa[:].rearrange("p c t -> p (c t)"), otb, [ok_])
                            P.dma('sp', oaTd[hg * 4:(hg + 1) * 4, :, i * 128:(i + 1) * 128].rearrange("c p t -> p c t"), oa[:],
                                  reads=[ok_], writes=[('oaTd', hg, i)])

                    lens = [len(st_) for st_ in streams]
                    for q in range(NS_):
                        stageA(q, 0)
                    for n in range(max(lens)):
                        for q in range(NS_):
                            if n < lens[q]:
                                stageB(q, n, 1)
                        for q in range(NS_):
                            if n + 1 < lens[q]:
                                stageA(q, n + 1)
                        for q in range(NS_):
                            if n < lens[q]:
                                stageB(q, n, 2)
                P.barrier()
                P.flush()
        mTd = dscr("mTd", [NKC, 128, OWN], BF16)
        y2d = dscr("y2d", [OWN, D], F32) if debug else None
        if debug:
            dbg_wdh = dscr("dbg_wdh", [2, 128, 8 * NEXP], F32)
            dbg_hact = dscr("dbg_hact", [2, 2, 128, 4 * 512], BF16)
            dbg_h2h = dscr("dbg_h2h", [2, 128, NKC * 1024], BF16)
            dbg_y0 = dscr("dbg_y0", [2, 128, 8 * D], F32)
            dbg_pad = dscr("dbg_pad", [2, 128, 1024], F32)

        def bcast_load(ph, name, src_row):
            t = sb(ph, name, [128, src_row.shape[-1]], F32)
            P.dma('sp', t[:], src_row.partition_broadcast(128).rearrange("p o f -> p (o f)"), writes=[name])
            return t

        if upto >= 4:
            with contextlib.ExitStack() as ph:
                oaT = sb(ph, "oaT", [128, 8, OWN], BF16)
                obT = sb(ph, "obT", [128, 8, OWN], BF16)
                P.dma('sp', oaT[:], oaTd.rearrange("c p t -> p c t"), writes=['oaT'])
                P.dma('act', obT[:], obTd.rearrange("c p t -> p c t"), writes=['obT'])
                was = [sb(ph, "was%d" % i, [128, 8, 128], F32) for i in range(2)]
                wbs = [sb(ph, "wbs%d" % i, [128, 8, 128], F32) for i in range(2)]
                wab = [sb(ph, "wab%d" % i, [128, 8, 128], BF16) for i in range(2)]
                wbb = [sb(ph, "wbb%d" % i, [128, 8, 128], BF16) for i in range(2)]
                gas = [sb(ph, "gas%d" % i, [128, OWN], BF16) for i in range(2)]
                gbs = [sb(ph, "gbs%d" % i, [128, OWN], BF16) for i in range(2)]
                t1 = [sb(ph, "t1%d" % i, [128, 512], F32) for i in range(2)]
                t2 = [sb(ph, "t2%d" % i, [128, 512], F32) for i in range(2)]
                mst = [sb(ph, "mst%d" % i, [128, OWN], BF16) for i in range(2)]
                nn = 0
                for nb in range(16):
                    p2 = nb % 2
                    P.dma('sp', was[p2][:], w_pa[:, nb * 128:(nb + 1) * 128].rearrange("(c p) n -> p c n", p=128), writes=['was%d' % p2])
                    P.dma('sp', wbs[p2][:], w_pb[:, nb * 128:(nb + 1) * 128].rearrange("(c p) n -> p c n", p=128), writes=['wbs%d' % p2])
                    P.op('pool', lambda g, p2=p2: g.tensor_copy(out=wab[p2][:], in_=was[p2][:]), reads=['was%d' % p2], writes=['wab%d' % p2])
                    P.op('pool', lambda g, p2=p2: g.tensor_copy(out=wbb[p2][:], in_=wbs[p2][:]), reads=['wbs%d' % p2], writes=['wbb%d' % p2])
                    P.dma('act', gas[p2][:], gaTd[nb], writes=['gas%d' % p2])
                    P.dma('act', gbs[p2][:], gbTd[nb], writes=['gbs%d' % p2])
                    for oc in range(4):
                        q2 = nn % 2
                        nn += 1
                        ba = 0 + q2
                        bb = 2 + q2
                        for kc in range(8):
                            P.op('pe', lambda pe, kc=kc, p2=p2, oc=oc, ba=ba: pe.matmul(
                                banks[ba][:, :], lhsT=wab[p2][:, kc, :], rhs=oaT[:, kc, oc * 512:(oc + 1) * 512],
                                start=(kc == 0), stop=(kc == 7)), reads=['wab%d' % p2, 'oaT'], writes=['b%d' % ba], excl=B(ba))
                        for kc in range(8):
                            P.op('pe', lambda pe, kc=kc, p2=p2, oc=oc, bb=bb: pe.matmul(
                                banks[bb][:, :], lhsT=wbb[p2][:, kc, :], rhs=obT[:, kc, oc * 512:(oc + 1) * 512],
                                start=(kc == 0), stop=(kc == 7)), reads=['wbb%d' % p2, 'obT'], writes=['b%d' % bb], excl=B(bb))
                        P.op('dve', lambda v, p2=p2, oc=oc, ba=ba, q2=q2: v.tensor_tensor(
                            out=t1[q2][:], in0=banks[ba][:, :], in1=gas[p2][:, oc * 512:(oc + 1) * 512], op=ALU.mult),
                            reads=['b%d' % ba, 'gas%d' % p2], writes=['t1%d' % q2], excl=B(ba))
                        P.op('dve', lambda v, p2=p2, oc=oc, bb=bb, q2=q2: v.tensor_tensor(
                            out=t2[q2][:], in0=banks[bb][:, :], in1=gbs[p2][:, oc * 512:(oc + 1) * 512], op=ALU.mult),
                            reads=['b%d' % bb, 'gbs%d' % p2], writes=['t2%d' % q2], excl=B(bb))
                        P.op('pool', lambda g, p2=p2, oc=oc, q2=q2: g.tensor_tensor(
                            out=mst[p2][:, oc * 512:(oc + 1) * 512], in0=t1[q2][:], in1=t2[q2][:], op=ALU.add),
                            reads=['t1%d' % q2, 't2%d' % q2], writes=[('mst%d' % p2, oc)])
                    P.dma('sp', mTd[nb], mst[p2][:], reads=[('mst%d' % p2, oc) for oc in range(4)], writes=[('mTd', nb)])
                P.barrier()
                P.flush()

        if upto >= 5:
            with contextlib.ExitStack() as ph:
                Wo = sb(ph, "Wo", [128, NKC, D], BF16)
                wst = [sb(ph, "wsto%d" % i, [128, 2048], F32) for i in range(2)]
                for kc in range(NKC):
                    w = wst[kc % 2]
                    wk = 'wsto%d' % (kc % 2)
                    P.dma('sp', w[:], w_out[kc * 128:(kc + 1) * 128, :], writes=[wk])
                    P.op('pool', lambda g, w=w, kc=kc: g.tensor_copy(out=Wo[:, kc, :], in_=w[:]), reads=[wk], writes=[('Wo', kc)])
                G1 = bcast_load(ph, "G1", modv[0:1, 2 * D:3 * D])
                L1g = bcast_load(ph, "L1g", ln1_g)
                L1b = bcast_load(ph, "L1b", ln1_b)
                brt = bcast_load(ph, "brt", b_rt)
                Wrt = sb(ph, "Wrt", [128, NKC, 36], F32)
                P.dma('sp', Wrt[:], w_rt.rearrange("(c p) n -> p c n", p=128), writes=['Wrt'])
                lb = ln_bufs(ph)
                xin, xn, st, mv, sd, cnt = lb
                mts = [sb(ph, "mts%d" % i, [128, NKC, 128], BF16) for i in range(2)]
                tg = sb(ph, "tg", [128, D], F32)
                rt = sb(ph, "rt", [128, D], F32)
                x1t = sb(ph, "x1t", [128, D], F32)
                h2t = [sb(ph, "h2t%d" % i, [128, NKC, 128], BF16) for i in range(2)]
                h2f = sb(ph, "h2f", [128, NKC, 128], F32)
                lg = sb(ph, "lg", [128, 36], F32)
                rs = sb(ph, "rs", [128, 64], F32)
                wdt = [sb(ph, "wdt%d" % i, [128, 32], F32) for i in range(2)]
                for t in range(16):
                    p2 = t % 2
                    mt = mts[p2]
                    P.dma('sp', mt[:], mTd[:, :, t * 128:(t + 1) * 128].rearrange("c p t -> p c t"), writes=['mts%d' % p2])
                    xt = xin[t % 2]
                    xk = 'xin%d' % (t % 2)
                    P.dma('act', xt[:], xo[t * 128:(t + 1) * 128, :], writes=[xk])
                    for fb in range(4):
                        for kc in range(NKC):
                            P.op('pe', lambda pe, kc=kc, fb=fb, mt=mt: pe.matmul(
                                banks[fb][:, :], lhsT=mt[:, kc, :], rhs=Wo[:, kc, fb * 512:(fb + 1) * 512],
                                start=(kc == 0), stop=(kc == NKC - 1)),
                                reads=['mts%d' % p2, ('Wo', kc)], writes=['b%d' % fb], excl=B(fb))
                        P.op('dve', lambda v, fb=fb: v.tensor_tensor(
                            out=tg[:, fb * 512:(fb + 1) * 512], in0=banks[fb][:, :], in1=G1[:, fb * 512:(fb + 1) * 512], op=ALU.mult),
                            reads=['b%d' % fb, 'G1'], writes=[('tg', fb)], excl=B(fb))
                    P.op('dve', lambda v, xt=xt: v.scalar_tensor_tensor(out=rt[:], in0=xt[:], scalar=ALPHA, in1=tg[:],
                                                                       op0=ALU.mult, op1=ALU.add),
                         reads=[xk] + [('tg', fb) for fb in range(4)], writes=['rt'])
                    layer_norm_tile(rt, 'rt', xn, 'xn', st, mv, sd, eps_t)
                    P.op('dve', lambda v: v.tensor_tensor(out=xn[:], in0=xn[:], in1=L1g[:], op=ALU.mult), reads=['xn', 'L1g'], writes=['xn'])
                    P.op('pool', lambda g: g.tensor_tensor(out=x1t[:], in0=xn[:], in1=L1b[:], op=ALU.add), reads=['xn', 'L1b'], writes=['x1t'])
                    P.dma('sp', x1d[t * 128:(t + 1) * 128, :], x1t[:], reads=['x1t'], writes=[('x1d', t)])
                    h2 = h2t[p2]
                    ln_T_tile(lb, None, h2, 'h2t%d' % p2, 0, 3, fdst=h2f, bo=4, xt_in=x1t, xk_in='x1t')
                    P.dma('sp', h2Td[:, :, t * 128:(t + 1) * 128].rearrange("c p t -> p c t"), h2[:],
                          reads=[('h2t%d' % p2, kc, 0) for kc in range(NKC)], writes=[('h2Td', t)])
                    for kc in range(NKC):
                        P.op('pe', lambda pe, kc=kc: pe.matmul(banks[0][:, 0:36], lhsT=h2f[:, kc, :], rhs=Wrt[:, kc, :],
                                                                start=(kc == 0), stop=(kc == NKC - 1)),
                             reads=[('fdst', kc), 'Wrt'], writes=['b0'], excl=B(0))
                    P.op('dve', lambda v: v.tensor_tensor(out=lg[:], in0=banks[0][:, 0:36], in1=brt[:], op=ALU.add),
                         reads=['b0', 'brt'], writes=['lg'], excl=B(0))
                    wd_ = wdt[p2]
                    V = lambda f, r, w: P.op('dve', f, reads=r, writes=w)
                    V(lambda v: v.reduce_max(out=rs[:, 0:1], in_=lg[:, 0:4], axis=mybir.AxisListType.X), ['lg'], ['r0'])
                    V(lambda v: v.tensor_scalar(out=rs[:, 1:2], in0=rs[:, 0:1], scalar1=-1.0, scalar2=None, op0=ALU.mult), ['r0'], ['r1'])
                    P.op('act', lambda a: a.activation(out=rs[:, 4:8], in_=lg[:, 0:4], func=AF.Exp, bias=rs[:, 1:2], scale=1.0,
                                                       accum_out=rs[:, 2:3]), reads=['lg', 'r1'], writes=['r2'])
                    V(lambda v: v.reciprocal(out=rs[:, 3:4], in_=rs[:, 2:3]), ['r2'], ['r3'])
                    V(lambda v: v.tensor_scalar(out=rs[:, 8:12], in0=lg[:, 0:4], scalar1=rs[:, 0:1], scalar2=None, op0=ALU.is_equal),
                      ['lg', 'r0'], ['gm'])
                    V(lambda v: v.tensor_scalar(out=rs[:, 12:20], in0=lg[:, 4:12], scalar1=rs[:, 8:9], scalar2=None, op0=ALU.mult),
                      ['lg', 'gm'], ['sel'])
                    for g in range(1, 4):
                        V(lambda v, g=g: v.scalar_tensor_tensor(out=rs[:, 12:20], in0=lg[:, 4 + 8 * g:12 + 8 * g], scalar=rs[:, 8 + g:9 + g],
                                                                in1=rs[:, 12:20], op0=ALU.mult, op1=ALU.add), ['lg', 'gm', 'sel'], ['sel'])
                    V(lambda v: v.max(out=rs[:, 20:28], in_=rs[:, 12:20]), ['sel'], ['top8'])
                    V(lambda v: v.tensor_scalar(out=rs[:, 28:36], in0=rs[:, 12:20], scalar1=rs[:, 20:21], scalar2=None, op0=ALU.is_equal),
                      ['sel', 'top8'], ['m1'])
                    V(lambda v: v.tensor_scalar(out=rs[:, 36:44], in0=rs[:, 12:20], scalar1=rs[:, 21:22], scalar2=None, op0=ALU.is_equal),
                      ['sel', 'top8'], ['m2'])
                    V(lambda v: v.tensor_tensor(out=rs[:, 44:45], in0=rs[:, 21:22], in1=rs[:, 20:21], op=ALU.subtract), ['top8'], ['rd'])
                    P.op('act', lambda a: a.activation(out=rs[:, 45:46], in_=rs[:, 44:45], func=AF.Exp), reads=['rd'], writes=['red'])
                    V(lambda v: v.tensor_scalar(out=rs[:, 46:47], in0=rs[:, 45:46], scalar1=1.0, scalar2=None, op0=ALU.add), ['red'], ['rden'])
                    V(lambda v: v.reciprocal(out=rs[:, 47:48], in_=rs[:, 46:47]), ['rden'], ['rw1'])
                    V(lambda v: v.tensor_tensor(out=rs[:, 48:49], in0=rs[:, 45:46], in1=rs[:, 47:48], op=ALU.mult), ['red', 'rw1'], ['rw2'])
                    V(lambda v: v.tensor_tensor(out=rs[:, 49:50], in0=rs[:, 47:48], in1=rs[:, 3:4], op=ALU.mult), ['rw1', 'r3'], ['rw1p'])
                    V(lambda v: v.tensor_tensor(out=rs[:, 50:51], in0=rs[:, 48:49], in1=rs[:, 3:4], op=ALU.mult), ['rw2', 'r3'], ['rw2p'])
                    V(lambda v: v.tensor_scalar(out=rs[:, 52:60], in0=rs[:, 28:36], scalar1=rs[:, 49:50], scalar2=None, op0=ALU.mult),
                      ['m1', 'rw1p'], ['cw'])
                    V(lambda v: v.scalar_tensor_tensor(out=rs[:, 52:60], in0=rs[:, 36:44], scalar=rs[:, 50:51], in1=rs[:, 52:60],
                                                       op0=ALU.mult, op1=ALU.add), ['m2', 'rw2p', 'cw'], ['cw'])
                    for g in range(4):
                        V(lambda v, g=g, wd_=wd_: v.tensor_scalar(out=wd_[:, g * 8:(g + 1) * 8], in0=rs[:, 52:60], scalar1=rs[:, 8 + g:9 + g],
                                                                  scalar2=None, op0=ALU.mult), ['cw', 'gm'], [('wdt%d' % p2, g)])
                    P.dma('sp', wdd[t * 128:(t + 1) * 128, :], wd_[:], reads=[('wdt%d' % p2, g) for g in range(4)], writes=[('wdd', t)])
                P.barrier()
                P.flush()

        if upto >= 6:
            for half in range(2):
                with contextlib.ExitStack() as ph:
                    y2 = sb(ph, "y2", [128, 8, D], F32)
                    wdh = sb(ph, "wdh", [128, 8, NEXP], F32)
                    h2h = sb(ph, "h2h", [128, NKC, 1024], BF16)
                    P.dma('sp', h2h[:], h2Td[:, :, half * 1024:(half + 1) * 1024].rearrange("c p t -> p c t"), writes=['h2h'])
                    P.dma('sp', wdh[:], wdd[half * 1024:(half + 1) * 1024, :].rearrange("(c p) e -> p c e", p=128), writes=['wdh'])
                    for ti in range(8):
                        P.op('pool', lambda g, ti=ti: g.memset(y2[:, ti, :], 0.0), writes=[('y2', ti, fb) for fb in range(4)])
                    if debug:
                        P.dma('sp', dbg_y0[half], y2[:].rearrange("p a b -> p (a b)"),
                              reads=[('y2', ti, fb) for ti in range(8) for fb in range(4)], writes=['dbg_y0'])
                        P.dma('sp', dbg_wdh[half], wdh[:].rearrange("p a b -> p (a b)"), reads=['wdh'], writes=['dbg_wdh'])
                        P.dma('sp', dbg_h2h[half], h2h[:].rearrange("p a b -> p (a b)"), reads=['h2h'], writes=['dbg_h2h'])
                    with contextlib.ExitStack() as ph2:
                        hact = [sb(ph2, "hact%d" % i, [128, 4, 512], BF16) for i in range(2)]
                        sil = [sb(ph2, "sil%d" % i, [128, 512], F32) for i in range(2)]
                        wg = sb(ph2, "wg", [128, NKC, DEXP], BF16)
                        wu = sb(ph2, "wu", [128, NKC, DEXP], BF16)
                        wdn = sb(ph2, "wdn", [128, 4, D], BF16)
                        NSTG = 3
                        stg = [sb(ph2, "stg%d" % i, [128, 1024], F32) for i in range(NSTG)]
                        pad = sb(ph2, "pad", [128, 1024], F32)
                        P.op('pool', lambda g: g.memset(pad[:], 0.0), writes=['pad'])
                        ns = 0
                        nsl = 0
                        nh = 0
                        nbk = 0
                        for e in range(NEXP):
                            for (src, dstw, dk_, nhalf, ceng) in ((w_gate[e], wg, 'wg', 2, 'pool'), (w_up[e], wu, 'wu', 2, 'act'),
                                                                  (w_down[e], wdn, 'wdn', 2, 'pool')):
                                for hf in range(8):
                                    sg = stg[ns % NSTG]
                                    sk = 'stg%d' % (ns % NSTG)
                                    ns += 1
                                    if dk_ == 'wdn':
                                        dcw, hh = hf // 2, hf % 2
                                        P.dma('sp', sg[:], src[dcw * 128:(dcw + 1) * 128, hh * 1024:(hh + 1) * 1024], writes=[sk])
                                        dst_ap = dstw[:, dcw, hh * 1024:(hh + 1) * 1024]
                                    else:
                                        P.dma('sp', sg[:].rearrange("p (c n) -> p c n", c=2),
                                              src[hf * 256:(hf + 1) * 256, :].rearrange("(c p) n -> p c n", p=128), writes=[sk])
                                        dst_ap = dstw[:, hf * 2:(hf + 1) * 2, :].rearrange("p c n -> p (c n)")
                                    if ceng == 'pool':
                                        P.op('pool', lambda g, sg=sg, dst_ap=dst_ap: g.tensor_copy(out=dst_ap, in_=sg[:]),
                                             reads=[sk], writes=[(dk_, hf)])
                                    else:
                                        P.op('act', lambda a, sg=sg, dst_ap=dst_ap: a.copy(out=dst_ap, in_=sg[:]),
                                             reads=[sk], writes=[(dk_, hf)])
                            for ch in range(2):
                                ha = hact[nh % 2]
                                hk = 'hact%d' % (nh % 2)
                                nh += 1
                                for dc in range(4):
                                    bg = nbk % 2
                                    bu = 2 + nbk % 2
                                    nbk += 1
                                    for kc in range(NKC):
                                        P.op('pe', lambda pe, kc=kc, dc=dc, ch=ch, bg=bg: pe.matmul(
                                            banks[bg][:, :], lhsT=wg[:, kc, dc * 128:(dc + 1) * 128], rhs=h2h[:, kc, ch * 512:(ch + 1) * 512],
                                            start=(kc == 0), stop=(kc == NKC - 1)), reads=[('wg', kc // 2), 'h2h'], writes=['b%d' % bg], excl=B(bg))
                                    for kc in range(NKC):
                                        P.op('pe', lambda pe, kc=kc, dc=dc, ch=ch, bu=bu: pe.matmul(
                                            banks[bu][:, :], lhsT=wu[:, kc, dc * 128:(dc + 1) * 128], rhs=h2h[:, kc, ch * 512:(ch + 1) * 512],
                                            start=(kc == 0), stop=(kc == NKC - 1)), reads=[('wu', kc // 2), 'h2h'], writes=['b%d' % bu], excl=B(bu))
                                    sl = sil[nsl % 2]
                                    slk = 'sil%d' % (nsl % 2)
                                    nsl += 1
                                    P.op('act', lambda a, sl=sl, bg=bg: a.activation(out=sl[:], in_=banks[bg][:, :], func=AF.Silu),
                                         reads=['b%d' % bg], writes=[slk], excl=B(bg))
                                    P.op('dve', lambda v, sl=sl, bu=bu, ha=ha, dc=dc: v.tensor_tensor(
                                        out=ha[:, dc, :], in0=banks[bu][:, :], in1=sl[:], op=ALU.mult),
                                        reads=['b%d' % bu, slk], writes=[(hk, dc)], excl=B(bu))
                                for tl in range(4):
                                    ti = ch * 4 + tl
                                    for fb in range(4):
                                        bd = 4 + fb
                                        for dc in range(4):
                                            P.op('pe', lambda pe, dc=dc, tl=tl, fb=fb, bd=bd, ha=ha: pe.matmul(
                                                banks[bd][:, :], lhsT=ha[:, dc, tl * 128:(tl + 1) * 128], rhs=wdn[:, dc, fb * 512:(fb + 1) * 512],
                                                start=(dc == 0), stop=(dc == 3)), reads=[(hk, dc), ('wdn', dc * 2 + fb // 2)], writes=['b%d' % bd], excl=B(bd))
                                        P.op('dve', lambda v, ti=ti, fb=fb, bd=bd, e=e: v.scalar_tensor_tensor(
                                            out=y2[:, ti, fb * 512:(fb + 1) * 512], in0=banks[bd][:, :], scalar=wdh[:, ti, e:e + 1],
                                            in1=y2[:, ti, fb * 512:(fb + 1) * 512], op0=ALU.mult, op1=ALU.add),
                                            reads=['b%d' % bd, 'wdh', ('y2', ti, fb)], writes=[('y2', ti, fb)], excl=B(bd))
                        if debug:
                            P.dma('sp', dbg_pad[half], pad[:], reads=['pad'], writes=['dbg_pad'])
                            for q in range(2):
                                P.dma('sp', dbg_hact[half, q], hact[q][:].rearrange("p a b -> p (a b)"),
                                      reads=[('hact%d' % q, dc) for dc in range(4)], writes=[('dbg_hact', q)])
                    P.barrier()
                    if debug:
                        for ti in range(8):
                            t = half * 8 + ti
                            P.dma('sp', y2d[t * 128:(t + 1) * 128, :], y2[:, ti, :],
                                  reads=[('y2', ti, fb) for fb in range(4)], writes=[('y2d', t)])
                    with contextlib.ExitStack() as ph3:
                        G2 = bcast_load(ph3, "G2", modv[0:1, 5 * D:6 * D])
                        L2g = bcast_load(ph3, "L2g", ln2_g)
                        L2b = bcast_load(ph3, "L2b", ln2_b)
                        lb = ln_bufs(ph3)
                        xin, xn, st, mv, sd, cnt = lb
                        tg2 = sb(ph3, "tg2", [128, D], F32)
                        rt2 = sb(ph3, "rt2", [128, D], F32)
                        ot = [sb(ph3, "ot%d" % i, [128, D], F32) for i in range(2)]
                        for ti in range(8):
                            t = half * 8 + ti
                            xt = xin[ti % 2]
                            xk = 'xin%d' % (ti % 2)
                            P.dma('act', xt[:], x1d[t * 128:(t + 1) * 128, :], writes=[xk])
                            P.op('pool', lambda g, ti=ti: g.tensor_tensor(out=tg2[:], in0=y2[:, ti, :], in1=G2[:], op=ALU.mult),
                                 reads=[('y2', ti, fb) for fb in range(4)] + ['G2'], writes=['tg2'])
                            P.op('dve', lambda v, xt=xt: v.scalar_tensor_tensor(out=rt2[:], in0=xt[:], scalar=ALPHA, in1=tg2[:],
                                                                               op0=ALU.mult, op1=ALU.add), reads=[xk, 'tg2'], writes=['rt2'])
                            layer_norm_tile(rt2, 'rt2', xn, 'xn', st, mv, sd, eps_t)
                            o = ot[ti % 2]
                            ok_ = 'ot%d' % (ti % 2)
                            P.op('dve', lambda v: v.tensor_tensor(out=xn[:], in0=xn[:], in1=L2g[:], op=ALU.mult), reads=['xn', 'L2g'], writes=['xn'])
                            P.op('pool', lambda g, o=o: g.tensor_tensor(out=o[:], in0=xn[:], in1=L2b[:], op=ALU.add), reads=['xn', 'L2b'], writes=[ok_])
                            P.dma('sp', out[t * 128:(t + 1) * 128, :], o[:], reads=[ok_], writes=[('out', t)])
                    P.barrier()
                    P.flush()
        P.barrier()
        P.flush()
    return nc


def make_in_maps(inputs):
    x = np.asarray(inputs["x"], dtype=np.float32)
    c = np.asarray(inputs["c"], dtype=np.float32)
    g = lambda k: np.ascontiguousarray(np.asarray(inputs[k], dtype=np.float32)[0])
    w_rt = np.ascontiguousarray(np.concatenate([g("w_group"), g("w_router")], axis=1))
    b_rt = np.ascontiguousarray(np.concatenate([g("b_group"), g("b_router")], axis=0)[None, :])
    sgu_wT = np.ascontiguousarray(g("sgu_w").transpose(2, 0, 1))
    shared = {
        "w_ada": g("w_ada"), "b_ada": g("b_ada")[None, :], "w_in": g("w_in"),
        "sgu_wT": sgu_wT, "sgu_b": g("sgu_b").reshape(1, -1),
        "sgu_ln_g": g("sgu_ln_g")[None, :], "sgu_ln_b": g("sgu_ln_b")[None, :],
        "w_proj_a": g("w_proj_a"), "w_proj_b": g("w_proj_b"), "w_out": g("w_out"),
        "ln1_g": g("ln1_g")[None, :], "ln1_b": g("ln1_b")[None, :],
        "w_rt": w_rt, "b_rt": b_rt,
        "w_gate": g("w_gate"), "w_up": g("w_up"), "w_down": g("w_down"),
        "ln2_g": g("ln2_g")[None, :], "ln2_b": g("ln2_b")[None, :],
    }
    maps = []
    for core in range(8):
        b = core // 4
        m = dict(shared)
        j = core % 4
        m["xb"] = np.ascontiguousarray(x[b])
        m["xo"] = np.ascontiguousarray(x[b].reshape(16, 4, 128, D)[:, j].reshape(OWN, D))
        mk = np.zeros((128, 4, 128), np.float32)
        for mm in range(4):
            if mm < j:
                mk[:, mm, :] = 1.0
            elif mm == j:
                mk[:, mm, :] = np.triu(np.ones((128, 128), np.float32), k=1)
        m["maskd"] = mk
        m["cT"] = np.ascontiguousarray(c[b].reshape(NKC, 128).T)
        maps.append(m)
    return maps


_NC_CACHE = {}


def kernel(**inputs):
    if "nc" not in _NC_CACHE:
        _NC_CACHE["nc"] = build()
    nc = _NC_CACHE["nc"]
    maps = make_in_maps(inputs)
    res = run_bass_kernel_spmd(nc, maps, core_ids=list(range(8)))
    full = np.zeros((2, S, D), np.float32)
    for core in range(8):
        b, j = core // 4, core % 4
        o = np.asarray(res.results[core]["out"], dtype=np.float32).reshape(16, 128, D)
        full[b].reshape(16, 4, 128, D)[:, j] = o
    return full
```

```python
import contextlib
import os
import numpy as np
import ml_dtypes
import concourse.bass as bass
import concourse.mybir as mybir
from concourse.bass_utils import run_bass_kernel_spmd

F32 = mybir.dt.float32
BF16 = mybir.dt.bfloat16
U32 = mybir.dt.uint32
AF = mybir.ActivationFunctionType
ALU = mybir.AluOpType

D = 2048
S = 8192
NKC = 16
OWN = 2048
LN_EPS = 1e-5
ALPHA = 2.0 ** 0.25
NEXP = 32
DEXP = 512


class Prog:
    LIMIT = 30000
    NDMA = 6

    def __init__(self, nc, stack):
        self.nc = nc
        self.stack = stack
        self.names = ['pe', 'act', 'dve', 'pool', 'sp']
        self.sems = []
        self.ops = {e: [] for e in self.names}
        self.cur = {}
        self.cnt = {}
        for e in self.names:
            self.cur[e] = self._new_sem("c_" + e)
            self.cnt[e] = 0
        self.waited = {e: {} for e in self.names}
        self.lastw = {}
        self.readers = {}
        self.latest = {}
        self.dq = {}
        self.dn = {}
        for q in ['sp', 'act', 'pool']:
            self.dq[q] = [self._new_sem("d_%s%d" % (q, i)) for i in range(self.NDMA)]
            self.dn[q] = 0
        self.n_instr = 0

    def _new_sem(self, name):
        h = self.stack.enter_context(self.nc.semaphore(name + "_%d" % len(self.sems)))
        self.sems.append(h)
        return len(self.sems) - 1

    def _deps(self, e, reads, writes, excl=()):
        deps = []
        for k in excl:
            t = self.lastw.get(k)
            if t is not None and t[0] != self.cur.get(e, -1):
                deps.append(t)
        for k in reads:
            t = self.lastw.get(k)
            if t is not None:
                deps.append(t)
        for k in writes:
            t = self.lastw.get(k)
            if t is not None:
                deps.append(t)
            deps.extend(self.readers.get(k, ()))
        waits = {}
        for (s, v) in deps:
            if e == 'pe' and s == self.cur['pe']:
                continue
            if self.waited[e].get(s, 0) >= v:
                continue
            if waits.get(s, 0) < v:
                waits[s] = v
        for s, v in waits.items():
            self.waited[e][s] = v
        return list(waits.items())

    def _commit(self, tok, reads, writes):
        for k in writes:
            self.lastw[k] = tok
            self.readers[k] = []
        for k in reads:
            if k in writes:
                continue
            self.readers.setdefault(k, []).append(tok)
        self.latest[tok[0]] = tok[1]

    def op(self, e, fn, reads=(), writes=(), excl=()):
        waits = self._deps(e, reads, writes, excl)
        if self.cnt[e] >= self.LIMIT:
            self.cur[e] = self._new_sem("c_" + e)
            self.cnt[e] = 0
        self.cnt[e] += 1
        tok = (self.cur[e], self.cnt[e])
        sems = self.sems

        def emit(eng, waits=waits, fn=fn, tok=tok):
            for (s, v) in waits:
                eng.wait_ge(sems[s], v)
            fn(eng).then_inc(sems[tok[0]], 1)
        self.ops[e].append(emit)
        self._commit(tok, reads, writes)
        for k in excl:
            self.lastw[k] = tok
        self.n_instr += 1
        return tok

    def dma(self, q, out, in_, reads=(), writes=(), **kw):
        waits = self._deps(q, reads, writes)
        i = self.dn[q]
        self.dn[q] += 1
        k = i % self.NDMA
        val = 16 * (i // self.NDMA + 1)
        s = self.dq[q][k]
        if i >= self.NDMA and self.waited[q].get(s, 0) < val - 16:
            waits.append((s, val - 16))
            self.waited[q][s] = val - 16
        tok = (s, val)
        sems = self.sems

        def emit(eng, waits=waits, tok=tok, out=out, in_=in_, kw=kw):
            for (ss, v) in waits:
                eng.wait_ge(sems[ss], v)
            eng.dma_start(out=out, in_=in_, **kw).then_inc(sems[tok[0]], 16)
        self.ops[q].append(emit)
        self._commit(tok, reads, writes)
        self.n_instr += 1
        return tok

    def barrier(self):
        for e in self.names:
            waits = []
            for s, v in self.latest.items():
                if self.waited[e].get(s, 0) < v:
                    waits.append((s, v))
                    self.waited[e][s] = v
            sems = self.sems

            def emit(eng, waits=waits):
                for (s, v) in waits:
                    eng.wait_ge(sems[s], v)
            self.ops[e].append(emit)
        self.lastw.clear()
        self.readers.clear()

    def flush(self):
        nc = self.nc
        ops = self.ops
        with nc.Block() as block:
            @block.tensor
            def _(eng):
                for f in ops['pe']:
                    f(eng)

            @block.scalar
            def _(eng):
                for f in ops['act']:
                    f(eng)

            @block.vector
            def _(eng):
                for f in ops['dve']:
                    f(eng)

            @block.gpsimd
            def _(eng):
                for f in ops['pool']:
                    f(eng)

            @block.sync
            def _(eng):
                for f in ops['sp']:
                    f(eng)
        self.ops = {e: [] for e in self.names}


def build(upto=99, debug=False, lite=False):
    nc = bass.Bass("TRN2", target_bir_lowering=False)
    dk = "ExternalOutput"

    def din(name, shape, dt=F32):
        return nc.dram_tensor(name, list(shape), dt, kind="ExternalInput").ap()

    xb = din("xb", [S, D])
    xo = din("xo", [OWN, D])
    maskd = din("maskd", [128, 4, 128])
    cT = din("cT", [128, NKC])
    w_ada = din("w_ada", [D, 6 * D])
    b_ada = din("b_ada", [1, 6 * D])
    w_in = din("w_in", [D, 9216])
    sgu_wT = din("sgu_wT", [128, 8, 128])
    sgu_b = din("sgu_b", [1, 8 * 128])
    sgu_ln_g = din("sgu_ln_g", [1, 1024])
    sgu_ln_b = din("sgu_ln_b", [1, 1024])
    w_pa = din("w_proj_a", [1024, D])
    w_pb = din("w_proj_b", [1024, D])
    w_out = din("w_out", [D, D])
    ln1_g = din("ln1_g", [1, D])
    ln1_b = din("ln1_b", [1, D])
    w_rt = din("w_rt", [D, 36])
    b_rt = din("b_rt", [1, 36])
    if not lite:
        w_gate = din("w_gate", [NEXP, D, DEXP])
        w_up = din("w_up", [NEXP, D, DEXP])
        w_down = din("w_down", [NEXP, DEXP, D])
    ln2_g = din("ln2_g", [1, D])
    ln2_b = din("ln2_b", [1, D])
    out = nc.dram_tensor("out", [OWN, D], F32, kind="ExternalOutput").ap()

    def dscr(name, shape, dt):
        return nc.dram_tensor(name, list(shape), dt, kind=dk).ap()

    modv = dscr("modv", [1, 6 * D], F32)
    KT = dscr("KT", [8, 128, S], BF16)
    VV = dscr("VV", [64, 128, 1024], BF16)
    QTd = dscr("QTd", [8, 128, OWN], BF16)
    obTd = dscr("obTd", [8, 128, OWN], BF16)
    gaTd = dscr("gaTd", [NKC, 128, OWN], BF16)
    gbTd = dscr("gbTd", [NKC, 128, OWN], BF16)
    oaTd = dscr("oaTd", [8, 128, OWN], BF16)
    x1d = dscr("x1d", [OWN, D], F32)
    h2Td = dscr("h2Td", [NKC, 128, OWN], BF16)
    wdd = dscr("wdd", [OWN, NEXP], F32)

    with contextlib.ExitStack() as top:
        P = Prog(nc, top)
        banks = [top.enter_context(nc.psum_tensor("bank%d" % i, [128, 512], F32)) for i in range(8)]
        ident = top.enter_context(nc.sbuf_tensor("ident", [128, 128], F32))
        identb = top.enter_context(nc.sbuf_tensor("identb", [128, 128], BF16))
        ones_r = top.enter_context(nc.sbuf_tensor("ones_r", [1, 128], F32))

        uniq = [0]

        def sb(ph, name, shape, dt):
            uniq[0] += 1
            return ph.enter_context(nc.sbuf_tensor("%s_%d" % (name, uniq[0]), list(shape), dt))

        P.op('pool', lambda g: g.memset(ident[:], 1.0), writes=['ident'])
        P.op('pool', lambda g: g.affine_select(out=ident[:], in_=ident[:], pattern=[[-1, 128]],
                                               compare_op=ALU.is_equal, fill=0.0, base=0,
                                               channel_multiplier=1),
             reads=['ident'], writes=['ident'])
        P.op('pool', lambda g: g.tensor_copy(out=identb[:], in_=ident[:]), reads=['ident'], writes=['identb'])
        P.op('pool', lambda g: g.memset(ones_r[:], 1.0), writes=['ones_r'])

        if upto >= 0:
            with contextlib.ExitStack() as ph:
                cact = sb(ph, "cact", [128, NKC], F32)
                craw = sb(ph, "craw", [128, NKC], F32)
                bada = sb(ph, "bada", [1, 6 * D], F32)
                wst = [sb(ph, "wst%d" % i, [128, 2048], F32) for i in range(3)]
                mrow = sb(ph, "mrow", [1, 2048], F32)
                P.dma('sp', craw[:], cT, writes=['craw'])
                P.dma('sp', bada[:], b_ada, writes=['bada'])
                P.op('act', lambda a: a.activation(out=cact[:], in_=craw[:], func=AF.Silu),
                     reads=['craw'], writes=['cact'])
                n = 0
                for g in range(6):
                    for kc in range(NKC):
                        w = wst[n % 3]
                        wk = 'wst%d' % (n % 3)
                        n += 1
                        P.dma('sp', w[:], w_ada[kc * 128:(kc + 1) * 128, g * 2048:(g + 1) * 2048], writes=[wk])
                        for jj in range(4):
                            P.op('pe', lambda pe, w=w, jj=jj, kc=kc: pe.matmul(
                                banks[jj][0:1, :], lhsT=cact[:, kc:kc + 1], rhs=w[:, jj * 512:(jj + 1) * 512],
                                start=(kc == 0), stop=(kc == NKC - 1)),
                                reads=[wk, 'cact'], writes=['b%d' % jj])
                    for jj in range(4):
                        P.op('dve', lambda v, jj=jj, g=g: v.tensor_tensor(
                            out=mrow[0:1, jj * 512:(jj + 1) * 512], in0=banks[jj][0:1, :],
                            in1=bada[0:1, g * 2048 + jj * 512: g * 2048 + (jj + 1) * 512], op=ALU.add),
                            reads=['b%d' % jj, 'bada'], writes=['mrow%d' % jj])
                    P.dma('sp', modv[0:1, g * 2048:(g + 1) * 2048], mrow[:],
                          reads=['mrow%d' % jj for jj in range(4)], writes=['modv'])
                P.barrier()
                P.flush()

        mfm = top.enter_context(nc.sbuf_tensor("mfm", [128, 6, NKC], F32))
        mview = modv.rearrange("o (s c p) -> p (o s) c", p=128, c=NKC)
        for s6 in range(6):
            P.dma('sp', mfm[:, s6, :], mview[:, s6, :], reads=['modv'], writes=['mfm'],
                  allow_slow_non_contiguous=True)
        P.op('dve', lambda v: v.tensor_scalar(out=mfm[:, 1, :], in0=mfm[:, 1, :], scalar1=1.0, scalar2=None,
                                              op0=ALU.add), reads=['mfm'], writes=['mfm'])
        P.op('dve', lambda v: v.tensor_scalar(out=mfm[:, 4, :], in0=mfm[:, 4, :], scalar1=1.0, scalar2=None,
                                              op0=ALU.add), reads=['mfm'], writes=['mfm'])

        def layer_norm_tile(xt, xk, xn, xnk, st, mv, sd, eps_t, sfx=''):
            for q in range(4):
                P.op('dve', lambda v, q=q: v.bn_stats(out=st[:, q, :], in_=xt[:, q * 512:(q + 1) * 512]),
                     reads=[xk], writes=['st%d' % q + sfx])
            P.op('dve', lambda v: v.bn_aggr(out=mv[:], in_=st[:].rearrange("p a b -> p (a b)")),
                 reads=['st%d' % q + sfx for q in range(4)], writes=['mv' + sfx])
            P.op('act', lambda a: a.activation(out=sd[:, 0:1], in_=mv[:, 1:2], func=AF.Sqrt, bias=eps_t[:, 0:1],
                                               scale=1.0),
                 reads=['mv' + sfx, 'eps'], writes=['sd0' + sfx])
            P.op('dve', lambda v: v.reciprocal(out=sd[:, 1:2], in_=sd[:, 0:1]), reads=['sd0' + sfx], writes=['sd1' + sfx])
            P.op('dve', lambda v: v.tensor_scalar(out=sd[:, 2:3], in0=mv[:, 0:1], scalar1=sd[:, 1:2], scalar2=-1.0,
                                                  op0=ALU.mult, op1=ALU.mult),
                 reads=['mv' + sfx, 'sd1' + sfx], writes=['sd2' + sfx])
            P.op('act', lambda a: a.activation(out=xn[:], in_=xt[:], func=AF.Identity, bias=sd[:, 2:3],
                                               scale=sd[:, 1:2]),
                 reads=[xk, 'sd1' + sfx, 'sd2' + sfx], writes=[xnk])

        eps_t = top.enter_context(nc.sbuf_tensor("eps_t", [128, 1], F32))
        P.op('pool', lambda g: g.memset(eps_t[:], LN_EPS), writes=['eps'])

        def B(bk):
            return ['B%d' % bk]

        def ln_T_tile(ph_bufs, src_rows, dst, dkey, col0, ms, fdst=None, bo=0, xt_in=None, xk_in=None):
            xin, xn, st, mv, sd, cnt = ph_bufs
            xt = xin[cnt[0] % 2]
            xk = 'xin%d' % (cnt[0] % 2)
            cnt[0] += 1
            if src_rows is not None:
                P.dma('act', xt[:], src_rows, writes=[xk])
            else:
                xt, xk = xt_in, xk_in
            layer_norm_tile(xt, xk, xn, 'xn', st, mv, sd, eps_t)
            transpose_evac(xn, 'xn', dst, dkey, col0, ms, fdst, bo)

        def transpose_evac(xn, xnk, dst, dkey, col0, ms, fdst=None, bo=0):
            for kc in range(NKC):
                bk = bo + kc // 4
                P.op('pe', lambda pe, kc=kc, bk=bk: pe.transpose(
                    banks[bk][:, (kc % 4) * 128:(kc % 4 + 1) * 128], xn[:, kc * 128:(kc + 1) * 128], ident[:]),
                    reads=[xnk, 'ident'], writes=[('bq', bk, kc % 4)], excl=B(bk))
            for kc in range(NKC):
                bk = bo + kc // 4
                q = kc % 4
                if bk - bo < 2:
                    P.op('dve', lambda v, kc=kc, bk=bk, q=q: v.tensor_scalar(
                        out=dst[:, kc, col0:col0 + 128], in0=banks[bk][:, q * 128:(q + 1) * 128],
                        scalar1=mfm[:, ms + 1, kc:kc + 1], scalar2=mfm[:, ms, kc:kc + 1],
                        op0=ALU.mult, op1=ALU.add),
                        reads=[('bq', bk, q), 'mfm'], writes=[(dkey, kc, col0)], excl=B(bk))
                else:
                    P.op('act', lambda a, kc=kc, bk=bk, q=q: a.activation(
                        out=dst[:, kc, col0:col0 + 128], in_=banks[bk][:, q * 128:(q + 1) * 128],
                        func=AF.Identity, scale=mfm[:, ms + 1, kc:kc + 1], bias=mfm[:, ms, kc:kc + 1]),
                        reads=[('bq', bk, q), 'mfm'], writes=[(dkey, kc, col0)], excl=B(bk))
                if fdst is not None:
                    P.op('dve', lambda v, kc=kc, bk=bk, q=q: v.tensor_scalar(
                        out=fdst[:, kc, :], in0=banks[bk][:, q * 128:(q + 1) * 128],
                        scalar1=mfm[:, ms + 1, kc:kc + 1], scalar2=mfm[:, ms, kc:kc + 1],
                        op0=ALU.mult, op1=ALU.add),
                        reads=[('bq', bk, q), 'mfm'], writes=[('fdst', kc)], excl=B(bk))

        def ln_bufs(ph):
            xin = [sb(ph, "xin%d" % i, [128, 2048], F32) for i in range(2)]
            xn = sb(ph, "xn", [128, 2048], F32)
            st = sb(ph, "st", [128, 4, 6], F32)
            mv = sb(ph, "mv", [128, 2], F32)
            sd = sb(ph, "sd", [128, 4], F32)
            return (xin, xn, st, mv, sd, [0])

        evc = [0]

        def evac_copy(dst_ap, bk, wkeys, src=None):
            src = banks[bk][:, :] if src is None else src
            evc[0] += 1
            if evc[0] % 2 == 0:
                P.op('dve', lambda v: v.tensor_copy(out=dst_ap, in_=src), reads=['b%d' % bk], writes=wkeys, excl=B(bk))
            else:
                P.op('act', lambda a: a.copy(out=dst_ap, in_=src), reads=['b%d' % bk], writes=wkeys, excl=B(bk))

        if upto >= 1:
            with contextlib.ExitStack() as ph:
                Wkv = sb(ph, "Wkv", [128, NKC, 2048], BF16)
                wst = [sb(ph, "wstb%d" % i, [128, 2048], F32) for i in range(2)]
                lb = ln_bufs(ph)
                hT = [sb(ph, "hT%d" % i, [128, NKC, 512], BF16) for i in range(2)]
                kst = [sb(ph, "kst%d" % i, [128, 8, 512], BF16) for i in range(2)]
                vst = [sb(ph, "vst%d" % i, [128, 1024], BF16) for i in range(2)]
                for kc in range(NKC):
                    w = wst[kc % 2]
                    wk = 'wstb%d' % (kc % 2)
                    P.dma('sp', w[:], w_in[kc * 128:(kc + 1) * 128, 1024:3072], writes=[wk])
                    P.op('pool', lambda g, w=w, kc=kc: g.tensor_copy(out=Wkv[:, kc, :], in_=w[:]),
                         reads=[wk], writes=[('Wkv', kc)])
                nkc = [0]

                def kv_units(gc):
                    hb = hT[gc % 2]
                    hk = 'hT%d' % (gc % 2)
                    ks = kst[gc % 2]
                    kk = 'kst%d' % (gc % 2)

                    def unit(k):
                        for h in (2 * k, 2 * k + 1):
                            bk = 4 + (nkc[0] % 4)
                            nkc[0] += 1
                            for kc in range(NKC):
                                P.op('pe', lambda pe, kc=kc, h=h, bk=bk: pe.matmul(
                                    banks[bk][:, :], lhsT=Wkv[:, kc, h * 128:(h + 1) * 128], rhs=hb[:, kc, :],
                                    start=(kc == 0), stop=(kc == NKC - 1)),
                                    reads=[('Wkv', kc)] + [(hk, kc, t4 * 128) for t4 in range(4)],
                                    writes=['b%d' % bk], excl=B(bk))
                            evac_copy(ks[:, h, :], bk, [(kk, h)])
                        if k == 3:
                            P.dma('sp', KT[:, :, gc * 512:(gc + 1) * 512].rearrange("h p t -> p h t"), ks[:],
                                  reads=[(kk, h) for h in range(8)], writes=[('KT', gc)])
                        tt = k
                        vs = vst[(gc * 4 + tt) % 2]
                        vk = 'vst%d' % ((gc * 4 + tt) % 2)
                        for half in range(2):
                            bk = 4 + (nkc[0] % 4)
                            nkc[0] += 1
                            for kc in range(NKC):
                                P.op('pe', lambda pe, kc=kc, half=half, bk=bk: pe.matmul(
                                    banks[bk][:, :], lhsT=hb[:, kc, tt * 128:(tt + 1) * 128],
                                    rhs=Wkv[:, kc, 1024 + half * 512:1024 + (half + 1) * 512],
                                    start=(kc == 0), stop=(kc == NKC - 1)),
                                    reads=[('Wkv', kc), (hk, kc, tt * 128)], writes=['b%d' % bk], excl=B(bk))
                            evac_copy(vs[:, half * 512:(half + 1) * 512], bk, [(vk, half)])
                        P.dma('sp', VV[gc * 4 + tt], vs[:], reads=[(vk, 0), (vk, 1)], writes=[('VV', gc * 4 + tt)])
                    return [lambda k=k: unit(k) for k in range(4)]

                xin_, xn_, st_, mv_, sd_, _c = lb
                xnB = [xn_, sb(ph, "xnB", [128, 2048], F32)]
                stB = [st_, sb(ph, "stB", [128, 4, 6], F32)]
                mvB = [mv_, sb(ph, "mvB", [128, 2], F32)]
                sdB = [sd_, sb(ph, "sdB", [128, 4], F32)]

                def S1(T):
                    p = T % 2
                    xk = 'xin%d' % p
                    P.dma('act', xin_[p][:], xb[T * 128:(T + 1) * 128, :], writes=[xk])
                    layer_norm_tile(xin_[p], xk, xnB[p], 'xn%d' % p, stB[p], mvB[p], sdB[p], eps_t, sfx='_%d' % p)

                def S2(T):
                    gc, tt = T // 4, T % 4
                    transpose_evac(xnB[T % 2], 'xn%d' % (T % 2), hT[gc % 2], 'hT%d' % (gc % 2), tt * 128, 0)

                pending = []
                S1(0)
                for gc in range(16):
                    for tt in range(4):
                        T = gc * 4 + tt
                        if T + 1 < 64:
                            S1(T + 1)
                        S2(T)
                        if pending:
                            pending[tt]()
                    pending = kv_units(gc)
                for u in pending:
                    u()
                P.barrier()
                P.flush()

        if upto >= 2:
            with contextlib.ExitStack() as ph:
                hO = sb(ph, "hO", [128, NKC, OWN], BF16)
                lb = ln_bufs(ph)
                wsh = [sb(ph, "wsh%d" % i, [128, 8, 512], F32) for i in range(2)]
                wbf = [sb(ph, "wbf%d" % i, [128, NKC, 512], BF16) for i in range(2)]
                ost = [sb(ph, "ost%d" % i, [128, 4, 512], BF16) for i in range(2)]
                for t in range(16):
                    ln_T_tile(lb, xo[t * 128:(t + 1) * 128, :], hO, 'hO', t * 128, 0)
                hO_keys = lambda kc, c0, n: [('hO', kc, c0 + 128 * q) for q in range(n)]
                nld = [0]

                def load_wblock(src2d, c0, wb, wbk, nkc=NKC):
                    for half in range(nkc // 8):
                        w = wsh[nld[0] % 2]
                        wk = 'wsh%d' % (nld[0] % 2)
                        nld[0] += 1
                        P.dma('sp', w[:], src2d[half * 1024:(half + 1) * 1024, c0:c0 + 512].rearrange(
                            "(c p) n -> p c n", p=128), writes=[wk])
                        P.op('pool', lambda g, w=w, half=half, wb=wb: g.tensor_copy(
                            out=wb[:, half * 8:(half + 1) * 8, :], in_=w[:]),
                            reads=[wk], writes=[(wbk, half)])

                blocks = []
                for i in range(2):
                    blocks.append((i * 512, 'copy', QTd, i * 4))
                for i in range(2):
                    blocks.append((3072 + i * 512, 'gelu', obTd, i * 4))
                for i in range(4):
                    blocks.append((5120 + i * 512, 'sig', gaTd, i * 4))
                for i in range(4):
                    blocks.append((7168 + i * 512, 'sig', gbTd, i * 4))
                nb = 0
                no = 0
                nbank = 0
                for (c0, fn, dstd, ch0) in blocks:
                    wb = wbf[nb % 2]
                    wbk = 'wbf%d' % (nb % 2)
                    nb += 1
                    load_wblock(w_in, c0, wb, wbk)
                    for oc in range(4):
                        os_ = ost[no % 2]
                        ok = 'ost%d' % (no % 2)
                        no += 1
                        for sub in range(4):
                            bk = 4 + (nbank % 4)
                            nbank += 1
                            for kc in range(NKC):
                                P.op('pe', lambda pe, kc=kc, sub=sub, bk=bk, wb=wb, oc=oc: pe.matmul(
                                    banks[bk][:, :], lhsT=wb[:, kc, sub * 128:(sub + 1) * 128],
                                    rhs=hO[:, kc, oc * 512:(oc + 1) * 512],
                                    start=(kc == 0), stop=(kc == NKC - 1)),
                                    reads=[(wbk, kc // 8)] + hO_keys(kc, oc * 512, 4), writes=['b%d' % bk], excl=B(bk))
                            if fn == 'copy':
                                evac_copy(os_[:, sub, :], bk, [(ok, sub)])
                            else:
                                f = AF.Gelu if fn == 'gelu' else AF.Sigmoid
                                P.op('act', lambda a, sub=sub, bk=bk, os_=os_, f=f: a.activation(
                                    out=os_[:, sub, :], in_=banks[bk][:, :], func=f),
                                    reads=['b%d' % bk], writes=[(ok, sub)], excl=B(bk))
                        P.dma('sp', dstd[ch0:ch0 + 4, :, oc * 512:(oc + 1) * 512].rearrange("c p t -> p c t"), os_[:],
                              reads=[(ok, q) for q in range(4)], writes=[(dstd.tensor.name, ch0, oc)])
                load_wblock(w_in, 4096, wbf[0], 'wbf0')
                load_wblock(w_in, 4608, wbf[1], 'wbf1')
                wsf = sb(ph, "wsf", [128, 8, 128], F32)
                wsb = sb(ph, "wsb", [128, 8, 128], BF16)
                bsb = sb(ph, "bsb", [128, 1024], F32)
                lnG = sb(ph, "lnG", [128, 1024], F32)
                lnB = sb(ph, "lnB", [128, 1024], F32)
                gv = sb(ph, "gv", [128, 1024], F32)
                vn0 = sb(ph, "vn0", [128, 1024], F32)
                vnb = sb(ph, "vnb", [128, 1024], BF16)
                uT = [sb(ph, "uT%d" % i, [128, 8, 128], BF16) for i in range(2)]
                tmpm = sb(ph, "tmpm", [128, 1024], F32)
                obs = [sb(ph, "obs%d" % i, [128, 8, 128], BF16) for i in range(2)]
                st2 = sb(ph, "st2", [128, 2, 6], F32)
                mv2 = sb(ph, "mv2", [128, 2], F32)
                sd2 = sb(ph, "sd2", [128, 4], F32)
                P.dma('sp', wsf[:], sgu_wT, writes=['wsf'])
                P.op('pool', lambda g: g.memset(wsf[64:128, :, 0:64], 0.0), reads=['wsf'], writes=['wsf'])
                P.op('pool', lambda g: g.tensor_copy(out=wsb[:], in_=wsf[:]), reads=['wsf'], writes=['wsb'])
                P.dma('sp', bsb[:], sgu_b.partition_broadcast(128).rearrange("p o f -> p (o f)"), writes=['bsb'])
                P.dma('sp', lnG[:], sgu_ln_g.partition_broadcast(128).rearrange("p o f -> p (o f)"), writes=['lnG'])
                P.dma('sp', lnB[:], sgu_ln_b.partition_broadcast(128).rearrange("p o f -> p (o f)"), writes=['lnB'])
                for t in range(16):
                    u = uT[t % 2]
                    uk = 'uT%d' % (t % 2)
                    ob = obs[t % 2]
                    obk = 'obs%d' % (t % 2)
                    P.dma('act', u[:], obTd[:, :, t * 128:(t + 1) * 128].rearrange("c p t -> p c t"),
                          reads=[('obTd', 0, t // 4), ('obTd', 4, t // 4)], writes=[uk])
                    for half in range(2):
                        bk = half
                        for kc in range(NKC):
                            P.op('pe', lambda pe, kc=kc, half=half, bk=bk, t=t: pe.matmul(
                                banks[bk][:, :], lhsT=hO[:, kc, t * 128:(t + 1) * 128], rhs=wbf[half][:, kc, :],
                                start=(kc == 0), stop=(kc == NKC - 1)),
                                reads=[('wbf%d' % half, kc // 8), ('hO', kc, t * 128)], writes=['b%d' % bk], excl=B(bk))
                        P.op('act', lambda a, half=half, bk=bk: a.activation(
                            out=gv[:, half * 512:(half + 1) * 512], in_=banks[bk][:, :], func=AF.Gelu),
                            reads=['b%d' % bk], writes=[('gv', half)], excl=B(bk))
                        P.op('dve', lambda v, half=half: v.bn_stats(out=st2[:, half, :], in_=gv[:, half * 512:(half + 1) * 512]),
                             reads=[('gv', half)], writes=[('st2', half)])
                    P.op('dve', lambda v: v.bn_aggr(out=mv2[:], in_=st2[:].rearrange("p a b -> p (a b)")),
                         reads=[('st2', 0), ('st2', 1)], writes=['mv2'])
                    P.op('act', lambda a: a.activation(out=sd2[:, 0:1], in_=mv2[:, 1:2], func=AF.Sqrt, bias=eps_t[:, 0:1], scale=1.0),
                         reads=['mv2', 'eps'], writes=['sd20'])
                    P.op('dve', lambda v: v.reciprocal(out=sd2[:, 1:2], in_=sd2[:, 0:1]), reads=['sd20'], writes=['sd21'])
                    P.op('dve', lambda v: v.tensor_scalar(out=sd2[:, 2:3], in0=mv2[:, 0:1], scalar1=sd2[:, 1:2], scalar2=-1.0,
                                                          op0=ALU.mult, op1=ALU.mult), reads=['mv2', 'sd21'], writes=['sd22'])
                    P.op('act', lambda a: a.activation(out=vn0[:], in_=gv[:], func=AF.Identity, bias=sd2[:, 2:3], scale=sd2[:, 1:2]),
                         reads=[('gv', 0), ('gv', 1), 'sd21', 'sd22'], writes=['vn0'])
                    P.op('dve', lambda v: v.tensor_tensor(out=vn0[:], in0=vn0[:], in1=lnG[:], op=ALU.mult),
                         reads=['vn0', 'lnG'], writes=['vn0'])
                    P.op('dve', lambda v: v.tensor_tensor(out=vnb[:], in0=vn0[:], in1=lnB[:], op=ALU.add),
                         reads=['vn0', 'lnB'], writes=['vnb'])
                    for g in range(8):
                        bk = 2 + g // 4
                        P.op('pe', lambda pe, g=g, bk=bk: pe.matmul(
                            banks[bk][:, (g % 4) * 128:(g % 4 + 1) * 128], lhsT=vnb[:, g * 128:(g + 1) * 128],
                            rhs=wsb[:, g, :], start=True, stop=True),
                            reads=['vnb', 'wsb'], writes=[('bq', bk, g % 4)], excl=B(bk))
                    for hh in range(2):
                        bk = 2 + hh
                        P.op('dve', lambda v, hh=hh, bk=bk: v.tensor_tensor(
                            out=tmpm[:, hh * 512:(hh + 1) * 512], in0=banks[bk][:, :], in1=bsb[:, hh * 512:(hh + 1) * 512],
                            op=ALU.add), reads=[('bq', bk, q) for q in range(4)] + ['bsb'], writes=[('tmpm', hh)], excl=B(bk))
                    P.op('pool', lambda gp, u=u, ob=ob: gp.tensor_tensor(
                        out=ob[:].rearrange("p c t -> p (c t)"), in0=tmpm[:], in1=u[:].rearrange("p c t -> p (c t)"), op=ALU.mult),
                        reads=[('tmpm', 0), ('tmpm', 1), uk], writes=[obk])
                    P.dma('sp', obTd[:, :, t * 128:(t + 1) * 128].rearrange("c p t -> p c t"), ob[:],
                          reads=[obk], writes=[('obTd2', t)])
                P.barrier()
                P.flush()

        if upto >= 3:
            with contextlib.ExitStack() as ph:
                KTh = sb(ph, "KTh", [128, 4, S], BF16)
                Vh = sb(ph, "Vh", [128, 64, 512], BF16)
                QTh = sb(ph, "QTh", [128, 4, OWN], BF16)
                mkf = sb(ph, "mkf", [128, 4, 128], F32)
                mk4 = sb(ph, "mk4", [128, 4, 4, 128], BF16)
                triI = sb(ph, "triI", [128, 128], BF16)
                triC = sb(ph, "triC", [128, 128], BF16)
                trif = sb(ph, "trif", [128, 128], F32)
                NS_ = 2
                eb = [[sb(ph, "eb%d_%d" % (q, i), [128, 512], F32) for i in range(2)] for q in range(NS_)]
                spb = [[sb(ph, "spb%d_%d" % (q, i), [128, 512], BF16) for i in range(2)] for q in range(NS_)]
                gb_ = [[sb(ph, "gb%d_%d" % (q, i), [128, 512], F32) for i in range(2)] for q in range(NS_)]
                wb_ = [[sb(ph, "wb%d_%d" % (q, i), [128, 512], BF16) for i in range(2)] for q in range(NS_)]
                w32 = [sb(ph, "w32_%d" % q, [128, 512], F32) for q in range(NS_)]
                oas = [[sb(ph, "oas%d_%d" % (q, i), [128, 4, 128], BF16) for i in range(2)] for q in range(NS_)]
                P.dma('sp', mkf[:], maskd, writes=['mkf'])
                for h in range(4):
                    P.op('pool', lambda g, h=h: g.tensor_copy(out=mk4[:, :, h, :], in_=mkf[:]), reads=['mkf'], writes=[('mk4', h)])
                mk4k = [('mk4', h) for h in range(4)]
                P.op('pool', lambda g: g.memset(trif[:], 1.0), writes=['trif'])
                P.op('pool', lambda g: g.affine_select(out=trif[:], in_=trif[:], pattern=[[-1, 128]], compare_op=ALU.is_ge,
                                                       fill=0.0, base=0, channel_multiplier=1), reads=['trif'], writes=['trif'])
                P.op('pool', lambda g: g.tensor_copy(out=triI[:], in_=trif[:]), reads=['trif'], writes=['triI'])
                P.op('pool', lambda g: g.memset(trif[:], 1.0), reads=['trif'], writes=['trif'])
                P.op('pool', lambda g: g.affine_select(out=trif[:], in_=trif[:], pattern=[[1, 128]], compare_op=ALU.is_gt,
                                                       fill=0.0, base=0, channel_multiplier=-1), reads=['trif'], writes=['trif'])
                P.op('pool', lambda g: g.tensor_copy(out=triC[:], in_=trif[:]), reads=['trif'], writes=['triC'])
                sc = 128.0 ** -0.5
                for hg in range(2):
                    for h in range(4):
                        P.dma('sp', KTh[:, h, :], KT[hg * 4 + h], writes=[('KTh', h)])
                    for q4 in range(4):
                        P.dma('act', Vh[:, q4 * 16:(q4 + 1) * 16, :],
                              VV[q4 * 16:(q4 + 1) * 16, :, hg * 512:(hg + 1) * 512].rearrange("b p f -> p b f"),
                              writes=[('Vh', q4)])
                    P.dma('sp', QTh[:], QTd[hg * 4:(hg + 1) * 4].rearrange("c p t -> p c t"), writes=['QTh'])
                    streams = [[(i, kb) for i in range(q, 16, NS_) for kb in range(4 * i + 3, -1, -1)] for q in range(NS_)]

                    def stageA(q, n):
                        i, kb = streams[q][n]
                        zb = 2 * q + (n % 2)
                        p = n % 2
                        e = eb[q][p]
                        sp_ = spb[q][p]
                        ek = 'e%d_%d' % (q, p)
                        sk = 'sp%d_%d' % (q, p)
                        for h in range(4):
                            P.op('pe', lambda pe, h=h, i=i, kb=kb, zb=zb: pe.matmul(
                                banks[zb][:, h * 128:(h + 1) * 128], lhsT=KTh[:, h, kb * 128:(kb + 1) * 128],
                                rhs=QTh[:, h, i * 128:(i + 1) * 128], start=True, stop=True),
                                reads=[('KTh', h), 'QTh'], writes=['b%d' % zb], excl=B(zb))
                        P.op('act', lambda a, zb=zb, e=e: a.activation(out=e[:], in_=banks[zb][:, :], func=AF.Exp, scale=sc),
                             reads=['b%d' % zb], writes=[ek], excl=B(zb))
                        P.op('act', lambda a, e=e, sp_=sp_: a.activation(out=sp_[:], in_=e[:], func=AF.Ln, bias=1.0, scale=1.0),
                             reads=[ek], writes=[sk])
                        if kb >= 4 * i:
                            m = kb - 4 * i
                            P.op('pool', lambda g, sp_=sp_, m=m: g.tensor_tensor(
                                out=sp_[:], in0=sp_[:], in1=mk4[:, m, :, :].rearrange("p h t -> p (h t)"), op=ALU.mult),
                                reads=[sk] + mk4k, writes=[sk])

                    def stageB(q, n, part):
                        i, kb = streams[q][n]
                        p = n % 2
                        e = eb[q][p]
                        sp_ = spb[q][p]
                        g_ = gb_[q][p]
                        w_ = wb_[q][p]
                        ek = 'e%d_%d' % (q, p)
                        sk = 'sp%d_%d' % (q, p)
                        gk = 'g%d_%d' % (q, p)
                        wk = 'w%d_%d' % (q, p)
                        btb = 4 + q
                        otb = 6 + q
                        first = (kb == 4 * i + 3)
                        last = (kb == 0)
                        if part == 1:
                            P.op('pe', lambda pe: pe.matmul(banks[btb][:, :], lhsT=triI[:], rhs=sp_[:], start=first, stop=False),
                                 reads=['triI', sk], writes=['b%d' % btb], excl=B(btb))
                            P.op('act', lambda a: a.activation(out=g_[:], in_=banks[btb][:, :], func=AF.Exp, scale=-1.0),
                                 reads=['b%d' % btb], writes=[gk], excl=B(btb))
                            return
                        P.op('pe', lambda pe: pe.matmul(banks[btb][:, :], lhsT=triC[:], rhs=sp_[:], start=False, stop=last),
                             reads=['triC', sk], writes=['b%d' % btb], excl=B(btb))
                        if kb >= 4 * i:
                            m = kb - 4 * i
                            P.op('dve', lambda v: v.tensor_tensor(out=w32[q][:], in0=e[:], in1=g_[:], op=ALU.mult),
                                 reads=[ek, gk], writes=['w32_%d' % q])
                            P.op('dve', lambda v: v.tensor_tensor(
                                out=w_[:], in0=w32[q][:], in1=mk4[:, m, :, :].rearrange("p h t -> p (h t)"), op=ALU.mult),
                                reads=['w32_%d' % q] + mk4k, writes=[wk])
                        else:
                            P.op('dve', lambda v: v.tensor_tensor(out=w_[:], in0=e[:], in1=g_[:], op=ALU.mult),
                                 reads=[ek, gk], writes=[wk])
                        for h in range(4):
                            P.op('pe', lambda pe, h=h: pe.matmul(
                                banks[otb][:, h * 128:(h + 1) * 128], lhsT=Vh[:, kb, h * 128:(h + 1) * 128],
                                rhs=w_[:, h * 128:(h + 1) * 128], start=(first and h == 0), stop=last),
                                reads=[('Vh', kb // 16), wk], writes=['b%d' % otb], excl=B(otb))
                        if last:
                            oa = oas[q][(i // NS_) % 2]
                            ok_ = 'oas%d_%d' % (q, (i // NS_) % 2)
                            evac_copy(oa[:].rearrange("p c t -> p (c t)"), otb, [ok_])
                            P.dma('sp', oaTd[hg * 4:(hg + 1) * 4, :, i * 128:(i + 1) * 128].rearrange("c p t -> p c t"), oa[:],
                                  reads=[ok_], writes=[('oaTd', hg, i)])

                    lens = [len(st_) for st_ in streams]
                    for q in range(NS_):
                        stageA(q, 0)
                    for n in range(max(lens)):
                        for q in range(NS_):
                            if n < lens[q]:
                                stageB(q, n, 1)
                        for q in range(NS_):
                            if n + 1 < lens[q]:
                                stageA(q, n + 1)
                        for q in range(NS_):
                            if n < lens[q]:
                                stageB(q, n, 2)
                P.barrier()
                P.flush()
        mTd = dscr("mTd", [NKC, 128, OWN], BF16)
        y2d = dscr("y2d", [OWN, D], F32) if debug else None
        if debug:
            dbg_wdh = dscr("dbg_wdh", [2, 128, 8 * NEXP], F32)
            dbg_hact = dscr("dbg_hact", [2, 2, 128, 4 * 512], BF16)
            dbg_h2h = dscr("dbg_h2h", [2, 128, NKC * 1024], BF16)
            dbg_y0 = dscr("dbg_y0", [2, 128, 8 * D], F32)
            dbg_pad = dscr("dbg_pad", [2, 128, 1024], F32)

        def bcast_load(ph, name, src_row):
            t = sb(ph, name, [128, src_row.shape[-1]], F32)
            P.dma('sp', t[:], src_row.partition_broadcast(128).rearrange("p o f -> p (o f)"), writes=[name])
            return t

        if upto >= 4:
            with contextlib.ExitStack() as ph:
                oaT = sb(ph, "oaT", [128, 8, OWN], BF16)
                obT = sb(ph, "obT", [128, 8, OWN], BF16)
                P.dma('sp', oaT[:], oaTd.rearrange("c p t -> p c t"), writes=['oaT'])
                P.dma('act', obT[:], obTd.rearrange("c p t -> p c t"), writes=['obT'])
                was = [sb(ph, "was%d" % i, [128, 8, 128], F32) for i in range(2)]
                wbs = [sb(ph, "wbs%d" % i, [128, 8, 128], F32) for i in range(2)]
                wab = [sb(ph, "wab%d" % i, [128, 8, 128], BF16) for i in range(2)]
                wbb = [sb(ph, "wbb%d" % i, [128, 8, 128], BF16) for i in range(2)]
                gas = [sb(ph, "gas%d" % i, [128, OWN], BF16) for i in range(2)]
                gbs = [sb(ph, "gbs%d" % i, [128, OWN], BF16) for i in range(2)]
                t1 = [sb(ph, "t1%d" % i, [128, 512], F32) for i in range(2)]
                t2 = [sb(ph, "t2%d" % i, [128, 512], F32) for i in range(2)]
                mst = [sb(ph, "mst%d" % i, [128, OWN], BF16) for i in range(2)]
                nn = 0
                for nb in range(16):
                    p2 = nb % 2
                    P.dma('sp', was[p2][:], w_pa[:, nb * 128:(nb + 1) * 128].rearrange("(c p) n -> p c n", p=128), writes=['was%d' % p2])
                    P.dma('sp', wbs[p2][:], w_pb[:, nb * 128:(nb + 1) * 128].rearrange("(c p) n -> p c n", p=128), writes=['wbs%d' % p2])
                    P.op('pool', lambda g, p2=p2: g.tensor_copy(out=wab[p2][:], in_=was[p2][:]), reads=['was%d' % p2], writes=['wab%d' % p2])
                    P.op('pool', lambda g, p2=p2: g.tensor_copy(out=wbb[p2][:], in_=wbs[p2][:]), reads=['wbs%d' % p2], writes=['wbb%d' % p2])
                    P.dma('act', gas[p2][:], gaTd[nb], writes=['gas%d' % p2])
                    P.dma('act', gbs[p2][:], gbTd[nb], writes=['gbs%d' % p2])
                    for oc in range(4):
                        q2 = nn % 2
                        nn += 1
                        ba = 0 + q2
                        bb = 2 + q2
                        for kc in range(8):
                            P.op('pe', lambda pe, kc=kc, p2=p2, oc=oc, ba=ba: pe.matmul(
                                banks[ba][:, :], lhsT=wab[p2][:, kc, :], rhs=oaT[:, kc, oc * 512:(oc + 1) * 512],
                                start=(kc == 0), stop=(kc == 7)), reads=['wab%d' % p2, 'oaT'], writes=['b%d' % ba], excl=B(ba))
                        for kc in range(8):
                            P.op('pe', lambda pe, kc=kc, p2=p2, oc=oc, bb=bb: pe.matmul(
                                banks[bb][:, :], lhsT=wbb[p2][:, kc, :], rhs=obT[:, kc, oc * 512:(oc + 1) * 512],
                                start=(kc == 0), stop=(kc == 7)), reads=['wbb%d' % p2, 'obT'], writes=['b%d' % bb], excl=B(bb))
                        P.op('dve', lambda v, p2=p2, oc=oc, ba=ba, q2=q2: v.tensor_tensor(
                            out=t1[q2][:], in0=banks[ba][:, :], in1=gas[p2][:, oc * 512:(oc + 1) * 512], op=ALU.mult),
                            reads=['b%d' % ba, 'gas%d' % p2], writes=['t1%d' % q2], excl=B(ba))
                        P.op('dve', lambda v, p2=p2, oc=oc, bb=bb, q2=q2: v.tensor_tensor(
                            out=t2[q2][:], in0=banks[bb][:, :], in1=gbs[p2][:, oc * 512:(oc + 1) * 512], op=ALU.mult),
                            reads=['b%d' % bb, 'gbs%d' % p2], writes=['t2%d' % q2], excl=B(bb))
                        P.op('pool', lambda g, p2=p2, oc=oc, q2=q2: g.tensor_tensor(
                            out=mst[p2][:, oc * 512:(oc + 1) * 512], in0=t1[q2][:], in1=t2[q2][:], op=ALU.add),
                            reads=['t1%d' % q2, 't2%d' % q2], writes=[('mst%d' % p2, oc)])
                    P.dma('sp', mTd[nb], mst[p2][:], reads=[('mst%d' % p2, oc) for oc in range(4)], writes=[('mTd', nb)])
                P.barrier()
                P.flush()

        if upto >= 5:
            with contextlib.ExitStack() as ph:
                Wo = sb(ph, "Wo", [128, NKC, D], BF16)
                wst = [sb(ph, "wsto%d" % i, [128, 2048], F32) for i in range(2)]
                for kc in range(NKC):
                    w = wst[kc % 2]
                    wk = 'wsto%d' % (kc % 2)
                    P.dma('sp', w[:], w_out[kc * 128:(kc + 1) * 128, :], writes=[wk])
                    P.op('pool', lambda g, w=w, kc=kc: g.tensor_copy(out=Wo[:, kc, :], in_=w[:]), reads=[wk], writes=[('Wo', kc)])
                G1 = bcast_load(ph, "G1", modv[0:1, 2 * D:3 * D])
                L1g = bcast_load(ph, "L1g", ln1_g)
                L1b = bcast_load(ph, "L1b", ln1_b)
                brt = bcast_load(ph, "brt", b_rt)
                Wrt = sb(ph, "Wrt", [128, NKC, 36], F32)
                P.dma('sp', Wrt[:], w_rt.rearrange("(c p) n -> p c n", p=128), writes=['Wrt'])
                lb = ln_bufs(ph)
                xin, xn, st, mv, sd, cnt = lb
                mts = [sb(ph, "mts%d" % i, [128, NKC, 128], BF16) for i in range(2)]
                tg = sb(ph, "tg", [128, D], F32)
                rt = sb(ph, "rt", [128, D], F32)
                x1t = sb(ph, "x1t", [128, D], F32)
                h2t = [sb(ph, "h2t%d" % i, [128, NKC, 128], BF16) for i in range(2)]
                h2f = sb(ph, "h2f", [128, NKC, 128], F32)
                lg = sb(ph, "lg", [128, 36], F32)
                rs = sb(ph, "rs", [128, 64], F32)
                wdt = [sb(ph, "wdt%d" % i, [128, 32], F32) for i in range(2)]
                for t in range(16):
                    p2 = t % 2
                    mt = mts[p2]
                    P.dma('sp', mt[:], mTd[:, :, t * 128:(t + 1) * 128].rearrange("c p t -> p c t"), writes=['mts%d' % p2])
                    xt = xin[t % 2]
                    xk = 'xin%d' % (t % 2)
                    P.dma('act', xt[:], xo[t * 128:(t + 1) * 128, :], writes=[xk])
                    for fb in range(4):
                        for kc in range(NKC):
                            P.op('pe', lambda pe, kc=kc, fb=fb, mt=mt: pe.matmul(
                                banks[fb][:, :], lhsT=mt[:, kc, :], rhs=Wo[:, kc, fb * 512:(fb + 1) * 512],
                                start=(kc == 0), stop=(kc == NKC - 1)),
                                reads=['mts%d' % p2, ('Wo', kc)], writes=['b%d' % fb], excl=B(fb))
                        P.op('dve', lambda v, fb=fb: v.tensor_tensor(
                            out=tg[:, fb * 512:(fb + 1) * 512], in0=banks[fb][:, :], in1=G1[:, fb * 512:(fb + 1) * 512], op=ALU.mult),
                            reads=['b%d' % fb, 'G1'], writes=[('tg', fb)], excl=B(fb))
                    P.op('dve', lambda v, xt=xt: v.scalar_tensor_tensor(out=rt[:], in0=xt[:], scalar=ALPHA, in1=tg[:],
                                                                       op0=ALU.mult, op1=ALU.add),
                         reads=[xk] + [('tg', fb) for fb in range(4)], writes=['rt'])
                    layer_norm_tile(rt, 'rt', xn, 'xn', st, mv, sd, eps_t)
                    P.op('dve', lambda v: v.tensor_tensor(out=xn[:], in0=xn[:], in1=L1g[:], op=ALU.mult), reads=['xn', 'L1g'], writes=['xn'])
                    P.op('pool', lambda g: g.tensor_tensor(out=x1t[:], in0=xn[:], in1=L1b[:], op=ALU.add), reads=['xn', 'L1b'], writes=['x1t'])
                    P.dma('sp', x1d[t * 128:(t + 1) * 128, :], x1t[:], reads=['x1t'], writes=[('x1d', t)])
                    h2 = h2t[p2]
                    ln_T_tile(lb, None, h2, 'h2t%d' % p2, 0, 3, fdst=h2f, bo=4, xt_in=x1t, xk_in='x1t')
                    P.dma('sp', h2Td[:, :, t * 128:(t + 1) * 128].rearrange("c p t -> p c t"), h2[:],
                          reads=[('h2t%d' % p2, kc, 0) for kc in range(NKC)], writes=[('h2Td', t)])
                    for kc in range(NKC):
                        P.op('pe', lambda pe, kc=kc: pe.matmul(banks[0][:, 0:36], lhsT=h2f[:, kc, :], rhs=Wrt[:, kc, :],
                                                                start=(kc == 0), stop=(kc == NKC - 1)),
                             reads=[('fdst', kc), 'Wrt'], writes=['b0'], excl=B(0))
                    P.op('dve', lambda v: v.tensor_tensor(out=lg[:], in0=banks[0][:, 0:36], in1=brt[:], op=ALU.add),
                         reads=['b0', 'brt'], writes=['lg'], excl=B(0))
                    wd_ = wdt[p2]
                    V = lambda f, r, w: P.op('dve', f, reads=r, writes=w)
                    V(lambda v: v.reduce_max(out=rs[:, 0:1], in_=lg[:, 0:4], axis=mybir.AxisListType.X), ['lg'], ['r0'])
                    V(lambda v: v.tensor_scalar(out=rs[:, 1:2], in0=rs[:, 0:1], scalar1=-1.0, scalar2=None, op0=ALU.mult), ['r0'], ['r1'])
                    P.op('act', lambda a: a.activation(out=rs[:, 4:8], in_=lg[:, 0:4], func=AF.Exp, bias=rs[:, 1:2], scale=1.0,
                                                       accum_out=rs[:, 2:3]), reads=['lg', 'r1'], writes=['r2'])
                    V(lambda v: v.reciprocal(out=rs[:, 3:4], in_=rs[:, 2:3]), ['r2'], ['r3'])
                    V(lambda v: v.tensor_scalar(out=rs[:, 8:12], in0=lg[:, 0:4], scalar1=rs[:, 0:1], scalar2=None, op0=ALU.is_equal),
                      ['lg', 'r0'], ['gm'])
                    V(lambda v: v.tensor_scalar(out=rs[:, 12:20], in0=lg[:, 4:12], scalar1=rs[:, 8:9], scalar2=None, op0=ALU.mult),
                      ['lg', 'gm'], ['sel'])
                    for g in range(1, 4):
                        V(lambda v, g=g: v.scalar_tensor_tensor(out=rs[:, 12:20], in0=lg[:, 4 + 8 * g:12 + 8 * g], scalar=rs[:, 8 + g:9 + g],
                                                                in1=rs[:, 12:20], op0=ALU.mult, op1=ALU.add), ['lg', 'gm', 'sel'], ['sel'])
                    V(lambda v: v.max(out=rs[:, 20:28], in_=rs[:, 12:20]), ['sel'], ['top8'])
                    V(lambda v: v.tensor_scalar(out=rs[:, 28:36], in0=rs[:, 12:20], scalar1=rs[:, 20:21], scalar2=None, op0=ALU.is_equal),
                      ['sel', 'top8'], ['m1'])
                    V(lambda v: v.tensor_scalar(out=rs[:, 36:44], in0=rs[:, 12:20], scalar1=rs[:, 21:22], scalar2=None, op0=ALU.is_equal),
                      ['sel', 'top8'], ['m2'])
                    V(lambda v: v.tensor_tensor(out=rs[:, 44:45], in0=rs[:, 21:22], in1=rs[:, 20:21], op=ALU.subtract), ['top8'], ['rd'])
                    P.op('act', lambda a: a.activation(out=rs[:, 45:46], in_=rs[:, 44:45], func=AF.Exp), reads=['rd'], writes=['red'])
                    V(lambda v: v.tensor_scalar(out=rs[:, 46:47], in0=rs[:, 45:46], scalar1=1.0, scalar2=None, op0=ALU.add), ['red'], ['rden'])
                    V(lambda v: v.reciprocal(out=rs[:, 47:48], in_=rs[:, 46:47]), ['rden'], ['rw1'])
                    V(lambda v: v.tensor_tensor(out=rs[:, 48:49], in0=rs[:, 45:46], in1=rs[:, 47:48], op=ALU.mult), ['red', 'rw1'], ['rw2'])
                    V(lambda v: v.tensor_tensor(out=rs[:, 49:50], in0=rs[:, 47:48], in1=rs[:, 3:4], op=ALU.mult), ['rw1', 'r3'], ['rw1p'])
                    V(lambda v: v.tensor_tensor(out=rs[:, 50:51], in0=rs[:, 48:49], in1=rs[:, 3:4], op=ALU.mult), ['rw2', 'r3'], ['rw2p'])
                    V(lambda v: v.tensor_scalar(out=rs[:, 52:60], in0=rs[:, 28:36], scalar1=rs[:, 49:50], scalar2=None, op0=ALU.mult),
                      ['m1', 'rw1p'], ['cw'])
                    V(lambda v: v.scalar_tensor_tensor(out=rs[:, 52:60], in0=rs[:, 36:44], scalar=rs[:, 50:51], in1=rs[:, 52:60],
                                                       op0=ALU.mult, op1=ALU.add), ['m2', 'rw2p', 'cw'], ['cw'])
                    for g in range(4):
                        V(lambda v, g=g, wd_=wd_: v.tensor_scalar(out=wd_[:, g * 8:(g + 1) * 8], in0=rs[:, 52:60], scalar1=rs[:, 8 + g:9 + g],
                                                                  scalar2=None, op0=ALU.mult), ['cw', 'gm'], [('wdt%d' % p2, g)])
                    P.dma('sp', wdd[t * 128:(t + 1) * 128, :], wd_[:], reads=[('wdt%d' % p2, g) for g in range(4)], writes=[('wdd', t)])
                P.barrier()
                P.flush()

        if upto >= 6:
            for half in range(2):
                with contextlib.ExitStack() as ph:
                    y2 = sb(ph, "y2", [128, 8, D], F32)
                    wdh = sb(ph, "wdh", [128, 8, NEXP], F32)
                    h2h = sb(ph, "h2h", [128, NKC, 1024], BF16)
                    P.dma('sp', h2h[:], h2Td[:, :, half * 1024:(half + 1) * 1024].rearrange("c p t -> p c t"), writes=['h2h'])
                    P.dma('sp', wdh[:], wdd[half * 1024:(half + 1) * 1024, :].rearrange("(c p) e -> p c e", p=128), writes=['wdh'])
                    for ti in range(8):
                        P.op('pool', lambda g, ti=ti: g.memset(y2[:, ti, :], 0.0), writes=[('y2', ti, fb) for fb in range(4)])
                    if debug:
                        P.dma('sp', dbg_y0[half], y2[:].rearrange("p a b -> p (a b)"),
                              reads=[('y2', ti, fb) for ti in range(8) for fb in range(4)], writes=['dbg_y0'])
                        P.dma('sp', dbg_wdh[half], wdh[:].rearrange("p a b -> p (a b)"), reads=['wdh'], writes=['dbg_wdh'])
                        P.dma('sp', dbg_h2h[half], h2h[:].rearrange("p a b -> p (a b)"), reads=['h2h'], writes=['dbg_h2h'])
                    with contextlib.ExitStack() as ph2:
                        hact = [sb(ph2, "hact%d" % i, [128, 4, 512], BF16) for i in range(2)]
                        sil = [sb(ph2, "sil%d" % i, [128, 512], F32) for i in range(2)]
                        wg = sb(ph2, "wg", [128, NKC, DEXP], BF16)
                        wu = sb(ph2, "wu", [128, NKC, DEXP], BF16)
                        wdn = sb(ph2, "wdn", [128, 4, D], BF16)
                        NSTG = 3
                        stg = [sb(ph2, "stg%d" % i, [128, 1024], F32) for i in range(NSTG)]
                        pad = sb(ph2, "pad", [128, 1024], F32)
                        P.op('pool', lambda g: g.memset(pad[:], 0.0), writes=['pad'])
                        ns = 0
                        nsl = 0
                        nh = 0
                        nbk = 0
                        for e in range(NEXP):
                            for (src, dstw, dk_, nhalf, ceng) in ((w_gate[e], wg, 'wg', 2, 'pool'), (w_up[e], wu, 'wu', 2, 'act'),
                                                                  (w_down[e], wdn, 'wdn', 2, 'pool')):
                                for hf in range(8):
                                    sg = stg[ns % NSTG]
                                    sk = 'stg%d' % (ns % NSTG)
                                    ns += 1
                                    if dk_ == 'wdn':
                                        dcw, hh = hf // 2, hf % 2
                                        P.dma('sp', sg[:], src[dcw * 128:(dcw + 1) * 128, hh * 1024:(hh + 1) * 1024], writes=[sk])
                                        dst_ap = dstw[:, dcw, hh * 1024:(hh + 1) * 1024]
                                    else:
                                        P.dma('sp', sg[:].rearrange("p (c n) -> p c n", c=2),
                                              src[hf * 256:(hf + 1) * 256, :].rearrange("(c p) n -> p c n", p=128), writes=[sk])
                                        dst_ap = dstw[:, hf * 2:(hf + 1) * 2, :].rearrange("p c n -> p (c n)")
                                    if ceng == 'pool':
                                        P.op('pool', lambda g, sg=sg, dst_ap=dst_ap: g.tensor_copy(out=dst_ap, in_=sg[:]),
                                             reads=[sk], writes=[(dk_, hf)])
                                    else:
                                        P.op('act', lambda a, sg=sg, dst_ap=dst_ap: a.copy(out=dst_ap, in_=sg[:]),
                                             reads=[sk], writes=[(dk_, hf)])
                            for ch in range(2):
                                ha = hact[nh % 2]
                                hk = 'hact%d' % (nh % 2)
                                nh += 1
                                for dc in range(4):
                                    bg = nbk % 2
                                    bu = 2 + nbk % 2
                                    nbk += 1
                                    for kc in range(NKC):
                                        P.op('pe', lambda pe, kc=kc, dc=dc, ch=ch, bg=bg: pe.matmul(
                                            banks[bg][:, :], lhsT=wg[:, kc, dc * 128:(dc + 1) * 128], rhs=h2h[:, kc, ch * 512:(ch + 1) * 512],
                                            start=(kc == 0), stop=(kc == NKC - 1)), reads=[('wg', kc // 2), 'h2h'], writes=['b%d' % bg], excl=B(bg))
                                    for kc in range(NKC):
                                        P.op('pe', lambda pe, kc=kc, dc=dc, ch=ch, bu=bu: pe.matmul(
                                            banks[bu][:, :], lhsT=wu[:, kc, dc * 128:(dc + 1) * 128], rhs=h2h[:, kc, ch * 512:(ch + 1) * 512],
                                            start=(kc == 0), stop=(kc == NKC - 1)), reads=[('wu', kc // 2), 'h2h'], writes=['b%d' % bu], excl=B(bu))
                                    sl = sil[nsl % 2]
                                    slk = 'sil%d' % (nsl % 2)
                                    nsl += 1
                                    P.op('act', lambda a, sl=sl, bg=bg: a.activation(out=sl[:], in_=banks[bg][:, :], func=AF.Silu),
                                         reads=['b%d' % bg], writes=[slk], excl=B(bg))
                                    P.op('dve', lambda v, sl=sl, bu=bu, ha=ha, dc=dc: v.tensor_tensor(
                                        out=ha[:, dc, :], in0=banks[bu][:, :], in1=sl[:], op=ALU.mult),
                                        reads=['b%d' % bu, slk], writes=[(hk, dc)], excl=B(bu))
                                for tl in range(4):
                                    ti = ch * 4 + tl
                                    for fb in range(4):
                                        bd = 4 + fb
                                        for dc in range(4):
                                            P.op('pe', lambda pe, dc=dc, tl=tl, fb=fb, bd=bd, ha=ha: pe.matmul(
                                                banks[bd][:, :], lhsT=ha[:, dc, tl * 128:(tl + 1) * 128], rhs=wdn[:, dc, fb * 512:(fb + 1) * 512],
                                                start=(dc == 0), stop=(dc == 3)), reads=[(hk, dc), ('wdn', dc * 2 + fb // 2)], writes=['b%d' % bd], excl=B(bd))
                                        P.op('dve', lambda v, ti=ti, fb=fb, bd=bd, e=e: v.scalar_tensor_tensor(
                                            out=y2[:, ti, fb * 512:(fb + 1) * 512], in0=banks[bd][:, :], scalar=wdh[:, ti, e:e + 1],
                                            in1=y2[:, ti, fb * 512:(fb + 1) * 512], op0=ALU.mult, op1=ALU.add),
                                            reads=['b%d' % bd, 'wdh', ('y2', ti, fb)], writes=[('y2', ti, fb)], excl=B(bd))
                        if debug:
                            P.dma('sp', dbg_pad[half], pad[:], reads=['pad'], writes=['dbg_pad'])
                            for q in range(2):
                                P.dma('sp', dbg_hact[half, q], hact[q][:].rearrange("p a b -> p (a b)"),
                                      reads=[('hact%d' % q, dc) for dc in range(4)], writes=[('dbg_hact', q)])
                    P.barrier()
                    if debug:
                        for ti in range(8):
                            t = half * 8 + ti
                            P.dma('sp', y2d[t * 128:(t + 1) * 128, :], y2[:, ti, :],
                                  reads=[('y2', ti, fb) for fb in range(4)], writes=[('y2d', t)])
                    with contextlib.ExitStack() as ph3:
                        G2 = bcast_load(ph3, "G2", modv[0:1, 5 * D:6 * D])
                        L2g = bcast_load(ph3, "L2g", ln2_g)
                        L2b = bcast_load(ph3, "L2b", ln2_b)
                        lb = ln_bufs(ph3)
                        xin, xn, st, mv, sd, cnt = lb
                        tg2 = sb(ph3, "tg2", [128, D], F32)
                        rt2 = sb(ph3, "rt2", [128, D], F32)
                        ot = [sb(ph3, "ot%d" % i, [128, D], F32) for i in range(2)]
                        for ti in range(8):
                            t = half * 8 + ti
                            xt = xin[ti % 2]
                            xk = 'xin%d' % (ti % 2)
                            P.dma('act', xt[:], x1d[t * 128:(t + 1) * 128, :], writes=[xk])
                            P.op('pool', lambda g, ti=ti: g.tensor_tensor(out=tg2[:], in0=y2[:, ti, :], in1=G2[:], op=ALU.mult),
                                 reads=[('y2', ti, fb) for fb in range(4)] + ['G2'], writes=['tg2'])
                            P.op('dve', lambda v, xt=xt: v.scalar_tensor_tensor(out=rt2[:], in0=xt[:], scalar=ALPHA, in1=tg2[:],
                                                                               op0=ALU.mult, op1=ALU.add), reads=[xk, 'tg2'], writes=['rt2'])
                            layer_norm_tile(rt2, 'rt2', xn, 'xn', st, mv, sd, eps_t)
                            o = ot[ti % 2]
                            ok_ = 'ot%d' % (ti % 2)
                            P.op('dve', lambda v: v.tensor_tensor(out=xn[:], in0=xn[:], in1=L2g[:], op=ALU.mult), reads=['xn', 'L2g'], writes=['xn'])
                            P.op('pool', lambda g, o=o: g.tensor_tensor(out=o[:], in0=xn[:], in1=L2b[:], op=ALU.add), reads=['xn', 'L2b'], writes=[ok_])
                            P.dma('sp', out[t * 128:(t + 1) * 128, :], o[:], reads=[ok_], writes=[('out', t)])
                    P.barrier()
                    P.flush()
        P.barrier()
        P.flush()
    return nc


def make_in_maps(inputs):
    x = np.asarray(inputs["x"], dtype=np.float32)
    c = np.asarray(inputs["c"], dtype=np.float32)
    g = lambda k: np.ascontiguousarray(np.asarray(inputs[k], dtype=np.float32)[0])
    w_rt = np.ascontiguousarray(np.concatenate([g("w_group"), g("w_router")], axis=1))
    b_rt = np.ascontiguousarray(np.concatenate([g("b_group"), g("b_router")], axis=0)[None, :])
    sgu_wT = np.ascontiguousarray(g("sgu_w").transpose(2, 0, 1))
    shared = {
        "w_ada": g("w_ada"), "b_ada": g("b_ada")[None, :], "w_in": g("w_in"),
        "sgu_wT": sgu_wT, "sgu_b": g("sgu_b").reshape(1, -1),
        "sgu_ln_g": g("sgu_ln_g")[None, :], "sgu_ln_b": g("sgu_ln_b")[None, :],
        "w_proj_a": g("w_proj_a"), "w_proj_b": g("w_proj_b"), "w_out": g("w_out"),
        "ln1_g": g("ln1_g")[None, :], "ln1_b": g("ln1_b")[None, :],
        "w_rt": w_rt, "b_rt": b_rt,
        "w_gate": g("w_gate"), "w_up": g("w_up"), "w_down": g("w_down"),
        "ln2_g": g("ln2_g")[None, :], "ln2_b": g("ln2_b")[None, :],
    }
    maps = []
    for core in range(8):
        b = core // 4
        m = dict(shared)
        j = core % 4
        m["xb"] = np.ascontiguousarray(x[b])
        m["xo"] = np.ascontiguousarray(x[b].reshape(16, 4, 128, D)[:, j].reshape(OWN, D))
        mk = np.zeros((128, 4, 128), np.float32)
        for mm in range(4):
            if mm < j:
                mk[:, mm, :] = 1.0
            elif mm == j:
                mk[:, mm, :] = np.triu(np.ones((128, 128), np.float32), k=1)
        m["maskd"] = mk
        m["cT"] = np.ascontiguousarray(c[b].reshape(NKC, 128).T)
        maps.append(m)
    return maps


_NC_CACHE = {}


def kernel(**inputs):
    if "nc" not in _NC_CACHE:
        _NC_CACHE["nc"] = build()
    nc = _NC_CACHE["nc"]
    maps = make_in_maps(inputs)
    res = run_bass_kernel_spmd(nc, maps, core_ids=list(range(8)))
    full = np.zeros((2, S, D), np.float32)
    for core in range(8):
        b, j = core // 4, core % 4
        o = np.asarray(res.results[core]["out"], dtype=np.float32).reshape(16, 128, D)
        full[b].reshape(16, 4, 128, D)[:, j] = o
    return full
```

```python
import contextlib
import os
import numpy as np
import ml_dtypes
import concourse.bass as bass
import concourse.mybir as mybir
from concourse.bass_utils import run_bass_kernel_spmd

F32 = mybir.dt.float32
BF16 = mybir.dt.bfloat16
U32 = mybir.dt.uint32
AF = mybir.ActivationFunctionType
ALU = mybir.AluOpType

D = 2048
S = 8192
NKC = 16
OWN = 2048
LN_EPS = 1e-5
ALPHA = 2.0 ** 0.25
NEXP = 32
DEXP = 512


class Prog:
    LIMIT = 30000
    NDMA = 6

    def __init__(self, nc, stack):
        self.nc = nc
        self.stack = stack
        self.names = ['pe', 'act', 'dve', 'pool', 'sp']
        self.sems = []
        self.ops = {e: [] for e in self.names}
        self.cur = {}
        self.cnt = {}
        for e in self.names:
            self.cur[e] = self._new_sem("c_" + e)
            self.cnt[e] = 0
        self.waited = {e: {} for e in self.names}
        self.lastw = {}
        self.readers = {}
        self.latest = {}
        self.dq = {}
        self.dn = {}
        for q in ['sp', 'act', 'pool']:
            self.dq[q] = [self._new_sem("d_%s%d" % (q, i)) for i in range(self.NDMA)]
            self.dn[q] = 0
        self.n_instr = 0

    def _new_sem(self, name):
        h = self.stack.enter_context(self.nc.semaphore(name + "_%d" % len(self.sems)))
        self.sems.append(h)
        return len(self.sems) - 1

    def _deps(self, e, reads, writes, excl=()):
        deps = []
        for k in excl:
            t = self.lastw.get(k)
            if t is not None and t[0] != self.cur.get(e, -1):
                deps.append(t)
        for k in reads:
            t = self.lastw.get(k)
            if t is not None:
                deps.append(t)
        for k in writes:
            t = self.lastw.get(k)
            if t is not None:
                deps.append(t)
            deps.extend(self.readers.get(k, ()))
        waits = {}
        for (s, v) in deps:
            if e == 'pe' and s == self.cur['pe']:
                continue
            if self.waited[e].get(s, 0) >= v:
                continue
            if waits.get(s, 0) < v:
                waits[s] = v
        for s, v in waits.items():
            self.waited[e][s] = v
        return list(waits.items())

    def _commit(self, tok, reads, writes):
        for k in writes:
            self.lastw[k] = tok
            self.readers[k] = []
        for k in reads:
            if k in writes:
                continue
            self.readers.setdefault(k, []).append(tok)
        self.latest[tok[0]] = tok[1]

    def op(self, e, fn, reads=(), writes=(), excl=()):
        waits = self._deps(e, reads, writes, excl)
        if self.cnt[e] >= self.LIMIT:
            self.cur[e] = self._new_sem("c_" + e)
            self.cnt[e] = 0
        self.cnt[e] += 1
        tok = (self.cur[e], self.cnt[e])
        sems = self.sems

        def emit(eng, waits=waits, fn=fn, tok=tok):
            for (s, v) in waits:
                eng.wait_ge(sems[s], v)
            fn(eng).then_inc(sems[tok[0]], 1)
        self.ops[e].append(emit)
        self._commit(tok, reads, writes)
        for k in excl:
            self.lastw[k] = tok
        self.n_instr += 1
        return tok

    def dma(self, q, out, in_, reads=(), writes=(), **kw):
        waits = self._deps(q, reads, writes)
        i = self.dn[q]
        self.dn[q] += 1
        k = i % self.NDMA
        val = 16 * (i // self.NDMA + 1)
        s = self.dq[q][k]
        if i >= self.NDMA and self.waited[q].get(s, 0) < val - 16:
            waits.append((s, val - 16))
            self.waited[q][s] = val - 16
        tok = (s, val)
        sems = self.sems

        def emit(eng, waits=waits, tok=tok, out=out, in_=in_, kw=kw):
            for (ss, v) in waits:
                eng.wait_ge(sems[ss], v)
            eng.dma_start(out=out, in_=in_, **kw).then_inc(sems[tok[0]], 16)
        self.ops[q].append(emit)
        self._commit(tok, reads, writes)
        self.n_instr += 1
        return tok

    def barrier(self):
        for e in self.names:
            waits = []
            for s, v in self.latest.items():
                if self.waited[e].get(s, 0) < v:
                    waits.append((s, v))
                    self.waited[e][s] = v
            sems = self.sems

            def emit(eng, waits=waits):
                for (s, v) in waits:
                    eng.wait_ge(sems[s], v)
            self.ops[e].append(emit)
        self.lastw.clear()
        self.readers.clear()

    def flush(self):
        nc = self.nc
        ops = self.ops
        with nc.Block() as block:
            @block.tensor
            def _(eng):
                for f in ops['pe']:
                    f(eng)

            @block.scalar
            def _(eng):
                for f in ops['act']:
                    f(eng)

            @block.vector
            def _(eng):
                for f in ops['dve']:
                    f(eng)

            @block.gpsimd
            def _(eng):
                for f in ops['pool']:
                    f(eng)

            @block.sync
            def _(eng):
                for f in ops['sp']:
                    f(eng)
        self.ops = {e: [] for e in self.names}


def build(upto=99, debug=False, lite=False):
    nc = bass.Bass("TRN2", target_bir_lowering=False)
    dk = "ExternalOutput"

    def din(name, shape, dt=F32):
        return nc.dram_tensor(name, list(shape), dt, kind="ExternalInput").ap()

    xb = din("xb", [S, D])
    xo = din("xo", [OWN, D])
    maskd = din("maskd", [128, 4, 128])
    cT = din("cT", [128, NKC])
    w_ada = din("w_ada", [D, 6 * D])
    b_ada = din("b_ada", [1, 6 * D])
    w_in = din("w_in", [D, 9216])
    sgu_wT = din("sgu_wT", [128, 8, 128])
    sgu_b = din("sgu_b", [1, 8 * 128])
    sgu_ln_g = din("sgu_ln_g", [1, 1024])
    sgu_ln_b = din("sgu_ln_b", [1, 1024])
    w_pa = din("w_proj_a", [1024, D])
    w_pb = din("w_proj_b", [1024, D])
    w_out = din("w_out", [D, D])
    ln1_g = din("ln1_g", [1, D])
    ln1_b = din("ln1_b", [1, D])
    w_rt = din("w_rt", [D, 36])
    b_rt = din("b_rt", [1, 36])
    if not lite:
        w_gate = din("w_gate", [NEXP, D, DEXP])
        w_up = din("w_up", [NEXP, D, DEXP])
        w_down = din("w_down", [NEXP, DEXP, D])
    ln2_g = din("ln2_g", [1, D])
    ln2_b = din("ln2_b", [1, D])
    out = nc.dram_tensor("out", [OWN, D], F32, kind="ExternalOutput").ap()

    def dscr(name, shape, dt):
        return nc.dram_tensor(name, list(shape), dt, kind=dk).ap()

    modv = dscr("modv", [1, 6 * D], F32)
    KT = dscr("KT", [8, 128, S], BF16)
    VV = dscr("VV", [64, 128, 1024], BF16)
    QTd = dscr("QTd", [8, 128, OWN], BF16)
    obTd = dscr("obTd", [8, 128, OWN], BF16)
    gaTd = dscr("gaTd", [NKC, 128, OWN], BF16)
    gbTd = dscr("gbTd", [NKC, 128, OWN], BF16)
    oaTd = dscr("oaTd", [8, 128, OWN], BF16)
    x1d = dscr("x1d", [OWN, D], F32)
    h2Td = dscr("h2Td", [NKC, 128, OWN], BF16)
    wdd = dscr("wdd", [OWN, NEXP], F32)

    with contextlib.ExitStack() as top:
        P = Prog(nc, top)
        banks = [top.enter_context(nc.psum_tensor("bank%d" % i, [128, 512], F32)) for i in range(8)]
        ident = top.enter_context(nc.sbuf_tensor("ident", [128, 128], F32))
        identb = top.enter_context(nc.sbuf_tensor("identb", [128, 128], BF16))
        ones_r = top.enter_context(nc.sbuf_tensor("ones_r", [1, 128], F32))

        uniq = [0]

        def sb(ph, name, shape, dt):
            uniq[0] += 1
            return ph.enter_context(nc.sbuf_tensor("%s_%d" % (name, uniq[0]), list(shape), dt))

        P.op('pool', lambda g: g.memset(ident[:], 1.0), writes=['ident'])
        P.op('pool', lambda g: g.affine_select(out=ident[:], in_=ident[:], pattern=[[-1, 128]],
                                               compare_op=ALU.is_equal, fill=0.0, base=0,
                                               channel_multiplier=1),
             reads=['ident'], writes=['ident'])
        P.op('pool', lambda g: g.tensor_copy(out=identb[:], in_=ident[:]), reads=['ident'], writes=['identb'])
        P.op('pool', lambda g: g.memset(ones_r[:], 1.0), writes=['ones_r'])

        if upto >= 0:
            with contextlib.ExitStack() as ph:
                cact = sb(ph, "cact", [128, NKC], F32)
                craw = sb(ph, "craw", [128, NKC], F32)
                bada = sb(ph, "bada", [1, 6 * D], F32)
                wst = [sb(ph, "wst%d" % i, [128, 2048], F32) for i in range(3)]
                mrow = sb(ph, "mrow", [1, 2048], F32)
                P.dma('sp', craw[:], cT, writes=['craw'])
                P.dma('sp', bada[:], b_ada, writes=['bada'])
                P.op('act', lambda a: a.activation(out=cact[:], in_=craw[:], func=AF.Silu),
                     reads=['craw'], writes=['cact'])
                n = 0
                for g in range(6):
                    for kc in range(NKC):
                        w = wst[n % 3]
                        wk = 'wst%d' % (n % 3)
                        n += 1
                        P.dma('sp', w[:], w_ada[kc * 128:(kc + 1) * 128, g * 2048:(g + 1) * 2048], writes=[wk])
                        for jj in range(4):
                            P.op('pe', lambda pe, w=w, jj=jj, kc=kc: pe.matmul(
                                banks[jj][0:1, :], lhsT=cact[:, kc:kc + 1], rhs=w[:, jj * 512:(jj + 1) * 512],
                                start=(kc == 0), stop=(kc == NKC - 1)),
                                reads=[wk, 'cact'], writes=['b%d' % jj])
                    for jj in range(4):
                        P.op('dve', lambda v, jj=jj, g=g: v.tensor_tensor(
                            out=mrow[0:1, jj * 512:(jj + 1) * 512], in0=banks[jj][0:1, :],
                            in1=bada[0:1, g * 2048 + jj * 512: g * 2048 + (jj + 1) * 512], op=ALU.add),
                            reads=['b%d' % jj, 'bada'], writes=['mrow%d' % jj])
                    P.dma('sp', modv[0:1, g * 2048:(g + 1) * 2048], mrow[:],
                          reads=['mrow%d' % jj for jj in range(4)], writes=['modv'])
                P.barrier()
                P.flush()

        mfm = top.enter_context(nc.sbuf_tensor("mfm", [128, 6, NKC], F32))
        mview = modv.rearrange("o (s c p) -> p (o s) c", p=128, c=NKC)
        for s6 in range(6):
            P.dma('sp', mfm[:, s6, :], mview[:, s6, :], reads=['modv'], writes=['mfm'],
                  allow_slow_non_contiguous=True)
        P.op('dve', lambda v: v.tensor_scalar(out=mfm[:, 1, :], in0=mfm[:, 1, :], scalar1=1.0, scalar2=None,
                                              op0=ALU.add), reads=['mfm'], writes=['mfm'])
        P.op('dve', lambda v: v.tensor_scalar(out=mfm[:, 4, :], in0=mfm[:, 4, :], scalar1=1.0, scalar2=None,
                                              op0=ALU.add), reads=['mfm'], writes=['mfm'])

        def layer_norm_tile(xt, xk, xn, xnk, st, mv, sd, eps_t, sfx=''):
            for q in range(4):
                P.op('dve', lambda v, q=q: v.bn_stats(out=st[:, q, :], in_=xt[:, q * 512:(q + 1) * 512]),
                     reads=[xk], writes=['st%d' % q + sfx])
            P.op('dve', lambda v: v.bn_aggr(out=mv[:], in_=st[:].rearrange("p a b -> p (a b)")),
                 reads=['st%d' % q + sfx for q in range(4)], writes=['mv' + sfx])
            P.op('act', lambda a: a.activation(out=sd[:, 0:1], in_=mv[:, 1:2], func=AF.Sqrt, bias=eps_t[:, 0:1],
                                               scale=1.0),
                 reads=['mv' + sfx, 'eps'], writes=['sd0' + sfx])
            P.op('dve', lambda v: v.reciprocal(out=sd[:, 1:2], in_=sd[:, 0:1]), reads=['sd0' + sfx], writes=['sd1' + sfx])
            P.op('dve', lambda v: v.tensor_scalar(out=sd[:, 2:3], in0=mv[:, 0:1], scalar1=sd[:, 1:2], scalar2=-1.0,
                                                  op0=ALU.mult, op1=ALU.mult),
                 reads=['mv' + sfx, 'sd1' + sfx], writes=['sd2' + sfx])
            P.op('act', lambda a: a.activation(out=xn[:], in_=xt[:], func=AF.Identity, bias=sd[:, 2:3],
                                               scale=sd[:, 1:2]),
                 reads=[xk, 'sd1' + sfx, 'sd2' + sfx], writes=[xnk])

        eps_t = top.enter_context(nc.sbuf_tensor("eps_t", [128, 1], F32))
        P.op('pool', lambda g: g.memset(eps_t[:], LN_EPS), writes=['eps'])

        def B(bk):
            return ['B%d' % bk]

        def ln_T_tile(ph_bufs, src_rows, dst, dkey, col0, ms, fdst=None, bo=0, xt_in=None, xk_in=None):
            xin, xn, st, mv, sd, cnt = ph_bufs
            xt = xin[cnt[0] % 2]
            xk = 'xin%d' % (cnt[0] % 2)
            cnt[0] += 1
            if src_rows is not None:
                P.dma('act', xt[:], src_rows, writes=[xk])
            else:
                xt, xk = xt_in, xk_in
            layer_norm_tile(xt, xk, xn, 'xn', st, mv, sd, eps_t)
            transpose_evac(xn, 'xn', dst, dkey, col0, ms, fdst, bo)

        def transpose_evac(xn, xnk, dst, dkey, col0, ms, fdst=None, bo=0):
            for kc in range(NKC):
                bk = bo + kc // 4
                P.op('pe', lambda pe, kc=kc, bk=bk: pe.transpose(
                    banks[bk][:, (kc % 4) * 128:(kc % 4 + 1) * 128], xn[:, kc * 128:(kc + 1) * 128], ident[:]),
                    reads=[xnk, 'ident'], writes=[('bq', bk, kc % 4)], excl=B(bk))
            for kc in range(NKC):
                bk = bo + kc // 4
                q = kc % 4
                if bk - bo < 2:
                    P.op('dve', lambda v, kc=kc, bk=bk, q=q: v.tensor_scalar(
                        out=dst[:, kc, col0:col0 + 128], in0=banks[bk][:, q * 128:(q + 1) * 128],
                        scalar1=mfm[:, ms + 1, kc:kc + 1], scalar2=mfm[:, ms, kc:kc + 1],
                        op0=ALU.mult, op1=ALU.add),
                        reads=[('bq', bk, q), 'mfm'], writes=[(dkey, kc, col0)], excl=B(bk))
                else:
                    P.op('act', lambda a, kc=kc, bk=bk, q=q: a.activation(
                        out=dst[:, kc, col0:col0 + 128], in_=banks[bk][:, q * 128:(q + 1) * 128],
                        func=AF.Identity, scale=mfm[:, ms + 1, kc:kc + 1], bias=mfm[:, ms, kc:kc + 1]),
                        reads=[('bq', bk, q), 'mfm'], writes=[(dkey, kc, col0)], excl=B(bk))
                if fdst is not None:
                    P.op('dve', lambda v, kc=kc, bk=bk, q=q: v.tensor_scalar(
                        out=fdst[:, kc, :], in0=banks[bk][:, q * 128:(q + 1) * 128],
                        scalar1=mfm[:, ms + 1, kc:kc + 1], scalar2=mfm[:, ms, kc:kc + 1],
                        op0=ALU.mult, op1=ALU.add),
                        reads=[('bq', bk, q), 'mfm'], writes=[('fdst', kc)], excl=B(bk))

        def ln_bufs(ph):
            xin = [sb(ph, "xin%d" % i, [128, 2048], F32) for i in range(2)]
            xn = sb(ph, "xn", [128, 2048], F32)
            st = sb(ph, "st", [128, 4, 6], F32)
            mv = sb(ph, "mv", [128, 2], F32)
            sd = sb(ph, "sd", [128, 4], F32)
            return (xin, xn, st, mv, sd, [0])

        evc = [0]

        def evac_copy(dst_ap, bk, wkeys, src=None):
            src = banks[bk][:, :] if src is None else src
            evc[0] += 1
            if evc[0] % 2 == 0:
                P.op('dve', lambda v: v.tensor_copy(out=dst_ap, in_=src), reads=['b%d' % bk], writes=wkeys, excl=B(bk))
            else:
                P.op('act', lambda a: a.copy(out=dst_ap, in_=src), reads=['b%d' % bk], writes=wkeys, excl=B(bk))

        if upto >= 1:
            with contextlib.ExitStack() as ph:
                Wkv = sb(ph, "Wkv", [128, NKC, 2048], BF16)
                wst = [sb(ph, "wstb%d" % i, [128, 2048], F32) for i in range(2)]
                lb = ln_bufs(ph)
                hT = [sb(ph, "hT%d" % i, [128, NKC, 512], BF16) for i in range(2)]
                kst = [sb(ph, "kst%d" % i, [128, 8, 512], BF16) for i in range(2)]
                vst = [sb(ph, "vst%d" % i, [128, 1024], BF16) for i in range(2)]
                for kc in range(NKC):
                    w = wst[kc % 2]
                    wk = 'wstb%d' % (kc % 2)
                    P.dma('sp', w[:], w_in[kc * 128:(kc + 1) * 128, 1024:3072], writes=[wk])
                    P.op('pool', lambda g, w=w, kc=kc: g.tensor_copy(out=Wkv[:, kc, :], in_=w[:]),
                         reads=[wk], writes=[('Wkv', kc)])
                nkc = [0]

                def kv_units(gc):
                    hb = hT[gc % 2]
                    hk = 'hT%d' % (gc % 2)
                    ks = kst[gc % 2]
                    kk = 'kst%d' % (gc % 2)

                    def unit(k):
                        for h in (2 * k, 2 * k + 1):
                            bk = 4 + (nkc[0] % 4)
                            nkc[0] += 1
                            for kc in range(NKC):
                                P.op('pe', lambda pe, kc=kc, h=h, bk=bk: pe.matmul(
                                    banks[bk][:, :], lhsT=Wkv[:, kc, h * 128:(h + 1) * 128], rhs=hb[:, kc, :],
                                    start=(kc == 0), stop=(kc == NKC - 1)),
                                    reads=[('Wkv', kc)] + [(hk, kc, t4 * 128) for t4 in range(4)],
                                    writes=['b%d' % bk], excl=B(bk))
                            evac_copy(ks[:, h, :], bk, [(kk, h)])
                        if k == 3:
                            P.dma('sp', KT[:, :, gc * 512:(gc + 1) * 512].rearrange("h p t -> p h t"), ks[:],
                                  reads=[(kk, h) for h in range(8)], writes=[('KT', gc)])
                        tt = k
                        vs = vst[(gc * 4 + tt) % 2]
                        vk = 'vst%d' % ((gc * 4 + tt) % 2)
                        for half in range(2):
                            bk = 4 + (nkc[0] % 4)
                            nkc[0] += 1
                            for kc in range(NKC):
                                P.op('pe', lambda pe, kc=kc, half=half, bk=bk: pe.matmul(
                                    banks[bk][:, :], lhsT=hb[:, kc, tt * 128:(tt + 1) * 128],
                                    rhs=Wkv[:, kc, 1024 + half * 512:1024 + (half + 1) * 512],
                                    start=(kc == 0), stop=(kc == NKC - 1)),
                                    reads=[('Wkv', kc), (hk, kc, tt * 128)], writes=['b%d' % bk], excl=B(bk))
                            evac_copy(vs[:, half * 512:(half + 1) * 512], bk, [(vk, half)])
                        P.dma('sp', VV[gc * 4 + tt], vs[:], reads=[(vk, 0), (vk, 1)], writes=[('VV', gc * 4 + tt)])
                    return [lambda k=k: unit(k) for k in range(4)]

                xin_, xn_, st_, mv_, sd_, _c = lb
                xnB = [xn_, sb(ph, "xnB", [128, 2048], F32)]
                stB = [st_, sb(ph, "stB", [128, 4, 6], F32)]
                mvB = [mv_, sb(ph, "mvB", [128, 2], F32)]
                sdB = [sd_, sb(ph, "sdB", [128, 4], F32)]

                def S1(T):
                    p = T % 2
                    xk = 'xin%d' % p
                    P.dma('act', xin_[p][:], xb[T * 128:(T + 1) * 128, :], writes=[xk])
                    layer_norm_tile(xin_[p], xk, xnB[p], 'xn%d' % p, stB[p], mvB[p], sdB[p], eps_t, sfx='_%d' % p)

                def S2(T):
                    gc, tt = T // 4, T % 4
                    transpose_evac(xnB[T % 2], 'xn%d' % (T % 2), hT[gc % 2], 'hT%d' % (gc % 2), tt * 128, 0)

                pending = []
                S1(0)
                for gc in range(16):
                    for tt in range(4):
                        T = gc * 4 + tt
                        if T + 1 < 64:
                            S1(T + 1)
                        S2(T)
                        if pending:
                            pending[tt]()
                    pending = kv_units(gc)
                for u in pending:
                    u()
                P.barrier()
                P.flush()

        if upto >= 2:
            with contextlib.ExitStack() as ph:
                hO = sb(ph, "hO", [128, NKC, OWN], BF16)
                lb = ln_bufs(ph)
                wsh = [sb(ph, "wsh%d" % i, [128, 8, 512], F32) for i in range(2)]
                wbf = [sb(ph, "wbf%d" % i, [128, NKC, 512], BF16) for i in range(2)]
                ost = [sb(ph, "ost%d" % i, [128, 4, 512], BF16) for i in range(2)]
                for t in range(16):
                    ln_T_tile(lb, xo[t * 128:(t + 1) * 128, :], hO, 'hO', t * 128, 0)
                hO_keys = lambda kc, c0, n: [('hO', kc, c0 + 128 * q) for q in range(n)]
                nld = [0]

                def load_wblock(src2d, c0, wb, wbk, nkc=NKC):
                    for half in range(nkc // 8):
                        w = wsh[nld[0] % 2]
                        wk = 'wsh%d' % (nld[0] % 2)
                        nld[0] += 1
                        P.dma('sp', w[:], src2d[half * 1024:(half + 1) * 1024, c0:c0 + 512].rearrange(
                            "(c p) n -> p c n", p=128), writes=[wk])
                        P.op('pool', lambda g, w=w, half=half, wb=wb: g.tensor_copy(
                            out=wb[:, half * 8:(half + 1) * 8, :], in_=w[:]),
                            reads=[wk], writes=[(wbk, half)])

                blocks = []
                for i in range(2):
                    blocks.append((i * 512, 'copy', QTd, i * 4))
                for i in range(2):
                    blocks.append((3072 + i * 512, 'gelu', obTd, i * 4))
                for i in range(4):
                    blocks.append((5120 + i * 512, 'sig', gaTd, i * 4))
                for i in range(4):
                    blocks.append((7168 + i * 512, 'sig', gbTd, i * 4))
                nb = 0
                no = 0
                nbank = 0
                for (c0, fn, dstd, ch0) in blocks:
                    wb = wbf[nb % 2]
                    wbk = 'wbf%d' % (nb % 2)
                    nb += 1
                    load_wblock(w_in, c0, wb, wbk)
                    for oc in range(4):
                        os_ = ost[no % 2]
                        ok = 'ost%d' % (no % 2)
                        no += 1
                        for sub in range(4):
                            bk = 4 + (nbank % 4)
                            nbank += 1
                            for kc in range(NKC):
                                P.op('pe', lambda pe, kc=kc, sub=sub, bk=bk, wb=wb, oc=oc: pe.matmul(
                                    banks[bk][:, :], lhsT=wb[:, kc, sub * 128:(sub + 1) * 128],
                                    rhs=hO[:, kc, oc * 512:(oc + 1) * 512],
                                    start=(kc == 0), stop=(kc == NKC - 1)),
                                    reads=[(wbk, kc // 8)] + hO_keys(kc, oc * 512, 4), writes=['b%d' % bk], excl=B(bk))
                            if fn == 'copy':
                                evac_copy(os_[:, sub, :], bk, [(ok, sub)])
                            else:
                                f = AF.Gelu if fn == 'gelu' else AF.Sigmoid
                                P.op('act', lambda a, sub=sub, bk=bk, os_=os_, f=f: a.activation(
                                    out=os_[:, sub, :], in_=banks[bk][:, :], func=f),
                                    reads=['b%d' % bk], writes=[(ok, sub)], excl=B(bk))
                        P.dma('sp', dstd[ch0:ch0 + 4, :, oc * 512:(oc + 1) * 512].rearrange("c p t -> p c t"), os_[:],
                              reads=[(ok, q) for q in range(4)], writes=[(dstd.tensor.name, ch0, oc)])
                load_wblock(w_in, 4096, wbf[0], 'wbf0')
                load_wblock(w_in, 4608, wbf[1], 'wbf1')
                wsf = sb(ph, "wsf", [128, 8, 128], F32)
                wsb = sb(ph, "wsb", [128, 8, 128], BF16)
                bsb = sb(ph, "bsb", [128, 1024], F32)
                lnG = sb(ph, "lnG", [128, 1024], F32)
                lnB = sb(ph, "lnB", [128, 1024], F32)
                gv = sb(ph, "gv", [128, 1024], F32)
                vn0 = sb(ph, "vn0", [128, 1024], F32)
                vnb = sb(ph, "vnb", [128, 1024], BF16)
                uT = [sb(ph, "uT%d" % i, [128, 8, 128], BF16) for i in range(2)]
                tmpm = sb(ph, "tmpm", [128, 1024], F32)
                obs = [sb(ph, "obs%d" % i, [128, 8, 128], BF16) for i in range(2)]
                st2 = sb(ph, "st2", [128, 2, 6], F32)
                mv2 = sb(ph, "mv2", [128, 2], F32)
                sd2 = sb(ph, "sd2", [128, 4], F32)
                P.dma('sp', wsf[:], sgu_wT, writes=['wsf'])
                P.op('pool', lambda g: g.memset(wsf[64:128, :, 0:64], 0.0), reads=['wsf'], writes=['wsf'])
                P.op('pool', lambda g: g.tensor_copy(out=wsb[:], in_=wsf[:]), reads=['wsf'], writes=['wsb'])
                P.dma('sp', bsb[:], sgu_b.partition_broadcast(128).rearrange("p o f -> p (o f)"), writes=['bsb'])
                P.dma('sp', lnG[:], sgu_ln_g.partition_broadcast(128).rearrange("p o f -> p (o f)"), writes=['lnG'])
                P.dma('sp', lnB[:], sgu_ln_b.partition_broadcast(128).rearrange("p o f -> p (o f)"), writes=['lnB'])
                for t in range(16):
                    u = uT[t % 2]
                    uk = 'uT%d' % (t % 2)
                    ob = obs[t % 2]
                    obk = 'obs%d' % (t % 2)
                    P.dma('act', u[:], obTd[:, :, t * 128:(t + 1) * 128].rearrange("c p t -> p c t"),
                          reads=[('obTd', 0, t // 4), ('obTd', 4, t // 4)], writes=[uk])
                    for half in range(2):
                        bk = half
                        for kc in range(NKC):
                            P.op('pe', lambda pe, kc=kc, half=half, bk=bk, t=t: pe.matmul(
                                banks[bk][:, :], lhsT=hO[:, kc, t * 128:(t + 1) * 128], rhs=wbf[half][:, kc, :],
                                start=(kc == 0), stop=(kc == NKC - 1)),
                                reads=[('wbf%d' % half, kc // 8), ('hO', kc, t * 128)], writes=['b%d' % bk], excl=B(bk))
                        P.op('act', lambda a, half=half, bk=bk: a.activation(
                            out=gv[:, half * 512:(half + 1) * 512], in_=banks[bk][:, :], func=AF.Gelu),
                            reads=['b%d' % bk], writes=[('gv', half)], excl=B(bk))
                        P.op('dve', lambda v, half=half: v.bn_stats(out=st2[:, half, :], in_=gv[:, half * 512:(half + 1) * 512]),
                             reads=[('gv', half)], writes=[('st2', half)])
                    P.op('dve', lambda v: v.bn_aggr(out=mv2[:], in_=st2[:].rearrange("p a b -> p (a b)")),
                         reads=[('st2', 0), ('st2', 1)], writes=['mv2'])
                    P.op('act', lambda a: a.activation(out=sd2[:, 0:1], in_=mv2[:, 1:2], func=AF.Sqrt, bias=eps_t[:, 0:1], scale=1.0),
                         reads=['mv2', 'eps'], writes=['sd20'])
                    P.op('dve', lambda v: v.reciprocal(out=sd2[:, 1:2], in_=sd2[:, 0:1]), reads=['sd20'], writes=['sd21'])
                    P.op('dve', lambda v: v.tensor_scalar(out=sd2[:, 2:3], in0=mv2[:, 0:1], scalar1=sd2[:, 1:2], scalar2=-1.0,
                                                          op0=ALU.mult, op1=ALU.mult), reads=['mv2', 'sd21'], writes=['sd22'])
                    P.op('act', lambda a: a.activation(out=vn0[:], in_=gv[:], func=AF.Identity, bias=sd2[:, 2:3], scale=sd2[:, 1:2]),
                         reads=[('gv', 0), ('gv', 1), 'sd21', 'sd22'], writes=['vn0'])
                    P.op('dve', lambda v: v.tensor_tensor(out=vn0[:], in0=vn0[:], in1=lnG[:], op=ALU.mult),
                         reads=['vn0', 'lnG'], writes=['vn0'])
                    P.op('dve', lambda v: v.tensor_tensor(out=vnb[:], in0=vn0[:], in1=lnB[:], op=ALU.add),
                         reads=['vn0', 'lnB'], writes=['vnb'])
                    for g in range(8):
                        bk = 2 + g // 4
                        P.op('pe', lambda pe, g=g, bk=bk: pe.matmul(
                            banks[bk][:, (g % 4) * 128:(g % 4 + 1) * 128], lhsT=vnb[:, g * 128:(g + 1) * 128],
                            rhs=wsb[:, g, :], start=True, stop=True),
                            reads=['vnb', 'wsb'], writes=[('bq', bk, g % 4)], excl=B(bk))
                    for hh in range(2):
                        bk = 2 + hh
                        P.op('dve', lambda v, hh=hh, bk=bk: v.tensor_tensor(
                            out=tmpm[:, hh * 512:(hh + 1) * 512], in0=banks[bk][:, :], in1=bsb[:, hh * 512:(hh + 1) * 512],
                            op=ALU.add), reads=[('bq', bk, q) for q in range(4)] + ['bsb'], writes=[('tmpm', hh)], excl=B(bk))
                    P.op('pool', lambda gp, u=u, ob=ob: gp.tensor_tensor(
                        out=ob[:].rearrange("p c t -> p (c t)"), in0=tmpm[:], in1=u[:].rearrange("p c t -> p (c t)"), op=ALU.mult),
                        reads=[('tmpm', 0), ('tmpm', 1), uk], writes=[obk])
                    P.dma('sp', obTd[:, :, t * 128:(t + 1) * 128].rearrange("c p t -> p c t"), ob[:],
                          reads=[obk], writes=[('obTd2', t)])
                P.barrier()
                P.flush()

        if upto >= 3:
            with contextlib.ExitStack() as ph:
                KTh = sb(ph, "KTh", [128, 4, S], BF16)
                Vh = sb(ph, "Vh", [128, 64, 512], BF16)
                QTh = sb(ph, "QTh", [128, 4, OWN], BF16)
                mkf = sb(ph, "mkf", [128, 4, 128], F32)
                mk4 = sb(ph, "mk4", [128, 4, 4, 128], BF16)
                triI = sb(ph, "triI", [128, 128], BF16)
                triC = sb(ph, "triC", [128, 128], BF16)
                trif = sb(ph, "trif", [128, 128], F32)
                NS_ = 2
                eb = [[sb(ph, "eb%d_%d" % (q, i), [128, 512], F32) for i in range(2)] for q in range(NS_)]
                spb = [[sb(ph, "spb%d_%d" % (q, i), [128, 512], BF16) for i in range(2)] for q in range(NS_)]
                gb_ = [[sb(ph, "gb%d_%d" % (q, i), [128, 512], F32) for i in range(2)] for q in range(NS_)]
                wb_ = [[sb(ph, "wb%d_%d" % (q, i), [128, 512], BF16) for i in range(2)] for q in range(NS_)]
                w32 = [sb(ph, "w32_%d" % q, [128, 512], F32) for q in range(NS_)]
                oas = [[sb(ph, "oas%d_%d" % (q, i), [128, 4, 128], BF16) for i in range(2)] for q in range(NS_)]
                P.dma('sp', mkf[:], maskd, writes=['mkf'])
                for h in range(4):
                    P.op('pool', lambda g, h=h: g.tensor_copy(out=mk4[:, :, h, :], in_=mkf[:]), reads=['mkf'], writes=[('mk4', h)])
                mk4k = [('mk4', h) for h in range(4)]
                P.op('pool', lambda g: g.memset(trif[:], 1.0), writes=['trif'])
                P.op('pool', lambda g: g.affine_select(out=trif[:], in_=trif[:], pattern=[[-1, 128]], compare_op=ALU.is_ge,
                                                       fill=0.0, base=0, channel_multiplier=1), reads=['trif'], writes=['trif'])
                P.op('pool', lambda g: g.tensor_copy(out=triI[:], in_=trif[:]), reads=['trif'], writes=['triI'])
                P.op('pool', lambda g: g.memset(trif[:], 1.0), reads=['trif'], writes=['trif'])
                P.op('pool', lambda g: g.affine_select(out=trif[:], in_=trif[:], pattern=[[1, 128]], compare_op=ALU.is_gt,
                                                       fill=0.0, base=0, channel_multiplier=-1), reads=['trif'], writes=['trif'])
                P.op('pool', lambda g: g.tensor_copy(out=triC[:], in_=trif[:]), reads=['trif'], writes=['triC'])
                sc = 128.0 ** -0.5
                for hg in range(2):
                    for h in range(4):
                        P.dma('sp', KTh[:, h, :], KT[hg * 4 + h], writes=[('KTh', h)])
                    for q4 in range(4):
                        P.dma('act', Vh[:, q4 * 16:(q4 + 1) * 16, :],
                              VV[q4 * 16:(q4 + 1) * 16, :, hg * 512:(hg + 1) * 512].rearrange("b p f -> p b f"),
                              writes=[('Vh', q4)])
                    P.dma('sp', QTh[:], QTd[hg * 4:(hg + 1) * 4].rearrange("c p t -> p c t"), writes=['QTh'])
                    streams = [[(i, kb) for i in range(q, 16, NS_) for kb in range(4 * i + 3, -1, -1)] for q in range(NS_)]

                    def stageA(q, n):
                        i, kb = streams[q][n]
                        zb = 2 * q + (n % 2)
                        p = n % 2
                        e = eb[q][p]
                        sp_ = spb[q][p]
                        ek = 'e%d_%d' % (q, p)
                        sk = 'sp%d_%d' % (q, p)
                        for h in range(4):
                            P.op('pe', lambda pe, h=h, i=i, kb=kb, zb=zb: pe.matmul(
                                banks[zb][:, h * 128:(h + 1) * 128], lhsT=KTh[:, h, kb * 128:(kb + 1) * 128],
                                rhs=QTh[:, h, i * 128:(i + 1) * 128], start=True, stop=True),
                                reads=[('KTh', h), 'QTh'], writes=['b%d' % zb], excl=B(zb))
                        P.op('act', lambda a, zb=zb, e=e: a.activation(out=e[:], in_=banks[zb][:, :], func=AF.Exp, scale=sc),
                             reads=['b%d' % zb], writes=[ek], excl=B(zb))
                        P.op('act', lambda a, e=e, sp_=sp_: a.activation(out=sp_[:], in_=e[:], func=AF.Ln, bias=1.0, scale=1.0),
                             reads=[ek], writes=[sk])
                        if kb >= 4 * i:
                            m = kb - 4 * i
                            P.op('pool', lambda g, sp_=sp_, m=m: g.tensor_tensor(
                                out=sp_[:], in0=sp_[:], in1=mk4[:, m, :, :].rearrange("p h t -> p (h t)"), op=ALU.mult),
                                reads=[sk] + mk4k, writes=[sk])

                    def stageB(q, n, part):
                        i, kb = streams[q][n]
                        p = n % 2
                        e = eb[q][p]
                        sp_ = spb[q][p]
                        g_ = gb_[q][p]
                        w_ = wb_[q][p]
                        ek = 'e%d_%d' % (q, p)
                        sk = 'sp%d_%d' % (q, p)
                        gk = 'g%d_%d' % (q, p)
                        wk = 'w%d_%d' % (q, p)
                        btb = 4 + q
                        otb = 6 + q
                        first = (kb == 4 * i + 3)
                        last = (kb == 0)
                        if part == 1:
                            P.op('pe', lambda pe: pe.matmul(banks[btb][:, :], lhsT=triI[:], rhs=sp_[:], start=first, stop=False),
                                 reads=['triI', sk], writes=['b%d' % btb], excl=B(btb))
                            P.op('act', lambda a: a.activation(out=g_[:], in_=banks[btb][:, :], func=AF.Exp, scale=-1.0),
                                 reads=['b%d' % btb], writes=[gk], excl=B(btb))
                            return
                        P.op('pe', lambda pe: pe.matmul(banks[btb][:, :], lhsT=triC[:], rhs=sp_[:], start=False, stop=last),
                             reads=['triC', sk], writes=['b%d' % btb], excl=B(btb))
                        if kb >= 4 * i:
                            m = kb - 4 * i
                            P.op('dve', lambda v: v.tensor_tensor(out=w32[q][:], in0=e[:], in1=g_[:], op=ALU.mult),
                                 reads=[ek, gk], writes=['w32_%d' % q])
                            P.op('dve', lambda v: v.tensor_tensor(
                                out=w_[:], in0=w32[q][:], in1=mk4[:, m, :, :].rearrange("p h t -> p (h t)"), op=ALU.mult),
                                reads=['w32_%d' % q] + mk4k, writes=[wk])
                        else:
                            P.op('dve', lambda v: v.tensor_tensor(out=w_[:], in0=e[:], in1=g_[:], op=ALU.mult),
                                 reads=[ek, gk], writes=[wk])
                        for h in range(4):
                            P.op('pe', lambda pe, h=h: pe.matmul(
                                banks[otb][:, h * 128:(h + 1) * 128], lhsT=Vh[:, kb, h * 128:(h + 1) * 128],
                                rhs=w_[:, h * 128:(h + 1) * 128], start=(first and h == 0), stop=last),
                                reads=[('Vh', kb // 16), wk], writes=['b%d' % otb], excl=B(otb))
                        if last:
                            oa = oas[q][(i // NS_) % 2]
                            ok_ = 'oas%d_%d' % (q, (i // NS_) % 2)
                            evac_copy(oa[:].rearrange("p c t -> p (c t)"), otb, [ok_])
                            P.dma('sp', oaTd[hg * 4:(hg + 1) * 4, :, i * 128:(i + 1) * 128].rearrange("c p t -> p c t"), oa[:],
                                  reads=[ok_], writes=[('oaTd', hg, i)])

                    lens = [len(st_) for st_ in streams]
                    for q in range(NS_):
                        stageA(q, 0)
                    for n in range(max(lens)):
                        for q in range(NS_):
                            if n < lens[q]:
                                stageB(q, n, 1)
                        for q in range(NS_):
                            if n + 1 < lens[q]:
                                stageA(q, n + 1)
                        for q in range(NS_):
                            if n < lens[q]:
                                stageB(q, n, 2)
                P.barrier()
                P.flush()
        mTd = dscr("mTd", [NKC, 128, OWN], BF16)
        y2d = dscr("y2d", [OWN, D], F32) if debug else None
        if debug:
            dbg_wdh = dscr("dbg_wdh", [2, 128, 8 * NEXP], F32)
            dbg_hact = dscr("dbg_hact", [2, 2, 128, 4 * 512], BF16)
            dbg_h2h = dscr("dbg_h2h", [2, 128, NKC * 1024], BF16)
            dbg_y0 = dscr("dbg_y0", [2, 128, 8 * D], F32)
            dbg_pad = dscr("dbg_pad", [2, 128, 1024], F32)

        def bcast_load(ph, name, src_row):
            t = sb(ph, name, [128, src_row.shape[-1]], F32)
            P.dma('sp', t[:], src_row.partition_broadcast(128).rearrange("p o f -> p (o f)"), writes=[name])
            return t

        if upto >= 4:
            with contextlib.ExitStack() as ph:
                oaT = sb(ph, "oaT", [128, 8, OWN], BF16)
                obT = sb(ph, "obT", [128, 8, OWN], BF16)
                P.dma('sp', oaT[:], oaTd.rearrange("c p t -> p c t"), writes=['oaT'])
                P.dma('act', obT[:], obTd.rearrange("c p t -> p c t"), writes=['obT'])
                was = [sb(ph, "was%d" % i, [128, 8, 128], F32) for i in range(2)]
                wbs = [sb(ph, "wbs%d" % i, [128, 8, 128], F32) for i in range(2)]
                wab = [sb(ph, "wab%d" % i, [128, 8, 128], BF16) for i in range(2)]
                wbb = [sb(ph, "wbb%d" % i, [128, 8, 128], BF16) for i in range(2)]
                gas = [sb(ph, "gas%d" % i, [128, OWN], BF16) for i in range(2)]
                gbs = [sb(ph, "gbs%d" % i, [128, OWN], BF16) for i in range(2)]
                t1 = [sb(ph, "t1%d" % i, [128, 512], F32) for i in range(2)]
                t2 = [sb(ph, "t2%d" % i, [128, 512], F32) for i in range(2)]
                mst = [sb(ph, "mst%d" % i, [128, OWN], BF16) for i in range(2)]
                nn = 0
                for nb in range(16):
                    p2 = nb % 2
                    P.dma('sp', was[p2][:], w_pa[:, nb * 128:(nb + 1) * 128].rearrange("(c p) n -> p c n", p=128), writes=['was%d' % p2])
                    P.dma('sp', wbs[p2][:], w_pb[:, nb * 128:(nb + 1) * 128].rearrange("(c p) n -> p c n", p=128), writes=['wbs%d' % p2])
                    P.op('pool', lambda g, p2=p2: g.tensor_copy(out=wab[p2][:], in_=was[p2][:]), reads=['was%d' % p2], writes=['wab%d' % p2])
                    P.op('pool', lambda g, p2=p2: g.tensor_copy(out=wbb[p2][:], in_=wbs[p2][:]), reads=['wbs%d' % p2], writes=['wbb%d' % p2])
                    P.dma('act', gas[p2][:], gaTd[nb], writes=['gas%d' % p2])
                    P.dma('act', gbs[p2][:], gbTd[nb], writes=['gbs%d' % p2])
                    for oc in range(4):
                        q2 = nn % 2
                        nn += 1
                        ba = 0 + q2
                        bb = 2 + q2
                        for kc in range(8):
                            P.op('pe', lambda pe, kc=kc, p2=p2, oc=oc, ba=ba: pe.matmul(
                                banks[ba][:, :], lhsT=wab[p2][:, kc, :], rhs=oaT[:, kc, oc * 512:(oc + 1) * 512],
                                start=(kc == 0), stop=(kc == 7)), reads=['wab%d' % p2, 'oaT'], writes=['b%d' % ba], excl=B(ba))
                        for kc in range(8):
                            P.op('pe', lambda pe, kc=kc, p2=p2, oc=oc, bb=bb: pe.matmul(
                                banks[bb][:, :], lhsT=wbb[p2][:, kc, :], rhs=obT[:, kc, oc * 512:(oc + 1) * 512],
                                start=(kc == 0), stop=(kc == 7)), reads=['wbb%d' % p2, 'obT'], writes=['b%d' % bb], excl=B(bb))
                        P.op('dve', lambda v, p2=p2, oc=oc, ba=ba, q2=q2: v.tensor_tensor(
                            out=t1[q2][:], in0=banks[ba][:, :], in1=gas[p2][:, oc * 512:(oc + 1) * 512], op=ALU.mult),
                            reads=['b%d' % ba, 'gas%d' % p2], writes=['t1%d' % q2], excl=B(ba))
                        P.op('dve', lambda v, p2=p2, oc=oc, bb=bb, q2=q2: v.tensor_tensor(
                            out=t2[q2][:], in0=banks[bb][:, :], in1=gbs[p2][:, oc * 512:(oc + 1) * 512], op=ALU.mult),
                            reads=['b%d' % bb, 'gbs%d' % p2], writes=['t2%d' % q2], excl=B(bb))
                        P.op('pool', lambda g, p2=p2, oc=oc, q2=q2: g.tensor_tensor(
                            out=mst[p2][:, oc * 512:(oc + 1) * 512], in0=t1[q2][:], in1=t2[q2][:], op=ALU.add),
                            reads=['t1%d' % q2, 't2%d' % q2], writes=[('mst%d' % p2, oc)])
                    P.dma('sp', mTd[nb], mst[p2][:], reads=[('mst%d' % p2, oc) for oc in range(4)], writes=[('mTd', nb)])
                P.barrier()
                P.flush()

        if upto >= 5:
            with contextlib.ExitStack() as ph:
                Wo = sb(ph, "Wo", [128, NKC, D], BF16)
                wst = [sb(ph, "wsto%d" % i, [128, 2048], F32) for i in range(2)]
                for kc in range(NKC):
                    w = wst[kc % 2]
                    wk = 'wsto%d' % (kc % 2)
                    P.dma('sp', w[:], w_out[kc * 128:(kc + 1) * 128, :], writes=[wk])
                    P.op('pool', lambda g, w=w, kc=kc: g.tensor_copy(out=Wo[:, kc, :], in_=w[:]), reads=[wk], writes=[('Wo', kc)])
                G1 = bcast_load(ph, "G1", modv[0:1, 2 * D:3 * D])
                L1g = bcast_load(ph, "L1g", ln1_g)
                L1b = bcast_load(ph, "L1b", ln1_b)
                brt = bcast_load(ph, "brt", b_rt)
                Wrt = sb(ph, "Wrt", [128, NKC, 36], F32)
                P.dma('sp', Wrt[:], w_rt.rearrange("(c p) n -> p c n", p=128), writes=['Wrt'])
                lb = ln_bufs(ph)
                xin, xn, st, mv, sd, cnt = lb
                mts = [sb(ph, "mts%d" % i, [128, NKC, 128], BF16) for i in range(2)]
                tg = sb(ph, "tg", [128, D], F32)
                rt = sb(ph, "rt", [128, D], F32)
                x1t = [sb(ph, "x1t%d" % i, [128, D], F32) for i in range(2)]
                xnY = sb(ph, "xnY", [128, 2048], F32)
                stY = sb(ph, "stY", [128, 4, 6], F32)
                mvY = sb(ph, "mvY", [128, 2], F32)
                sdY = sb(ph, "sdY", [128, 4], F32)
                h2t = [sb(ph, "h2t%d" % i, [128, NKC, 128], BF16) for i in range(2)]
                h2f = sb(ph, "h2f", [128, NKC, 128], F32)
                lg = sb(ph, "lg", [128, 36], F32)
                rs = sb(ph, "rs", [128, 64], F32)
                wdt = [sb(ph, "wdt%d" % i, [128, 32], F32) for i in range(2)]
                def stageX4(t):
                    p2 = t % 2
                    x1 = x1t[p2]
                    x1k = "x1t%d" % p2
                    mt = mts[p2]
                    P.dma('sp', mt[:], mTd[:, :, t * 128:(t + 1) * 128].rearrange("c p t -> p c t"), writes=['mts%d' % p2])
                    xt = xin[t % 2]
                    xk = 'xin%d' % (t % 2)
                    P.dma('act', xt[:], xo[t * 128:(t + 1) * 128, :], writes=[xk])
                    for fb in range(4):
                        for kc in range(NKC):
                            P.op('pe', lambda pe, kc=kc, fb=fb, mt=mt: pe.matmul(
                                banks[fb][:, :], lhsT=mt[:, kc, :], rhs=Wo[:, kc, fb * 512:(fb + 1) * 512],
                                start=(kc == 0), stop=(kc == NKC - 1)),
                                reads=['mts%d' % p2, ('Wo', kc)], writes=['b%d' % fb], excl=B(fb))
                        P.op('dve', lambda v, fb=fb: v.tensor_tensor(
                            out=tg[:, fb * 512:(fb + 1) * 512], in0=banks[fb][:, :], in1=G1[:, fb * 512:(fb + 1) * 512], op=ALU.mult),
                            reads=['b%d' % fb, 'G1'], writes=[('tg', fb)], excl=B(fb))
                    P.op('dve', lambda v, xt=xt: v.scalar_tensor_tensor(out=rt[:], in0=xt[:], scalar=ALPHA, in1=tg[:],
                                                                       op0=ALU.mult, op1=ALU.add),
                         reads=[xk] + [('tg', fb) for fb in range(4)], writes=['rt'])
                    layer_norm_tile(rt, 'rt', xn, 'xnX', st, mv, sd, eps_t, sfx='_x')
                    P.op('dve', lambda v: v.tensor_tensor(out=xn[:], in0=xn[:], in1=L1g[:], op=ALU.mult), reads=['xnX', 'L1g'], writes=['xnX'])
                    P.op('pool', lambda g: g.tensor_tensor(out=x1[:], in0=xn[:], in1=L1b[:], op=ALU.add), reads=['xnX', 'L1b'], writes=[x1k])
                    P.dma('sp', x1d[t * 128:(t + 1) * 128, :], x1[:], reads=[x1k], writes=[('x1d', t)])

                def stageY4(t):
                    p2 = t % 2
                    x1 = x1t[p2]
                    x1k = 'x1t%d' % p2
                    h2 = h2t[p2]
                    layer_norm_tile(x1, x1k, xnY, 'xnY', stY, mvY, sdY, eps_t, sfx='_y')
                    transpose_evac(xnY, 'xnY', h2, 'h2t%d' % p2, 0, 3, fdst=h2f, bo=4)
                    P.dma('sp', h2Td[:, :, t * 128:(t + 1) * 128].rearrange("c p t -> p c t"), h2[:],
                          reads=[('h2t%d' % p2, kc, 0) for kc in range(NKC)], writes=[('h2Td', t)])
                    for kc in range(NKC):
                        P.op('pe', lambda pe, kc=kc: pe.matmul(banks[4][:, 0:36], lhsT=h2f[:, kc, :], rhs=Wrt[:, kc, :],
                                                                start=(kc == 0), stop=(kc == NKC - 1)),
                             reads=[('fdst', kc), 'Wrt'], writes=['b4'], excl=B(4))
                    P.op('dve', lambda v: v.tensor_tensor(out=lg[:], in0=banks[4][:, 0:36], in1=brt[:], op=ALU.add),
                         reads=['b4', 'brt'], writes=['lg'], excl=B(4))
                    wd_ = wdt[p2]
                    V = lambda f, r, w: P.op('dve', f, reads=r, writes=w)
                    V(lambda v: v.reduce_max(out=rs[:, 0:1], in_=lg[:, 0:4], axis=mybir.AxisListType.X), ['lg'], ['r0'])
                    V(lambda v: v.tensor_scalar(out=rs[:, 1:2], in0=rs[:, 0:1], scalar1=-1.0, scalar2=None, op0=ALU.mult), ['r0'], ['r1'])
                    P.op('act', lambda a: a.activation(out=rs[:, 4:8], in_=lg[:, 0:4], func=AF.Exp, bias=rs[:, 1:2], scale=1.0,
                                                       accum_out=rs[:, 2:3]), reads=['lg', 'r1'], writes=['r2'])
                    V(lambda v: v.reciprocal(out=rs[:, 3:4], in_=rs[:, 2:3]), ['r2'], ['r3'])
                    V(lambda v: v.tensor_scalar(out=rs[:, 8:12], in0=lg[:, 0:4], scalar1=rs[:, 0:1], scalar2=None, op0=ALU.is_equal),
                      ['lg', 'r0'], ['gm'])
                    V(lambda v: v.tensor_scalar(out=rs[:, 12:20], in0=lg[:, 4:12], scalar1=rs[:, 8:9], scalar2=None, op0=ALU.mult),
                      ['lg', 'gm'], ['sel'])
                    for g in range(1, 4):
                        V(lambda v, g=g: v.scalar_tensor_tensor(out=rs[:, 12:20], in0=lg[:, 4 + 8 * g:12 + 8 * g], scalar=rs[:, 8 + g:9 + g],
                                                                in1=rs[:, 12:20], op0=ALU.mult, op1=ALU.add), ['lg', 'gm', 'sel'], ['sel'])
                    V(lambda v: v.max(out=rs[:, 20:28], in_=rs[:, 12:20]), ['sel'], ['top8'])
                    V(lambda v: v.tensor_scalar(out=rs[:, 28:36], in0=rs[:, 12:20], scalar1=rs[:, 20:21], scalar2=None, op0=ALU.is_equal),
                      ['sel', 'top8'], ['m1'])
                    V(lambda v: v.tensor_scalar(out=rs[:, 36:44], in0=rs[:, 12:20], scalar1=rs[:, 21:22], scalar2=None, op0=ALU.is_equal),
                      ['sel', 'top8'], ['m2'])
                    V(lambda v: v.tensor_tensor(out=rs[:, 44:45], in0=rs[:, 21:22], in1=rs[:, 20:21], op=ALU.subtract), ['top8'], ['rd'])
                    P.op('act', lambda a: a.activation(out=rs[:, 45:46], in_=rs[:, 44:45], func=AF.Exp), reads=['rd'], writes=['red'])
                    V(lambda v: v.tensor_scalar(out=rs[:, 46:47], in0=rs[:, 45:46], scalar1=1.0, scalar2=None, op0=ALU.add), ['red'], ['rden'])
                    V(lambda v: v.reciprocal(out=rs[:, 47:48], in_=rs[:, 46:47]), ['rden'], ['rw1'])
                    V(lambda v: v.tensor_tensor(out=rs[:, 48:49], in0=rs[:, 45:46], in1=rs[:, 47:48], op=ALU.mult), ['red', 'rw1'], ['rw2'])
                    V(lambda v: v.tensor_tensor(out=rs[:, 49:50], in0=rs[:, 47:48], in1=rs[:, 3:4], op=ALU.mult), ['rw1', 'r3'], ['rw1p'])
                    V(lambda v: v.tensor_tensor(out=rs[:, 50:51], in0=rs[:, 48:49], in1=rs[:, 3:4], op=ALU.mult), ['rw2', 'r3'], ['rw2p'])
                    V(lambda v: v.tensor_scalar(out=rs[:, 52:60], in0=rs[:, 28:36], scalar1=rs[:, 49:50], scalar2=None, op0=ALU.mult),
                      ['m1', 'rw1p'], ['cw'])
                    V(lambda v: v.scalar_tensor_tensor(out=rs[:, 52:60], in0=rs[:, 36:44], scalar=rs[:, 50:51], in1=rs[:, 52:60],
                                                       op0=ALU.mult, op1=ALU.add), ['m2', 'rw2p', 'cw'], ['cw'])
                    for g in range(4):
                        V(lambda v, g=g, wd_=wd_: v.tensor_scalar(out=wd_[:, g * 8:(g + 1) * 8], in0=rs[:, 52:60], scalar1=rs[:, 8 + g:9 + g],
                                                                  scalar2=None, op0=ALU.mult), ['cw', 'gm'], [('wdt%d' % p2, g)])
                    P.dma('sp', wdd[t * 128:(t + 1) * 128, :], wd_[:], reads=[('wdt%d' % p2, g) for g in range(4)], writes=[('wdd', t)])

                stageX4(0)
                for t in range(16):
                    if t + 1 < 16:
                        stageX4(t + 1)
                    stageY4(t)
                P.barrier()
                P.flush()

        if upto >= 6:
            for half in range(2):
                with contextlib.ExitStack() as ph:
                    y2 = sb(ph, "y2", [128, 8, D], F32)
                    wdh = sb(ph, "wdh", [128, 8, NEXP], F32)
                    h2h = sb(ph, "h2h", [128, NKC, 1024], BF16)
                    P.dma('sp', h2h[:], h2Td[:, :, half * 1024:(half + 1) * 1024].rearrange("c p t -> p c t"), writes=['h2h'])
                    P.dma('sp', wdh[:], wdd[half * 1024:(half + 1) * 1024, :].rearrange("(c p) e -> p c e", p=128), writes=['wdh'])
                    for ti in range(8):
                        P.op('pool', lambda g, ti=ti: g.memset(y2[:, ti, :], 0.0), writes=[('y2', ti, fb) for fb in range(4)])
                    if debug:
                        P.dma('sp', dbg_y0[half], y2[:].rearrange("p a b -> p (a b)"),
                              reads=[('y2', ti, fb) for ti in range(8) for fb in range(4)], writes=['dbg_y0'])
                        P.dma('sp', dbg_wdh[half], wdh[:].rearrange("p a b -> p (a b)"), reads=['wdh'], writes=['dbg_wdh'])
                        P.dma('sp', dbg_h2h[half], h2h[:].rearrange("p a b -> p (a b)"), reads=['h2h'], writes=['dbg_h2h'])
                    with contextlib.ExitStack() as ph2:
                        hact = [sb(ph2, "hact%d" % i, [128, 4, 512], BF16) for i in range(2)]
                        sil = [sb(ph2, "sil%d" % i, [128, 512], F32) for i in range(2)]
                        wg = sb(ph2, "wg", [128, NKC, DEXP], BF16)
                        wu = sb(ph2, "wu", [128, NKC, DEXP], BF16)
                        wdn = sb(ph2, "wdn", [128, 4, D], BF16)
                        NSTG = 3
                        stg = [sb(ph2, "stg%d" % i, [128, 1024], F32) for i in range(NSTG)]
                        pad = sb(ph2, "pad", [128, 1024], F32)
                        P.op('pool', lambda g: g.memset(pad[:], 0.0), writes=['pad'])
                        ns = 0
                        nsl = 0
                        nh = 0
                        nbk = 0
                        for e in range(NEXP):
                            for (src, dstw, dk_, nhalf, ceng) in ((w_gate[e], wg, 'wg', 2, 'pool'), (w_up[e], wu, 'wu', 2, 'act'),
                                                                  (w_down[e], wdn, 'wdn', 2, 'pool')):
                                for hf in range(8):
                                    sg = stg[ns % NSTG]
                                    sk = 'stg%d' % (ns % NSTG)
                                    ns += 1
                                    if dk_ == 'wdn':
                                        dcw, hh = hf // 2, hf % 2
                                        P.dma('sp', sg[:], src[dcw * 128:(dcw + 1) * 128, hh * 1024:(hh + 1) * 1024], writes=[sk])
                                        dst_ap = dstw[:, dcw, hh * 1024:(hh + 1) * 1024]
                                    else:
                                        P.dma('sp', sg[:].rearrange("p (c n) -> p c n", c=2),
                                              src[hf * 256:(hf + 1) * 256, :].rearrange("(c p) n -> p c n", p=128), writes=[sk])
                                        dst_ap = dstw[:, hf * 2:(hf + 1) * 2, :].rearrange("p c n -> p (c n)")
                                    if ceng == 'pool':
                                        P.op('pool', lambda g, sg=sg, dst_ap=dst_ap: g.tensor_copy(out=dst_ap, in_=sg[:]),
                                             reads=[sk], writes=[(dk_, hf)])
                                    else:
                                        P.op('act', lambda a, sg=sg, dst_ap=dst_ap: a.copy(out=dst_ap, in_=sg[:]),
                                             reads=[sk], writes=[(dk_, hf)])
                            for ch in range(2):
                                ha = hact[nh % 2]
                                hk = 'hact%d' % (nh % 2)
                                nh += 1
                                for dc in range(4):
                                    bg = nbk % 2
                                    bu = 2 + nbk % 2
                                    nbk += 1
                                    for kc in range(NKC):
                                        P.op('pe', lambda pe, kc=kc, dc=dc, ch=ch, bg=bg: pe.matmul(
                                            banks[bg][:, :], lhsT=wg[:, kc, dc * 128:(dc + 1) * 128], rhs=h2h[:, kc, ch * 512:(ch + 1) * 512],
                                            start=(kc == 0), stop=(kc == NKC - 1)), reads=[('wg', kc // 2), 'h2h'], writes=['b%d' % bg], excl=B(bg))
                                    for kc in range(NKC):
                                        P.op('pe', lambda pe, kc=kc, dc=dc, ch=ch, bu=bu: pe.matmul(
                                            banks[bu][:, :], lhsT=wu[:, kc, dc * 128:(dc + 1) * 128], rhs=h2h[:, kc, ch * 512:(ch + 1) * 512],
                                            start=(kc == 0), stop=(kc == NKC - 1)), reads=[('wu', kc // 2), 'h2h'], writes=['b%d' % bu], excl=B(bu))
                                    sl = sil[nsl % 2]
                                    slk = 'sil%d' % (nsl % 2)
                                    nsl += 1
                                    P.op('act', lambda a, sl=sl, bg=bg: a.activation(out=sl[:], in_=banks[bg][:, :], func=AF.Silu),
                                         reads=['b%d' % bg], writes=[slk], excl=B(bg))
                                    P.op('dve', lambda v, sl=sl, bu=bu, ha=ha, dc=dc: v.tensor_tensor(
                                        out=ha[:, dc, :], in0=banks[bu][:, :], in1=sl[:], op=ALU.mult),
                                        reads=['b%d' % bu, slk], writes=[(hk, dc)], excl=B(bu))
                                for tl in range(4):
                                    ti = ch * 4 + tl
                                    for fb in range(4):
                                        bd = 4 + fb
                                        for dc in range(4):
                                            P.op('pe', lambda pe, dc=dc, tl=tl, fb=fb, bd=bd, ha=ha: pe.matmul(
                                                banks[bd][:, :], lhsT=ha[:, dc, tl * 128:(tl + 1) * 128], rhs=wdn[:, dc, fb * 512:(fb + 1) * 512],
                                                start=(dc == 0), stop=(dc == 3)), reads=[(hk, dc), ('wdn', dc * 2 + fb // 2)], writes=['b%d' % bd], excl=B(bd))
                                        P.op('dve', lambda v, ti=ti, fb=fb, bd=bd, e=e: v.scalar_tensor_tensor(
                                            out=y2[:, ti, fb * 512:(fb + 1) * 512], in0=banks[bd][:, :], scalar=wdh[:, ti, e:e + 1],
                                            in1=y2[:, ti, fb * 512:(fb + 1) * 512], op0=ALU.mult, op1=ALU.add),
                                            reads=['b%d' % bd, 'wdh', ('y2', ti, fb)], writes=[('y2', ti, fb)], excl=B(bd))
                        if debug:
                            P.dma('sp', dbg_pad[half], pad[:], reads=['pad'], writes=['dbg_pad'])
                            for q in range(2):
                                P.dma('sp', dbg_hact[half, q], hact[q][:].rearrange("p a b -> p (a b)"),
                                      reads=[('hact%d' % q, dc) for dc in range(4)], writes=[('dbg_hact', q)])
                    P.barrier()
                    if debug:
                        for ti in range(8):
                            t = half * 8 + ti
                            P.dma('sp', y2d[t * 128:(t + 1) * 128, :], y2[:, ti, :],
                                  reads=[('y2', ti, fb) for fb in range(4)], writes=[('y2d', t)])
                    with contextlib.ExitStack() as ph3:
                        G2 = bcast_load(ph3, "G2", modv[0:1, 5 * D:6 * D])
                        L2g = bcast_load(ph3, "L2g", ln2_g)
                        L2b = bcast_load(ph3, "L2b", ln2_b)
                        lb = ln_bufs(ph3)
                        xin, xn, st, mv, sd, cnt = lb
                        tg2 = sb(ph3, "tg2", [128, D], F32)
                        rt2 = sb(ph3, "rt2", [128, D], F32)
                        ot = [sb(ph3, "ot%d" % i, [128, D], F32) for i in range(2)]
                        for ti in range(8):
                            t = half * 8 + ti
                            xt = xin[ti % 2]
                            xk = 'xin%d' % (ti % 2)
                            P.dma('act', xt[:], x1d[t * 128:(t + 1) * 128, :], writes=[xk])
                            P.op('pool', lambda g, ti=ti: g.tensor_tensor(out=tg2[:], in0=y2[:, ti, :], in1=G2[:], op=ALU.mult),
                                 reads=[('y2', ti, fb) for fb in range(4)] + ['G2'], writes=['tg2'])
                            P.op('dve', lambda v, xt=xt: v.scalar_tensor_tensor(out=rt2[:], in0=xt[:], scalar=ALPHA, in1=tg2[:],
                                                                               op0=ALU.mult, op1=ALU.add), reads=[xk, 'tg2'], writes=['rt2'])
                            layer_norm_tile(rt2, 'rt2', xn, 'xn', st, mv, sd, eps_t)
                            o = ot[ti % 2]
                            ok_ = 'ot%d' % (ti % 2)
                            P.op('dve', lambda v: v.tensor_tensor(out=xn[:], in0=xn[:], in1=L2g[:], op=ALU.mult), reads=['xn', 'L2g'], writes=['xn'])
                            P.op('pool', lambda g, o=o: g.tensor_tensor(out=o[:], in0=xn[:], in1=L2b[:], op=ALU.add), reads=['xn', 'L2b'], writes=[ok_])
                            P.dma('sp', out[t * 128:(t + 1) * 128, :], o[:], reads=[ok_], writes=[('out', t)])
                    P.barrier()
                    P.flush()
        P.barrier()
        P.flush()
    return nc


def make_in_maps(inputs):
    x = np.asarray(inputs["x"], dtype=np.float32)
    c = np.asarray(inputs["c"], dtype=np.float32)
    g = lambda k: np.ascontiguousarray(np.asarray(inputs[k], dtype=np.float32)[0])
    w_rt = np.ascontiguousarray(np.concatenate([g("w_group"), g("w_router")], axis=1))
    b_rt = np.ascontiguousarray(np.concatenate([g("b_group"), g("b_router")], axis=0)[None, :])
    sgu_wT = np.ascontiguousarray(g("sgu_w").transpose(2, 0, 1))
    shared = {
        "w_ada": g("w_ada"), "b_ada": g("b_ada")[None, :], "w_in": g("w_in"),
        "sgu_wT": sgu_wT, "sgu_b": g("sgu_b").reshape(1, -1),
        "sgu_ln_g": g("sgu_ln_g")[None, :], "sgu_ln_b": g("sgu_ln_b")[None, :],
        "w_proj_a": g("w_proj_a"), "w_proj_b": g("w_proj_b"), "w_out": g("w_out"),
        "ln1_g": g("ln1_g")[None, :], "ln1_b": g("ln1_b")[None, :],
        "w_rt": w_rt, "b_rt": b_rt,
        "w_gate": g("w_gate"), "w_up": g("w_up"), "w_down": g("w_down"),
        "ln2_g": g("ln2_g")[None, :], "ln2_b": g("ln2_b")[None, :],
    }
    maps = []
    for core in range(8):
        b = core // 4
        m = dict(shared)
        j = core % 4
        m["xb"] = np.ascontiguousarray(x[b])
        m["xo"] = np.ascontiguousarray(x[b].reshape(16, 4, 128, D)[:, j].reshape(OWN, D))
        mk = np.zeros((128, 4, 128), np.float32)
        for mm in range(4):
            if mm < j:
                mk[:, mm, :] = 1.0
            elif mm == j:
                mk[:, mm, :] = np.triu(np.ones((128, 128), np.float32), k=1)
        m["maskd"] = mk
        m["cT"] = np.ascontiguousarray(c[b].reshape(NKC, 128).T)
        maps.append(m)
    return maps


_NC_CACHE = {}


def kernel(**inputs):
    if "nc" not in _NC_CACHE:
        _NC_CACHE["nc"] = build()
    nc = _NC_CACHE["nc"]
    maps = make_in_maps(inputs)
    res = run_bass_kernel_spmd(nc, maps, core_ids=list(range(8)))
    full = np.zeros((2, S, D), np.float32)
    for core in range(8):
        b, j = core // 4, core % 4
        o = np.asarray(res.results[core]["out"], dtype=np.float32).reshape(16, 128, D)
        full[b].reshape(16, 4, 128, D)[:, j] = o
    return full
```
